# Optimizing a Trainium2 kernel written in Bass

```python
import math
import jax
import jax.numpy as jnp
from jax import lax
import numpy as np

D_MODEL = 1024
BATCH = 8
SEQ = 2048
DEPTH = 2

GRID_W = 64
CTX_LEN = 256
N_DIR = 2
EPS = 1e-6

ML_HEADS = 4
ML_HEAD_DIM = 128
ML_WIDTH = ML_HEADS * ML_HEAD_DIM
ML_CHUNK = 64

SSD_HEADS = 8
SSD_HEAD_DIM = 64
SSD_WIDTH = SSD_HEADS * SSD_HEAD_DIM
SSD_STATE = 64
SSD_GROUPS = 2
SSD_CONV = 3
SSD_CHUNK = 64
SSD_XBC = SSD_WIDTH + 2 * SSD_GROUPS * SSD_STATE

ATT_Q_HEADS = 8
ATT_KV_HEADS = 2
ATT_HEAD_DIM = 64
ATT_WIDTH = ATT_Q_HEADS * ATT_HEAD_DIM
ATT_KV_WIDTH = ATT_KV_HEADS * ATT_HEAD_DIM
WINDOW = 128
ATT_BLOCK = 128
ROPE_BASE = 10000.0

MIX_WIDTH = ML_WIDTH + SSD_WIDTH + ATT_WIDTH
D_FF = 4 * D_MODEL

IN_LAYOUT = (
    ('ml_q', ML_WIDTH), ('ml_k', ML_WIDTH), ('ml_v', ML_WIDTH), ('ml_o', ML_WIDTH),
    ('ml_i', N_DIR * ML_HEADS), ('ml_f', N_DIR * ML_HEADS),
    ('ssd_z', SSD_WIDTH), ('ssd_xbc', SSD_XBC), ('ssd_dt', N_DIR * SSD_HEADS),
    ('att_q', ATT_WIDTH), ('att_k', ATT_KV_WIDTH), ('att_v', ATT_KV_WIDTH),
)
IN_WIDTH = (4 * ML_WIDTH + 2 * N_DIR * ML_HEADS + SSD_WIDTH + SSD_XBC + N_DIR * SSD_HEADS
            + ATT_WIDTH + 2 * ATT_KV_WIDTH)

kernel_name = 'hybrid_mlstm_ssd_swa_dit_block'


def rms_norm(x, g):
    xf = x.astype(jnp.float32)
    y = xf * lax.rsqrt(jnp.mean(xf * xf, axis=-1, keepdims=True) + EPS)
    return (y * g.astype(jnp.float32)).astype(x.dtype)


def modulate(h, shift, scale):
    return h * (1 + scale) + shift


def split_cols(u):
    out, off = {}, 0
    for name, width in IN_LAYOUT:
        out[name] = u[..., off:off + width]
        off += width
    return out


def to_chunks(a, q):
    bsz, n = a.shape[:2]
    a = a.reshape((bsz, n // q, q) + a.shape[2:])
    return jnp.moveaxis(jnp.moveaxis(a, 1, 0), 3, 2)


def from_chunks(a):
    nc, bsz, h, q, d = a.shape
    return jnp.moveaxis(jnp.moveaxis(a, 0, 1), 2, 3).reshape(bsz, nc * q, h, d)


def axial_rope(x, rows, cols):
    half = x.shape[-1] // 2
    nfreq = half // 2
    inv = ROPE_BASE ** (-jnp.arange(nfreq, dtype=jnp.float32) / nfreq)

    def rot(xa, pos):
        ang = pos.astype(jnp.float32)[:, None] * inv
        cos, sin = jnp.cos(ang)[None, :, None, :], jnp.sin(ang)[None, :, None, :]
        x1, x2 = xa[..., :nfreq].astype(jnp.float32), xa[..., nfreq:].astype(jnp.float32)
        return jnp.concatenate([x1 * cos - x2 * sin, x2 * cos + x1 * sin], axis=-1)

    return jnp.concatenate([rot(x[..., :half], rows), rot(x[..., half:], cols)], axis=-1).astype(x.dtype)


def depthwise_conv(u, w, b):
    pad = (SSD_CONV - 1) // 2
    y = lax.conv_general_dilated(u, w[:, None, :].astype(u.dtype), window_strides=(1,),
                                 padding=((pad, pad),), dimension_numbers=('NWC', 'WIO', 'NWC'),
                                 feature_group_count=u.shape[-1])
    return y + b.astype(u.dtype)


def mlstm_chunk_scan(q, k, v, log_i, log_f, state, with_outputs):
    tri = jnp.tril(jnp.ones((ML_CHUNK, ML_CHUNK), dtype=bool))

    def step(carry, inp):
        cmat, nvec, m = carry
        qc, kc, vc, li, lf = inp
        b = jnp.cumsum(lf, axis=-1)
        b_end = b[..., -1]
        wlog = b_end[..., None] - b + li
        m_new = jnp.maximum(b_end + m, jnp.max(wlog, axis=-1))
        w = jnp.exp(wlog - m_new[..., None])
        decay = jnp.exp(b_end + m - m_new)
        c_new = decay[..., None, None] * cmat + jnp.einsum('bhs,bhsk,bhsv->bhkv', w, kc, vc)
        n_new = decay[..., None] * nvec + jnp.einsum('bhs,bhsk->bhk', w, kc)
        if not with_outputs:
            return (c_new, n_new, m_new), None
        dlog = jnp.where(tri, b[..., :, None] - b[..., None, :] + li[..., None, :], -jnp.inf)
        inter = b + m[..., None]
        m_t = jnp.maximum(inter, jnp.max(dlog, axis=-1))
        s = jnp.einsum('bhtd,bhsd->bhts', qc, kc) * jnp.exp(dlog - m_t[..., None])
        g = jnp.exp(inter - m_t)
        num = jnp.einsum('bhts,bhsv->bhtv', s, vc) + g[..., None] * jnp.einsum('bhtk,bhkv->bhtv', qc, cmat)
        den = jnp.sum(s, axis=-1) + g * jnp.einsum('bhtk,bhk->bht', qc, nvec)
        h = num / jnp.maximum(jnp.abs(den), jnp.exp(-m_t))[..., None]
        return (c_new, n_new, m_new), h

    inputs = tuple(to_chunks(t, ML_CHUNK) for t in (q, k, v, log_i, log_f))
    state, hs = lax.scan(step, state, inputs)
    return (from_chunks(hs) if with_outputs else None), state


def ssd_chunk_scan(x, bm, cm, dt, a_head, state, with_outputs):
    la = dt * a_head
    tri = jnp.tril(jnp.ones((SSD_CHUNK, SSD_CHUNK), dtype=bool))

    def step(h, inp):
        xc, bc, cc, dtc, lac = inp
        acum = jnp.cumsum(lac, axis=-1)
        a_end = acum[..., -1]
        w = jnp.exp(a_end[..., None] - acum) * dtc
        h_new = jnp.exp(a_end)[..., None, None] * h + jnp.einsum('bhs,bhsp,bhsn->bhpn', w, xc, bc)
        if not with_outputs:
            return h_new, None
        seg = jnp.exp(jnp.where(tri, acum[..., :, None] - acum[..., None, :], -jnp.inf))
        scores = jnp.einsum('bhtn,bhsn->bhts', cc, bc) * seg * dtc[..., None, :]
        y = (jnp.einsum('bhts,bhsp->bhtp', scores, xc)
             + jnp.exp(acum)[..., None] * jnp.einsum('bhtn,bhpn->bhtp', cc, h))
        return h_new, y

    inputs = tuple(to_chunks(t, SSD_CHUNK) for t in (x, bm, cm, dt, la))
    state, ys = lax.scan(step, state, inputs)
    return (from_chunks(ys) if with_outputs else None), state


def orient_fn(d):
    return (lambda a: jnp.flip(a, axis=1)) if d == 1 else (lambda a: a)


def mlstm_mixer(pl, pc, b_i, b_f, norm_g, need_ctx_out):
    f32 = jnp.float32

    def prep(p):
        bsz, n = p['ml_q'].shape[:2]
        shp, gshp = (bsz, n, ML_HEADS, ML_HEAD_DIM), (bsz, n, N_DIR, ML_HEADS)
        q = p['ml_q'].astype(f32).reshape(shp)
        k = p['ml_k'].astype(f32).reshape(shp) * (ML_HEAD_DIM ** -0.5)
        v = p['ml_v'].astype(f32).reshape(shp)
        log_i = p['ml_i'].astype(f32).reshape(gshp) + b_i.astype(f32)
        log_f = jax.nn.log_sigmoid(p['ml_f'].astype(f32).reshape(gshp) + b_f.astype(f32))
        return q, k, v, log_i, log_f

    def finish(h, p):
        bsz, n = h.shape[:2]
        h = rms_norm(h, norm_g.reshape(ML_HEADS, ML_HEAD_DIM)).reshape(bsz, n, ML_WIDTH)
        return (h * jax.nn.sigmoid(p['ml_o'].astype(f32))).astype(p['ml_o'].dtype)

    lat, con = prep(pl), prep(pc)
    bsz = lat[0].shape[0]
    h_lat, h_ctx = [], []
    for d in range(N_DIR):
        orient = orient_fn(d)
        state0 = (jnp.zeros((bsz, ML_HEADS, ML_HEAD_DIM, ML_HEAD_DIM), f32),
                  jnp.zeros((bsz, ML_HEADS, ML_HEAD_DIM), f32),
                  jnp.zeros((bsz, ML_HEADS), f32))
        args_c = [orient(a) for a in (con[0], con[1], con[2], con[3][:, :, d], con[4][:, :, d])]
        args_l = [orient(a) for a in (lat[0], lat[1], lat[2], lat[3][:, :, d], lat[4][:, :, d])]
        hc, st = mlstm_chunk_scan(*args_c, state0, need_ctx_out)
        hl, _ = mlstm_chunk_scan(*args_l, st, True)
        h_lat.append(orient(hl))
        if need_ctx_out:
            h_ctx.append(orient(hc))
    out_l = finish(h_lat[0] + h_lat[1], pl)
    out_c = finish(h_ctx[0] + h_ctx[1], pc) if need_ctx_out else None
    return out_l, out_c


def ssd_mixer(pl, pc, conv_w, conv_b, a_log, dt_bias, d_skip, norm_g, need_ctx_out):
    f32 = jnp.float32
    gn = SSD_GROUPS * SSD_STATE
    rep = SSD_HEADS // SSD_GROUPS

    def prep(p):
        bsz, n = p['ssd_xbc'].shape[:2]
        xbc = jax.nn.silu(depthwise_conv(p['ssd_xbc'], conv_w, conv_b)).astype(f32)
        xs = xbc[..., :SSD_WIDTH].reshape(bsz, n, SSD_HEADS, SSD_HEAD_DIM)
        bm = jnp.repeat(xbc[..., SSD_WIDTH:SSD_WIDTH + gn].reshape(bsz, n, SSD_GROUPS, SSD_STATE), rep, axis=2)
        cm = jnp.repeat(xbc[..., SSD_WIDTH + gn:].reshape(bsz, n, SSD_GROUPS, SSD_STATE), rep, axis=2)
        dt = jax.nn.softplus(p['ssd_dt'].astype(f32).reshape(bsz, n, N_DIR, SSD_HEADS) + dt_bias.astype(f32))
        return xs, bm, cm, dt

    def finish(y, xs, p):
        bsz, n = y.shape[:2]
        y = (y + d_skip.astype(f32)[:, None] * xs).reshape(bsz, n, SSD_WIDTH)
        return rms_norm(y * jax.nn.silu(p['ssd_z'].astype(f32)), norm_g).astype(p['ssd_z'].dtype)

    a = -jnp.exp(a_log.astype(f32))
    lat, con = prep(pl), prep(pc)
    bsz = lat[0].shape[0]
    y_lat, y_ctx = [], []
    for d in range(N_DIR):
        orient = orient_fn(d)
        state0 = jnp.zeros((bsz, SSD_HEADS, SSD_HEAD_DIM, SSD_STATE), f32)
        args_c = [orient(t) for t in (con[0], con[1], con[2], con[3][:, :, d])]
        args_l = [orient(t) for t in (lat[0], lat[1], lat[2], lat[3][:, :, d])]
        yc, st = ssd_chunk_scan(*args_c, a[d], state0, need_ctx_out)
        yl, _ = ssd_chunk_scan(*args_l, a[d], st, True)
        y_lat.append(orient(yl))
        if need_ctx_out:
            y_ctx.append(orient(yc))
    out_l = finish(y_lat[0] + y_lat[1], lat[0], pl)
    out_c = finish(y_ctx[0] + y_ctx[1], con[0], pc) if need_ctx_out else None
    return out_l, out_c


def window_attention(pl, pc, qn_g, kn_g, sink, rows, cols, need_ctx_out):
    f32 = jnp.float32
    bsz, n = pl['att_q'].shape[:2]
    lc = pc['att_k'].shape[1]
    rep = ATT_Q_HEADS // ATT_KV_HEADS
    dh = ATT_HEAD_DIM
    scale = dh ** -0.5
    ql = axial_rope(rms_norm(pl['att_q'].reshape(bsz, n, ATT_Q_HEADS, dh), qn_g), rows, cols)
    kl = axial_rope(rms_norm(pl['att_k'].reshape(bsz, n, ATT_KV_HEADS, dh), kn_g), rows, cols)
    vl = pl['att_v'].reshape(bsz, n, ATT_KV_HEADS, dh)
    kc = rms_norm(pc['att_k'].reshape(bsz, lc, ATT_KV_HEADS, dh), kn_g)
    vc = pc['att_v'].reshape(bsz, lc, ATT_KV_HEADS, dh)
    sink_g = sink.astype(f32).reshape(ATT_KV_HEADS, rep)

    nb = n // ATT_BLOCK
    qb = ql.reshape(bsz, nb, ATT_BLOCK, ATT_KV_HEADS, rep, dh)

    def band(t):
        tp = jnp.pad(t, ((0, 0), (ATT_BLOCK, ATT_BLOCK), (0, 0), (0, 0)))
        tp = tp.reshape(bsz, nb + 2, ATT_BLOCK, ATT_KV_HEADS, dh)
        return jnp.concatenate([tp[:, :-2], tp[:, 1:-1], tp[:, 2:]], axis=2)

    kb, vb = band(kl), band(vl)
    blk = jnp.arange(nb)[:, None, None]
    qpos = blk * ATT_BLOCK + jnp.arange(ATT_BLOCK)[None, :, None]
    kpos = (blk - 1) * ATT_BLOCK + jnp.arange(3 * ATT_BLOCK)[None, None, :]
    valid = (jnp.abs(kpos - qpos) <= WINDOW) & (kpos >= 0) & (kpos < n)
    s_band = jnp.einsum('bnqgrd,bnkgd->bngrqk', qb, kb).astype(f32) * scale
    s_band = jnp.where(valid[None, :, None, None], s_band, -jnp.inf)
    s_ctx = jnp.einsum('bnqgrd,bkgd->bngrqk', qb, kc).astype(f32) * scale
    s_sink = jnp.broadcast_to(sink_g[None, None, :, :, None, None], s_ctx.shape[:-1] + (1,))
    prob = jax.nn.softmax(jnp.concatenate([s_sink, s_ctx, s_band], axis=-1), axis=-1).astype(vl.dtype)
    o = (jnp.einsum('bngrqk,bkgd->bnqgrd', prob[..., 1:1 + lc], vc)
         + jnp.einsum('bngrqk,bnkgd->bnqgrd', prob[..., 1 + lc:], vb))
    out_l = o.reshape(bsz, n, ATT_WIDTH)

    out_c = None
    if need_ctx_out:
        qc = rms_norm(pc['att_q'].reshape(bsz, lc, ATT_Q_HEADS, dh), qn_g).reshape(bsz, lc, ATT_KV_HEADS, rep, dh)
        s_c = jnp.einsum('bqgrd,bkgd->bgrqk', qc, kc).astype(f32) * scale
        s_cs = jnp.broadcast_to(sink_g[None, :, :, None, None], s_c.shape[:-1] + (1,))
        prob_c = jax.nn.softmax(jnp.concatenate([s_cs, s_c], axis=-1), axis=-1).astype(vc.dtype)
        out_c = jnp.einsum('bgrqk,bkgd->bqgrd', prob_c[..., 1:], vc).reshape(bsz, lc, ATT_WIDTH)
    return out_l, out_c


def token_mixers(ul, uc, ml_b_i, ml_b_f, ml_norm_g, ssd_conv_w, ssd_conv_b, ssd_a_log, ssd_dt_bias,
                 ssd_d, ssd_norm_g, att_qn_g, att_kn_g, att_sink, rows, cols, need_ctx_out):
    pl, pc = split_cols(ul), split_cols(uc)
    m_l, m_c = mlstm_mixer(pl, pc, ml_b_i, ml_b_f, ml_norm_g, need_ctx_out)
    s_l, s_c = ssd_mixer(pl, pc, ssd_conv_w, ssd_conv_b, ssd_a_log, ssd_dt_bias, ssd_d, ssd_norm_g, need_ctx_out)
    a_l, a_c = window_attention(pl, pc, att_qn_g, att_kn_g, att_sink, rows, cols, need_ctx_out)
    y_l = jnp.concatenate([m_l, s_l, a_l], axis=-1)
    y_c = jnp.concatenate([m_c, s_c, a_c], axis=-1) if need_ctx_out else None
    return y_l, y_c


def sq_relu_mlp(h, w_up, w_down):
    return jnp.square(jax.nn.relu(h @ w_up)) @ w_down


def setup_inputs(seed: int = 0) -> dict:
    key = jax.random.key(seed)
    ks = jax.random.split(key, 24)
    f32 = jnp.float32

    def nrm(k, shape, scale):
        return jax.random.normal(k, shape, f32) * scale

    def gain(k, shape):
        return 1.0 + 0.02 * jax.random.normal(k, shape, f32)

    dt0 = jnp.exp(jax.random.uniform(ks[14], (DEPTH, N_DIR, SSD_HEADS), f32, math.log(1e-3), math.log(1e-1)))
    return {
        'x': nrm(ks[0], (BATCH, SEQ, D_MODEL), 1.0),
        'c': nrm(ks[1], (BATCH, D_MODEL), 1.0),
        'ctx': nrm(ks[2], (BATCH, CTX_LEN, D_MODEL), 1.0),
        'c_ctx': nrm(ks[3], (D_MODEL,), 1.0),
        'w_mod': nrm(ks[4], (DEPTH, D_MODEL, 6 * D_MODEL), D_MODEL ** -0.5),
        'b_mod': nrm(ks[5], (DEPTH, 6 * D_MODEL), 0.02),
        'norm1_g': gain(ks[6], (DEPTH, D_MODEL)),
        'w_in': nrm(ks[7], (DEPTH, D_MODEL, IN_WIDTH), D_MODEL ** -0.5),
        'ml_b_i': nrm(ks[8], (DEPTH, N_DIR, ML_HEADS), 0.1),
        'ml_b_f': jnp.linspace(3.0, 6.0, ML_HEADS, dtype=f32) + nrm(ks[9], (DEPTH, N_DIR, ML_HEADS), 0.1),
        'ml_norm_g': gain(ks[10], (DEPTH, ML_WIDTH)),
        'ssd_conv_w': nrm(ks[11], (DEPTH, SSD_CONV, SSD_XBC), SSD_CONV ** -0.5),
        'ssd_conv_b': nrm(ks[12], (DEPTH, SSD_XBC), 0.02),
        'ssd_a_log': jnp.log(jax.random.uniform(ks[13], (DEPTH, N_DIR, SSD_HEADS), f32, 1.0, 16.0)),
        'ssd_dt_bias': dt0 + jnp.log(-jnp.expm1(-dt0)),
        'ssd_d': gain(ks[15], (DEPTH, SSD_HEADS)),
        'ssd_norm_g': gain(ks[16], (DEPTH, SSD_WIDTH)),
        'att_qn_g': gain(ks[17], (DEPTH, ATT_HEAD_DIM)),
        'att_kn_g': gain(ks[18], (DEPTH, ATT_HEAD_DIM)),
        'att_sink': nrm(ks[19], (DEPTH, ATT_Q_HEADS), 0.5),
        'w_out': nrm(ks[20], (DEPTH, MIX_WIDTH, D_MODEL), MIX_WIDTH ** -0.5),
        'norm2_g': gain(ks[21], (DEPTH, D_MODEL)),
        'w_up': nrm(ks[22], (DEPTH, D_MODEL, D_FF), D_MODEL ** -0.5),
        'w_down': nrm(ks[23], (DEPTH, D_FF, D_MODEL), D_FF ** -0.5),
    }


def reference(x, c, ctx, c_ctx, w_mod, b_mod, norm1_g, w_in, ml_b_i, ml_b_f, ml_norm_g,
              ssd_conv_w, ssd_conv_b, ssd_a_log, ssd_dt_bias, ssd_d, ssd_norm_g,
              att_qn_g, att_kn_g, att_sink, w_out, norm2_g, w_up, w_down):
    n_lat = x.shape[1]
    n_rows = n_lat // GRID_W
    rows = jnp.repeat(jnp.arange(n_rows, dtype=jnp.int32), GRID_W)
    cols = jnp.tile(jnp.arange(GRID_W, dtype=jnp.int32), n_rows)
    s_lat = jax.nn.silu(c)
    s_ctx = jax.nn.silu(c_ctx)
    h_ctx = ctx
    for layer in range(DEPTH):
        need_ctx_out = layer < DEPTH - 1
        mod_l = jnp.split((s_lat @ w_mod[layer] + b_mod[layer])[:, None, :], 6, axis=-1)
        mod_c = jnp.split(s_ctx @ w_mod[layer] + b_mod[layer], 6, axis=-1)
        ul = modulate(rms_norm(x, norm1_g[layer]), mod_l[0], mod_l[1]) @ w_in[layer]
        uc = modulate(rms_norm(h_ctx, norm1_g[layer]), mod_c[0], mod_c[1]) @ w_in[layer]
        y_l, y_c = token_mixers(ul, uc, ml_b_i[layer], ml_b_f[layer], ml_norm_g[layer],
                                ssd_conv_w[layer], ssd_conv_b[layer], ssd_a_log[layer], ssd_dt_bias[layer],
                                ssd_d[layer], ssd_norm_g[layer], att_qn_g[layer], att_kn_g[layer],
                                att_sink[layer], rows, cols, need_ctx_out)
        x = x + mod_l[2] * (y_l @ w_out[layer])
        x = x + mod_l[5] * sq_relu_mlp(modulate(rms_norm(x, norm2_g[layer]), mod_l[3], mod_l[4]),
                                       w_up[layer], w_down[layer])
        if need_ctx_out:
            h_ctx = h_ctx + mod_c[2] * (y_c @ w_out[layer])
            h_ctx = h_ctx + mod_c[5] * sq_relu_mlp(modulate(rms_norm(h_ctx, norm2_g[layer]), mod_c[3], mod_c[4]),
                                                   w_up[layer], w_down[layer])
    return x
```

```python
import math
from contextlib import ExitStack

import numpy as np
import ml_dtypes
import concourse.bass as bass
import concourse.mybir as mybir
from concourse.bass_utils import run_bass_kernel_spmd

F32 = mybir.dt.float32
BF16 = mybir.dt.bfloat16
ALU = mybir.AluOpType
AF = mybir.ActivationFunctionType
AX = mybir.AxisListType

D = 1024
SEQ = 2048
CTX = 256
NT = SEQ + CTX
NB = NT // 128
DEPTH = 2
EPS = 1e-6
IN_W = 4128
OFF_MLQ, OFF_MLK, OFF_MLV, OFF_MLO, OFF_MLI, OFF_MLF = 0, 512, 1024, 1536, 2048, 2056
OFF_Z, OFF_XBC, OFF_DT, OFF_AQ, OFF_AK, OFF_AV = 2064, 2576, 3344, 3360, 3872, 4000
TILES = [(0, 256, 1)] + [(256 + 512 * i, 512, 0) for i in range(4)]
NEG = -30000.0


class Buf:
    __slots__ = ("name", "writer", "readers", "dma_readers", "excl")

    def __init__(self, name=""):
        self.excl = False
        self.name = name
        self.writer = None
        self.readers = {}
        self.dma_readers = []


class Rec:
    __slots__ = ("eng", "fn", "deps", "inc", "is_dma", "sem", "count")

    def __init__(self, eng, fn, is_dma):
        self.eng = eng
        self.fn = fn
        self.deps = []
        self.inc = False
        self.is_dma = is_dma
        self.sem = None
        self.count = 0


class Sched:
    ENGS = ("pe", "dve", "act", "pool", "sp")
    NDMA = 24

    def __init__(self, nc):
        self.nc = nc
        self.streams = {e: [] for e in self.ENGS}
        self.dma_rr = 0
        self.dma_rr2 = [0, 0]
        self.dma_last = [None] * self.NDMA
        self.dma_cnt = [0] * self.NDMA
        self.last = {e: None for e in self.ENGS}

    def op(self, eng, fn, R=(), W=(), dma=False, extra=()):
        rec = Rec(eng, fn, dma)
        deps = {}

        def add(d):
            if d is None or d is rec:
                return
            if d.eng == "pe" and eng == "pe" and not d.is_dma and not dma:
                return
            deps[id(d)] = d

        xr = [b for b in R if b.excl and b not in W]
        if xr:
            W = list(W) + xr
        for b in R:
            add(b.writer)
        for b in W:
            add(b.writer)
            for r in b.readers.values():
                add(r)
            for r in b.dma_readers:
                add(r)
        for d in extra:
            add(d)
        if dma:
            half = self.NDMA // 2
            sw = 1 if eng == "pool" else 0
            k = sw * half + self.dma_rr2[sw]
            self.dma_rr2[sw] = (self.dma_rr2[sw] + 1) % half
            add(self.dma_last[k])
            self.dma_last[k] = rec
            self.dma_cnt[k] += 16
            rec.sem = k
            rec.count = self.dma_cnt[k]
        elif fn is not None:
            self.last[eng] = rec
        rec.deps = list(deps.values())
        for d in rec.deps:
            d.inc = True
        for b in R:
            if dma:
                b.dma_readers.append(rec)
            else:
                b.readers[eng] = rec
        for b in W:
            b.writer = rec
            b.readers = {}
            b.dma_readers = []
        self.streams[eng].append(rec)
        return rec

    def fence(self):
        pend = [r for r in self.last.values() if r is not None]
        pend += [r for r in self.dma_last if r is not None]
        for e in self.ENGS:
            self.op(e, None, extra=pend)

    def emit(self, final_recs):
        nc = self.nc
        for r in final_recs:
            r.inc = True
        for e in self.ENGS:
            c = 0
            for r in self.streams[e]:
                if r.is_dma or r.fn is None:
                    continue
                if r.inc:
                    c += 1
                    r.count = c
        with ExitStack() as es:
            esem = {e: es.enter_context(nc.semaphore(f"sem_{e}")) for e in self.ENGS}
            dsem = [es.enter_context(nc.semaphore(f"dsem{k}")) for k in range(self.NDMA)]
            block = es.enter_context(nc.Block())

            def replay(e, engine, extra_final=None):
                waited = {}

                def wait(d):
                    if d.is_dma:
                        key, sem = ("d", d.sem), dsem[d.sem]
                    else:
                        key, sem = ("e", d.eng), esem[d.eng]
                    if waited.get(key, 0) >= d.count:
                        return
                    waited[key] = d.count
                    engine.wait_ge(sem, d.count)

                for r in self.streams[e]:
                    for d in r.deps:
                        wait(d)
                    if r.fn is None:
                        continue
                    ins = r.fn(engine)
                    if r.is_dma:
                        ins.then_inc(dsem[r.sem], 16)
                    elif r.inc:
                        ins.then_inc(esem[e], 1)
                for d in extra_final or ():
                    wait(d)

            @block.tensor
            def _(eng):
                replay("pe", eng)

            @block.vector
            def _(eng):
                replay("dve", eng)

            @block.scalar
            def _(eng):
                replay("act", eng)

            @block.gpsimd
            def _(eng):
                replay("pool", eng)

            @block.sync
            def _(eng):
                replay("sp", eng, extra_final=final_recs)


class Tl:
    def __init__(self, t, nb, name):
        self.t = t
        self.b = [Buf(f"{name}.{i}") for i in range(nb)]

    def __getitem__(self, idx):
        return self.t[idx]


class Region:
    def alias(self, name, shape, dtype, off, bufs):
        self.n += 1
        t = self.nc.alloc_sbuf_tensor_at(f"{self.name}_{name}_{self.n}", list(shape), dtype, offset=self.base + off)
        tl = Tl(t, 0, name)
        tl.b = bufs
        return tl

    def __init__(self, nc, base, size, name):
        self.nc, self.base, self.size, self.cur, self.name, self.n = nc, base, size, 0, name, 0

    def reset(self):
        self.cur = 0

    def alloc(self, name, shape, dtype, nb=1):
        nbytes = int(np.prod(shape[1:])) * (4 if dtype == F32 else 2)
        nbytes = (nbytes + 31) // 32 * 32
        assert self.cur + nbytes <= self.size, f"region {self.name} overflow at {name}: {self.cur}+{nbytes}>{self.size}"
        self.n += 1
        t = self.nc.alloc_sbuf_tensor_at(f"{self.name}_{name}_{self.n}", list(shape), dtype, offset=self.base + self.cur)
        self.cur += nbytes
        return Tl(t, nb, name)


class _Stop(Exception):
    pass


def build(nc, depth=DEPTH, dbg=(), stop_after=None):
    S = Sched(nc)
    dbg_out = {}

    def din(name, shape, dt=F32):
        return nc.dram_tensor(name, list(shape), dt, kind="ExternalInput").ap()

    x_d = din("x", [SEQ, D])
    ctx_d = din("ctx", [CTX, D])
    cc_d = din("cc", [128, 16])
    wmod_d = din("w_mod", [depth, D, 6 * D])
    bmod_d = din("b_mod", [depth, 128, 48])
    g1_d = din("norm1_g", [depth, 128, 8])
    g2_d = din("norm2_g", [depth, 128, 8])
    win_d = din("w_in", [depth, D, IN_W])
    wout_d = din("w_out", [depth, 1536, D])
    wup_d = din("w_up", [depth, D, 4 * D])
    wdn_d = din("w_down", [depth, 4 * D, D])
    rowv_d = din("rowv", [depth, 1, 1280])
    convp_d = din("convp", [depth, 128, 24])
    cst_d = din("cst", [128, 128 * 6])
    m01_d = din("m01", [128, 1024])
    rope_d = din("rope", [128, 2 * 16 * 32])
    out_d = nc.dram_tensor("out", [SEQ, D], F32, kind="ExternalOutput").ap()
    xs_d = nc.dram_tensor("xs_scratch", [128, 8 * NT], F32, kind="Internal").ap()

    base0 = 16512
    slab = nc.alloc_sbuf_tensor("slab", [128, (229344 - base0) // 4 - 8], F32)
    sizes = dict(C=16384, X=73728, H=36864, Y=55296)
    off = base0
    RC = Region(nc, off, sizes["C"], "C"); off += sizes["C"]
    RX = Region(nc, off, sizes["X"], "X"); off += sizes["X"]
    RH = Region(nc, off, sizes["H"], "H"); off += sizes["H"]
    RY = Region(nc, off, sizes["Y"], "Y"); off += sizes["Y"]
    RW = Region(nc, off, 229344 - off - 64, "W")

    ps32 = [Tl(nc.alloc_psum_tensor(f"ps{i}", [128, 512], F32), 1, f"ps{i}") for i in range(6)]
    psb = [Tl(nc.alloc_psum_tensor(f"psb{i}", [128, 1024], BF16), 1, f"psb{i}") for i in range(2)]
    for _t in ps32 + psb:
        _t.b[0].excl = True
    pctr = [0, 0, 0]

    def P32():
        pctr[0] += 1
        return ps32[pctr[0] % 4]

    def PD():
        pctr[2] += 1
        return ps32[4 + pctr[2] % 2]

    def PB():
        pctr[1] += 1
        return psb[pctr[1] % 2]

    def MM(out, lhsT, rhs, start, stop, R, W):
        S.op("pe", lambda e: e.matmul(out, lhsT=lhsT, rhs=rhs, start=start, stop=stop), R, W)

    def TR(out, in_, ident, R, W):
        S.op("pe", lambda e: e.transpose(out=out, in_=in_, identity=ident), R, W)

    def ACT(out, in_, func, R, W, bias=None, scale=None):
        kw = {}
        if bias is not None:
            kw["bias"] = bias
        if scale is not None:
            kw["scale"] = scale
        S.op("act", lambda e: e.activation(out=out, in_=in_, func=func, **kw), R, W)

    import os as _os
    _nopool = _os.environ.get("KPOOL") != "1"

    def _pe(eng):
        return "dve" if (_nopool and eng == "pool") else eng

    def TT(eng, out, in0, in1, op, R, W):
        eng = _pe(eng)
        S.op(eng, lambda e: e.tensor_tensor(out=out, in0=in0, in1=in1, op=op), R, W)

    def TS(eng, out, in0, s1, op0, R, W, s2=None, op1=None):
        eng = _pe(eng)
        if op1 is None:
            S.op(eng, lambda e: e.tensor_scalar(out=out, in0=in0, scalar1=s1, scalar2=None, op0=op0), R, W)
        else:
            S.op(eng, lambda e: e.tensor_scalar(out=out, in0=in0, scalar1=s1, scalar2=s2, op0=op0, op1=op1), R, W)

    def STT(out, in0, scalar, in1, op0, op1, R, W):
        S.op("dve", lambda e: e.scalar_tensor_tensor(out=out, in0=in0, scalar=scalar, in1=in1, op0=op0, op1=op1), R, W)

    def CP(eng, out, in_, R, W):
        eng = _pe(eng)
        if eng == "act":
            S.op("act", lambda e: e.copy(out=out, in_=in_), R, W)
        else:
            S.op(eng, lambda e: e.tensor_copy(out=out, in_=in_), R, W)

    def RED(out, in_, op, R, W):
        S.op("dve", lambda e: e.tensor_reduce(out=out, in_=in_, axis=AX.X, op=op), R, W)

    def RECIP(out, in_, R, W):
        S.op("dve", lambda e: e.reciprocal(out=out, in_=in_), R, W)

    def MSET(eng, ap, val, W):
        eng = _pe(eng)
        S.op(eng, lambda e: e.memset(ap, val), (), W)

    def DMA(eng, out, in_, R, W, slow=False):
        if slow:
            return S.op(eng, lambda e: e.dma_start(out=out, in_=in_, allow_slow_non_contiguous=True), R, W, dma=True)
        return S.op(eng, lambda e: e.dma_start(out=out, in_=in_), R, W, dma=True)

    def dump(name, tl, shape, dt=F32):
        if name in dbg and name not in dbg_out:
            d = nc.dram_tensor("dbg_" + name, list(shape), dt, kind="ExternalOutput").ap()
            dbg_out[name] = DMA("sp", d, tl.t[:], tl.b, ())

    def rsqrt_chain(out, in_, add, R, W):
        ACT(out, in_, AF.Ln, R, W, bias=add, scale=1.0)
        ACT(out, out, AF.Exp, W, W, scale=-0.5)

    cst = RC.alloc("cst", [128, 768], F32)
    DMA("sp", cst[:], cst_d, (), cst.b)
    ident_f, triF, triR = cst[:, 0:128], cst[:, 128:256], cst[:, 256:384]
    mneg = [cst[:, 384:512], cst[:, 512:640]]
    ones_f = cst[:, 640:768]
    cstb = RC.alloc("cstb", [128, 256], BF16)
    DMA("pool", cstb[:, 0:128], cst_d[:, 0:128], (), cstb.b)
    DMA("pool", cstb[:, 128:256], cst_d[:, 640:768], (), cstb.b)
    ident_b, ones_b = cstb[:, 0:128], cstb[:, 128:256]
    m01 = RC.alloc("m01", [128, 1024], BF16)
    DMA("pool", m01[:], m01_d, (), m01.b)
    rope = RC.alloc("rope", [128, 1024], F32)
    DMA("sp", rope[:], rope_d, (), rope.b)
    rcos = rope[:, 0:512].rearrange("p (b h i) -> p b h i", b=16, h=2)
    rsin = rope[:, 512:1024].rearrange("p (b h i) -> p b h i", b=16, h=2)
    CB = cst.b + cstb.b

    modp = [RC.alloc(f"modp{l}", [128, 48, 2], F32) for l in range(depth)]
    rowv = RC.alloc("rowv", [128, 1280], F32)
    convp = RC.alloc("convp", [128, 24], F32)

    cc = RC.alloc("cc", [128, 16], F32)
    DMA("sp", cc[:], cc_d, (), cc.b)
    s2b = RC.alloc("s2b", [128, 8, 2], BF16)
    ACT(s2b[:].rearrange("p k j -> p (k j)"), cc[:], AF.Silu, cc.b, s2b.b)
    mnegb = RC.alloc("mnegb", [128, 256], BF16)
    DMA("pool", mnegb[:], cst_d[:, 384:640], (), mnegb.b)
    CB = CB + mnegb.b

    def mod_gen(l, wm, bm, g12):
        DMA("sp", bm[:], bmod_d[l], (), bm.b)
        DMA("sp", g12[:, 0:8], g1_d[l], (), g12.b)
        DMA("sp", g12[:, 8:16], g2_d[l], (), g12.b)
        mp = modp[l]
        def ld(fg):
            w_ = wm[fg % len(wm)]
            DMA("pool", w_[:], wmod_d[l].rearrange("(k p) f -> p k f", p=128)[:, :, fg * 512:(fg + 1) * 512], (), w_.b)

        ld(0)
        for fg in range(12):
            w = wm[fg % len(wm)]
            if fg + 1 < 12 and len(wm) > 1:
                ld(fg + 1)
            elif fg > 0 and len(wm) == 1:
                ld(fg)
            ps = P32()
            for j in range(4):
                for k in range(8):
                    MM(ps[:, 2 * j:2 * j + 2], w[:, k, j * 128:(j + 1) * 128], s2b[:, k, :], k == 0, k == 7,
                       w.b + s2b.b, ps.b)
            TT("dve", mp[:, fg * 4:(fg + 1) * 4, :], ps[:, 0:8].rearrange("p (a j) -> p a j", j=2),
               bm[:, fg * 4:(fg + 1) * 4].unsqueeze(2).to_broadcast([128, 4, 2]), ALU.add, ps.b + bm.b, mp.b)
            yield
        for (so, go) in ((8, 0), (32, 8)):
            TS("dve", mp[:, so:so + 8, :], mp[:, so:so + 8, :], 1.0, ALU.add, mp.b, mp.b, s2=32.0, op1=ALU.mult)
            TT("dve", mp[:, so:so + 8, :], mp[:, so:so + 8, :],
               g12[:, go:go + 8].unsqueeze(2).to_broadcast([128, 8, 2]), ALU.mult, mp.b + g12.b, mp.b)
        yield

    bm0 = RW.alloc("bm", [128, 48], F32)
    g120 = RW.alloc("g12", [128, 16], F32)
    wm0 = [RW.alloc(f"wm{i}", [128, 8, 512], BF16) for i in range(2)]
    for _ in mod_gen(0, wm0, bm0, g120):
        pass
    S.fence()
    RW.reset()

    xT = RX.alloc("xT", [128, 8, NT], F32, nb=1)
    xin = [RW.alloc(f"xin{i}", [128, D], F32) for i in range(3)]
    for tb in range(NB):
        xi = xin[tb % 3]
        src = ctx_d[tb * 128:(tb + 1) * 128, :] if tb < 2 else x_d[(tb - 2) * 128:(tb - 1) * 128, :]
        DMA("sp", xi[:], src, (), xi.b)
        for hf in range(2):
            ps = P32()
            for j in range(4):
                k = hf * 4 + j
                TR(ps[:, j * 128:(j + 1) * 128], xi[:, k * 128:(k + 1) * 128], ident_f, xi.b + CB, ps.b)
            CP("act" if hf else "dve", xT[:, hf * 4:(hf + 1) * 4, tb * 128:(tb + 1) * 128],
               ps[:, :].rearrange("p (a t) -> p a t", a=4), ps.b, xT.b)
    S.fence()
    RW.reset()

    hT = RH.alloc("hT", [128, 8, NT], BF16, nb=len(TILES))
    yT = RY.alloc("yT", [128, 12, NT], BF16, nb=1)

    def norm_to_h(l, a_off, b_off, tiles):
        mp = modp[l]
        sq = RW.alloc("nsq", [128, 8, 512], BF16)
        rs = RW.alloc("nrs", [128, 512], F32)
        tmp = [RW.alloc(f"ntmp{i}", [128, 512], F32) for i in range(2)]
        for ti, (s0, n, j) in enumerate(TILES):
            if ti not in tiles:
                continue
            ACT(sq[:, :, 0:n], xT[:, :, s0:s0 + n], AF.Square, xT.b, sq.b)
            ps = P32()
            for k in range(8):
                MM(ps[:, 0:n], ones_b, sq[:, k, 0:n], k == 0, k == 7, sq.b + CB, ps.b)
            rsqrt_chain(rs[:, 0:n], ps[:, 0:n], float(D * EPS), ps.b, rs.b)
            for k in range(8):
                t = tmp[k % 2]
                STT(t[:, 0:n], xT[:, k, s0:s0 + n], mp[:, a_off + k, j:j + 1], rs[:, 0:n], ALU.mult, ALU.mult,
                    xT.b + mp.b + rs.b, t.b)
                ACT(hT[:, k, s0:s0 + n], t[:, 0:n], AF.Identity, t.b + mp.b, [hT.b[ti]],
                    bias=mp[:, b_off + k, j:j + 1], scale=1.0)

    win_v = [win_d[l].rearrange("(k p) f -> p k f", p=128) for l in range(depth)]

    def load_w(tl, l, col0, ncols, dst0=0):
        DMA("pool", tl[:, :, dst0:dst0 + ncols], win_v[l][:, :, col0:col0 + ncols], (), tl.b)

    def proj_tok(w, c0, ncols, tb, ti, ps=None):
        ps = ps or P32()
        for k in range(8):
            MM(ps[:, 0:ncols], hT[:, k, tb * 128:(tb + 1) * 128], w[:, k, c0:c0 + ncols], k == 0, k == 7,
               w.b + [hT.b[ti]], ps.b)
        return ps

    def proj_feat(w, c0, ti, ps=None):
        s0, n, j = TILES[ti]
        ps = ps or P32()
        for k in range(8):
            MM(ps[:, 0:n], w[:, k, c0:c0 + 128], hT[:, k, s0:s0 + n], k == 0, k == 7, w.b + [hT.b[ti]], ps.b)
        return ps

    def tile_of(tb):
        return 0 if tb < 2 else 1 + (tb - 2) // 4

    def transpose_to_yT(src, chunk, blocks):
        blocks = list(blocks)
        for i in range(0, len(blocks), 4):
            grp = blocks[i:i + 4]
            pb = PB()
            for jj, tb in enumerate(grp):
                TR(pb[:, jj * 128:(jj + 1) * 128], src[:, tb, :], ident_b, src.b + CB, pb.b)
            if grp[-1] - grp[0] == len(grp) - 1:
                CP("act", yT[:, chunk, grp[0] * 128:(grp[-1] + 1) * 128], pb[:, 0:len(grp) * 128], pb.b, yT.b)
            else:
                for jj, tb in enumerate(grp):
                    CP("act", yT[:, chunk, tb * 128:(tb + 1) * 128], pb[:, jj * 128:(jj + 1) * 128], pb.b, yT.b)

    def layer(l):
        nonlocal xT, yT
        last = l == depth - 1
        need_ctx = not last
        out_blocks = list(range(NB)) if need_ctx else list(range(2, NB))
        tiles_all = list(range(len(TILES)))
        mp = modp[l]
        DMA("sp", rowv[:], rowv_d[l].partition_broadcast(128), (), rowv.b)
        DMA("sp", convp[:], convp_d[l], (), convp.b)
        rv = lambda a, n: rowv[:, a:a + n]
        RV_BI, RV_BF, RV_MLG, RV_ALOG, RV_DTB, RV_DSK, RV_SSDG, RV_QG, RV_KG, RV_SINK = 0, 8, 16, 528, 544, 560, 568, 1080, 1144, 1208

        norm_to_h(l, 8, 0, tiles_all)
        if stop_after == 'norm1' and l == 0:
            raise _Stop()
        DMA("sp", xs_d, xT[:].rearrange("p k t -> p (k t)"), xT.b, ())
        S.fence()
        RW.reset()
        RX.reset()

        wg = RW.alloc("wg", [128, 8, 32], BF16)
        load_w(wg, l, OFF_MLI, 16, 0)
        load_w(wg, l, OFF_DT, 16, 16)
        GW = 24
        wt = RX.alloc("wt", [128, NB, GW], F32)
        wsp = RX.alloc("wsp", [128, NB, GW], F32)
        cbias = RX.alloc("cbias", [128, NB, GW], F32)
        dec = RX.alloc("dec", [128, NB, GW], F32)
        GTt = {k: RW.alloc(f"GT{k}", [72, 128], F32) for k in range(6)}
        GTh = {k: RX.alloc(f"GTh{k}", [72, 128], BF16) for k in range(6)}
        GTl = {k: RX.alloc(f"GTl{k}", [72, 128], BF16) for k in range(6)}
        gate_mark = RX.cur
        graw = RW.alloc("graw", [128, NB, 32], F32)
        for tb in range(NB):
            ps = proj_tok(wg, 0, 32, tb, tile_of(tb))
            CP("dve", graw[:, tb, :], ps[:, 0:32], ps.b, graw.b)
        lnb = RW.alloc("lnb", [128, NB, GW], F32)
        ldec = RW.alloc("ldec", [128, NB, GW], F32)
        fcs = RW.alloc("fcs", [128, NB, GW], F32)
        tot = RW.alloc("tot", [128, NB, GW], F32)
        gtmp = RW.alloc("gtmp", [128, NB, 16], F32)
        gtmp2 = RW.alloc("gtmp2", [128, NB, 16], F32)

        def bc(a, n):
            return rv(a, n).unsqueeze(1).to_broadcast([128, NB, n])

        TT("dve", lnb[:, :, 0:8], graw[:, :, 0:8], bc(RV_BI, 8), ALU.add, graw.b + rowv.b, lnb.b)
        TT("dve", gtmp[:, :, 0:8], graw[:, :, 8:16], bc(RV_BF, 8), ALU.add, graw.b + rowv.b, gtmp.b)
        ACT(gtmp[:, :, 0:8], gtmp[:, :, 0:8], AF.Exp, gtmp.b, gtmp.b, scale=-1.0)
        ACT(gtmp[:, :, 0:8], gtmp[:, :, 0:8], AF.Ln, gtmp.b, gtmp.b, bias=1.0, scale=1.0)
        TS("dve", ldec[:, :, 0:8], gtmp[:, :, 0:8], -1.0, ALU.mult, gtmp.b, ldec.b)
        TT("dve", gtmp[:], graw[:, :, 16:32], bc(RV_DTB, 16), ALU.add, graw.b + rowv.b, gtmp.b)
        STT(gtmp2[:], gtmp[:], -1.0, gtmp[:], ALU.mult, ALU.max, gtmp.b, gtmp2.b)
        ACT(gtmp2[:], gtmp2[:], AF.Exp, gtmp2.b, gtmp2.b, scale=-1.0)
        ACT(gtmp2[:], gtmp2[:], AF.Ln, gtmp2.b, gtmp2.b, bias=1.0, scale=1.0)
        STT(gtmp[:], gtmp[:], 0.0, gtmp2[:], ALU.max, ALU.add, gtmp.b + gtmp2.b, gtmp.b)
        ACT(lnb[:, :, 8:24], gtmp[:], AF.Ln, gtmp.b, lnb.b)
        aexp = RW.alloc("aexp", [128, 16], F32)
        ACT(aexp[:], rv(RV_ALOG, 16), AF.Exp, rowv.b, aexp.b)
        TT("dve", gtmp2[:], gtmp[:], aexp[:].unsqueeze(1).to_broadcast([128, NB, 16]), ALU.mult, gtmp.b + aexp.b, gtmp2.b)
        TS("dve", ldec[:, :, 8:24], gtmp2[:], -1.0, ALU.mult, gtmp2.b, ldec.b)
        colsets = [(0, 4, 0), (4, 8, 1), (8, 16, 0), (16, 24, 1)]
        for (c0, c1, d) in colsets:
            ps = P32()
            nn = (c1 - c0) * NB
            MM(ps[:, 0:nn].rearrange("p (b c) -> p b c", b=NB), triR if d else triF, ldec[:, :, c0:c1], True, True, ldec.b + CB, ps.b)
            CP("dve", fcs[:, :, c0:c1], ps[:, 0:nn].rearrange("p (b c) -> p b c", b=NB), ps.b, fcs.b)
        ps = P32()
        MM(ps[:, 0:NB * GW], ones_f, ldec[:].rearrange("p b c -> p (b c)"), True, True, ldec.b + CB, ps.b)
        CP("dve", tot[:].rearrange("p b c -> p (b c)"), ps[:, 0:NB * GW], ps.b, tot.b)
        ACT(wt[:], fcs[:], AF.Exp, fcs.b, wt.b)
        ACT(dec[:], tot[:], AF.Exp, tot.b, dec.b)
        TT("dve", cbias[:], lnb[:], fcs[:], ALU.subtract, lnb.b + fcs.b, cbias.b)
        TT("dve", wsp[:], cbias[:], tot[:], ALU.add, cbias.b + tot.b, wsp.b)
        ACT(wsp[:], wsp[:], AF.Exp, wsp.b, wsp.b)
        fd = RW.alloc("fd", [128, NB, 4], F32)
        GT = {}
        gsets = [("m", 0, 0, 0), ("m", 1, 0, 4), ("s", 0, 0, 8), ("s", 0, 1, 12), ("s", 1, 0, 16), ("s", 1, 1, 20)]
        for gi, (fam, d, g, c0) in enumerate(gsets):
            CP("dve", fd[:], fcs[:, :, c0:c0 + 4], fcs.b, fd.b)
            ps = P32()
            TR(ps[0:72, 0:128], fd[:].rearrange("p b c -> p (b c)"), ident_f, fd.b + CB, ps.b)
            gt = GTt[gi]
            CP("dve", gt[:], ps[0:72, 0:128], ps.b, gt.b)
            CP("act", GTh[gi][:], gt[:], gt.b, GTh[gi].b)
            TT("dve", GTl[gi][:], gt[:], GTh[gi][:], ALU.subtract, gt.b + GTh[gi].b, GTl[gi].b)
            GT[(fam, d, g)] = (GTh[gi], GTl[gi])
        dump("cbias", cbias, [128, NB, GW]); dump("wt", wt, [128, NB, GW]); dump("wsp", wsp, [128, NB, GW])
        S.fence()
        RW.reset()

        def scan_order(d):
            return list(range(NB)) if d == 0 else [1, 0] + list(range(NB - 1, 1, -1))

        def dt_tile(gt, r, d, tb, col, dst, ps=None, c0=0):
            ps = ps or PD()
            gh, gl = gt
            sel = ident_b[0:72, r:r + 1].to_broadcast([72, 128])
            MM(ps[:, c0:c0 + 128], sel, gh[:, :], True, False, gh.b + CB, ps.b)
            MM(ps[:, c0:c0 + 128], sel, gl[:, :], False, False, gl.b + CB, ps.b)
            MM(ps[:, c0:c0 + 128], ident_b, mnegb[:, d * 128:(d + 1) * 128], False, True, CB, ps.b)
            ACT(dst[:, :], ps[:, c0:c0 + 128], AF.Exp, ps.b + cbias.b, dst.b, bias=cbias[:, tb, col:col + 1], scale=1.0)

        if stop_after == 'gates' and l == 0:
            raise _Stop()
        wq = [RW.alloc(f"wq{i}", [128, 8, 512], BF16) for i in range(2)]
        hbuf = []
        for si in range(2):
            if si == 0:
                al = lambda nm, shp, dt: RX.alloc(nm, shp, dt)
            else:
                yoff = [4 * NT * 2]

                def al(nm, shp, dt):
                    nbytes = (int(np.prod(shp[1:])) * (4 if dt == F32 else 2) + 31) // 32 * 32
                    t_ = RY.alias(nm, shp, dt, yoff[0], [Buf(nm)])
                    yoff[0] += nbytes
                    assert yoff[0] <= 12 * NT * 2
                    return t_
            hbuf.append([al(f"qT{si}", [128, NT], BF16), al(f"kT{si}", [128, NT], BF16),
                         al(f"ktok{si}", [128, NB, 128], BF16), al(f"vaug{si}", [128, NB, 130], BF16),
                         al(f"osig{si}", [128, NB, 128], F32)])
        hacc = RX.alloc("hacc", [128, NB, 128], F32, nb=NB)
        C32c = [RX.alloc(f"C32{i}", [128, 132], F32) for i in range(2)]
        Cbc = [[RX.alloc(f"Cb{i}{j}", [128, 132], BF16) for j in range(2)] for i in range(2)]
        DTs = [RX.alloc(f"DTs{i}", [128, 128], F32) for i in range(2)]
        PTs = [RX.alloc(f"PTs{i}", [128, 128], BF16) for i in range(2)]
        kws = [RX.alloc(f"kws{i}", [128, 128], BF16) for i in range(2)]
        tA = [RX.alloc(f"tA{i}", [128, 132], F32) for i in range(2)]
        tB = [RX.alloc(f"tB{i}", [128, 132], F32) for i in range(2)]
        dn = [RX.alloc(f"dn{i}", [128, 2], F32) for i in range(2)]
        mout = RX.alloc("mout", [128, NB, 128], BF16)
        ssq = RX.alloc("ssq", [128, NB], F32)
        sqt = RX.alloc("sqt", [128, NB, 128], F32)
        for si in range(2):
            MSET("dve", hbuf[si][3][:, :, 128:130], 1.0, hbuf[si][3].b)
        kscale = 128.0 ** -0.5
        orders = [scan_order(0), scan_order(1)]
        hset = set()

        def ml_proj(h):
            qT, kT, ktok, vaug, osig = hbuf[h % 2]
            w = wq[h % 2]
            for gq in range(4):
                load_w(w, l, gq * 512 + h * 128, 128, gq * 128)
            pc = 0
            for ti in tiles_all:
                s0, n, j = TILES[ti]
                ps = proj_feat(w, 0, ti, ps=ps32[4 + pc % 2]); pc += 1
                CP("act", qT[:, s0:s0 + n], ps[:, 0:n], ps.b, qT.b)
                yield
                ps = proj_feat(w, 128, ti, ps=ps32[4 + pc % 2]); pc += 1
                ACT(kT[:, s0:s0 + n], ps[:, 0:n], AF.Copy, ps.b, kT.b, scale=kscale)
                yield
            for tb in range(NB):
                ps = proj_tok(w, 128, 384, tb, tile_of(tb), ps=ps32[4 + pc % 2]); pc += 1
                ACT(ktok[:, tb, :], ps[:, 0:128], AF.Copy, ps.b, ktok.b, scale=kscale)
                CP("dve", vaug[:, tb, 0:128], ps[:, 128:256], ps.b, vaug.b)
                ACT(osig[:, tb, :], ps[:, 256:384], AF.Sigmoid, ps.b, osig.b)
                yield

        def ml_chain(d, h, hb_):
            for it in range(NB):
                tb = orders[d][it]
                gt = GT[("m", d, 0)]
                col = d * 4 + h
                blk = slice(tb * 128, (tb + 1) * 128)
                want = tb in out_blocks
                bank1, bank2 = ps32[2 * d], ps32[2 * d + 1]
                qT, kT, ktok, vaug = hb_[0], hb_[1], hb_[2], hb_[3]
                S_, B_ = bank1[:, 0:128], bank1[:, 128:258]
                A_, C_ = bank2[:, 0:130], bank2[:, 130:260]
                cb_cur, cb_nxt = Cbc[d][it % 2], Cbc[d][(it + 1) % 2]
                if want:
                    MM(S_, kT[:, blk], qT[:, blk], True, True, kT.b + qT.b, bank1.b)
                    yield
                    MM(B_, qT[:, blk], cb_cur[:, 0:130], True, True, qT.b + cb_cur.b, bank1.b)
                    yield
                if it < NB - 1:
                    ACT(kws[d][:], ktok[:, tb, :], AF.Identity, ktok.b + wsp.b, kws[d].b, scale=wsp[:, tb, col:col + 1])
                    yield
                    MM(C_, kws[d][:], vaug[:, tb, 0:130], True, True, kws[d].b + vaug.b, bank2.b)
                    yield
                    STT(C32c[d][:, 0:130], C32c[d][:, 0:130], dec[:, tb, col:col + 1], C_, ALU.mult, ALU.add,
                        C32c[d].b + dec.b + bank2.b, C32c[d].b)
                    yield
                    CP("act", cb_nxt[:, 0:130], C32c[d][:, 0:130], C32c[d].b, cb_nxt.b)
                    yield
                if want:
                    dt_tile(gt, tb * 4 + h, d, tb, col, DTs[d], ps=bank1, c0=260)
                    yield
                    TT("dve", PTs[d][:], S_, DTs[d][:], ALU.mult, bank1.b + DTs[d].b, PTs[d].b)
                    yield
                    MM(A_, PTs[d][:], vaug[:, tb, 0:130], True, True, PTs[d].b + vaug.b, bank2.b)
                    yield
                    ACT(tB[d][:, 0:130], B_, AF.Identity, bank1.b + wt.b, tB[d].b, scale=wt[:, tb, col:col + 1])
                    yield
                    TT("dve", tA[d][:, 0:130], A_, tB[d][:, 0:130], ALU.add, bank2.b + tB[d].b, tA[d].b)
                    yield
                    TS("dve", dn[d][:, 0:1], tA[d][:, 128:129], 1.0, ALU.max, tA[d].b, dn[d].b)
                    yield
                    STT(dn[d][:, 0:1], tA[d][:, 128:129], -1.0, dn[d][:, 0:1], ALU.mult, ALU.max, tA[d].b + dn[d].b, dn[d].b)
                    yield
                    RECIP(dn[d][:, 1:2], dn[d][:, 0:1], dn[d].b, dn[d].b)
                    yield
                    hb = [hacc.b[tb]]
                    if tb not in hset:
                        hset.add(tb)
                        TS("dve", hacc[:, tb, :], tA[d][:, 0:128], dn[d][:, 1:2], ALU.mult, tA[d].b + dn[d].b, hb)
                        yield
                    else:
                        STT(hacc[:, tb, :], tA[d][:, 0:128], dn[d][:, 1:2], hacc[:, tb, :], ALU.mult, ALU.add,
                            tA[d].b + dn[d].b + hb, hb)
                        yield


        def run_rr(gens, reps=None):
            gens = list(gens)
            reps = dict(zip(map(id, gens), reps or [1] * len(gens)))
            while gens:
                for g_ in list(gens):
                    for _ in range(reps[id(g_)]):
                        try:
                            next(g_)
                        except StopIteration:
                            gens.remove(g_)
                            break

        run_rr([ml_proj(0)])
        for h in range(4):
            osig = hbuf[h % 2][4]
            for d in range(2):
                MSET("dve", C32c[d][:], 0.0, C32c[d].b)
                MSET("dve", Cbc[d][0][:], 0.0, Cbc[d][0].b)
            hset.clear()
            gl_ = [ml_chain(0, h, hbuf[h % 2]), ml_chain(1, h, hbuf[h % 2])]
            rp = [8, 8]
            if h + 1 < 4:
                gl_.append(ml_proj(h + 1))
                rp.append(1)
            run_rr(gl_, rp)
            b0 = out_blocks[0]
            nbk = len(out_blocks)
            hv = hacc[:, b0:NB, :]
            TT("dve", osig[:, b0:NB, :], osig[:, b0:NB, :],
               rv(RV_MLG + h * 128, 128).unsqueeze(1).to_broadcast([128, nbk, 128]), ALU.mult, osig.b + rowv.b, osig.b)
            TT("dve", sqt[:, b0:NB, :], hv, hv, ALU.mult, hacc.b, sqt.b)
            RED(ssq[:, b0:NB], sqt[:, b0:NB, :], ALU.add, sqt.b, ssq.b)
            ACT(ssq[:, b0:NB], ssq[:, b0:NB], AF.Ln, ssq.b, ssq.b, bias=EPS, scale=1.0 / 128)
            ACT(ssq[:, b0:NB], ssq[:, b0:NB], AF.Exp, ssq.b, ssq.b, scale=-0.5)
            TT("dve", hv, hv, ssq[:, b0:NB].unsqueeze(2).to_broadcast([128, nbk, 128]), ALU.mult, hacc.b + ssq.b, hacc.b)
            TT("dve", mout[:, b0:NB, :], hv, osig[:, b0:NB, :], ALU.mult, hacc.b + osig.b, mout.b)
            transpose_to_yT(mout, h, out_blocks)
        S.fence()
        RX.cur = gate_mark
        RW.reset()

        if stop_after == 'mlstm' and l == 0:
            raise _Stop()
        wz = [RW.alloc(f"wz{i}", [128, 8, 256], BF16) for i in range(2)]
        wx = [RW.alloc(f"wx{i}", [128, 8, 256], BF16) for i in range(2)]
        wbc = RW.alloc("wbc", [128, 8, 256], BF16)
        DTs = [RW.alloc(f"DTs{i}", [128, 128], F32) for i in range(8)]
        PTs = [RW.alloc(f"PTs{i}", [128, 128], BF16) for i in range(8)]
        BT = RX.alloc("BT", [128, NT], BF16)
        CT = RX.alloc("CT", [128, NT], BF16)
        Btok = RX.alloc("Btok", [128, NB, 128], BF16)
        raw0_off = RX.cur
        raw = [RX.alloc(f"raw{i}", [128, NT], F32) for i in range(2)]
        raw[1].b = raw[0].b
        zs = RX.alias("zs", [128, NB, 256], BF16, raw0_off, raw[0].b)
        gg = RX.alias("gg", [128, 512], F32, raw0_off + 9216, raw[0].b)
        sout = RX.alias("sout", [128, NB, 128], BF16, raw0_off + 9216 + 2048, raw[0].b)
        xcT = RX.alloc("xcT", [128, NT], BF16)
        yacc = RX.alloc("yacc", [128, NB, 256], F32, nb=NB)
        H32c = [RX.alloc(f"H32{i}", [128, 256], F32) for i in range(2)]
        Hbc = [[RX.alloc(f"Hb{i}{j}", [128, 256], BF16) for j in range(2)] for i in range(2)]
        BwT = [RX.alloc(f"BwT{i}", [128, 128], BF16) for i in range(8)]
        ytmp = [RX.alloc(f"ytmp{i}", [128, 256], F32) for i in range(2)]
        ssq2 = RX.alloc("ssq2", [128, NB, 2], F32)
        rstd = RX.alloc("rstd", [128, NB], F32)
        G = RY.alias("G", [128, NB, 512], BF16, 8 * NT * 2, yT.b)
        xtok = RY.alias("xtok", [128, NB, 256], BF16, 4 * NT * 2, yT.b)

        def conv_silu(ps_src_fn, chunk, dst):
            r = raw[chunk % 2]
            for ti in tiles_all:
                s0, n, j = TILES[ti]
                ps = ps_src_fn(ti)
                CP("dve", r[:, s0:s0 + n], ps[:, 0:n], ps.b, r.b)
            acc = raw[(chunk + 1) % 2]
            cw = lambda i: convp[:, chunk * 4 + i:chunk * 4 + i + 1]
            ACT(acc[:, :], r[:, :], AF.Identity, r.b + convp.b, acc.b, bias=cw(3), scale=cw(1))
            for (a, b) in ((0, CTX), (CTX, NT)):
                STT(acc[:, a + 1:b], r[:, a:b - 1], cw(0), acc[:, a + 1:b], ALU.mult, ALU.add, r.b + acc.b + convp.b, acc.b)
                STT(acc[:, a:b - 1], r[:, a + 1:b], cw(2), acc[:, a:b - 1], ALU.mult, ALU.add, r.b + acc.b + convp.b, acc.b)
            ACT(dst[:, :], acc[:, :], AF.Silu, acc.b, dst.b)

        load_w(wbc, l, OFF_XBC + 512, 256)
        conv_silu(lambda ti: proj_feat(wbc, 0, ti), 4, BT)
        conv_silu(lambda ti: proj_feat(wbc, 128, ti), 5, CT)
        for i in range(0, NB, 4):
            pb = PB()
            for jj in range(4):
                tb = i + jj
                if tb < NB:
                    TR(pb[:, jj * 128:(jj + 1) * 128], BT[:, tb * 128:(tb + 1) * 128], ident_b, BT.b + CB, pb.b)
            nn = min(4, NB - i)
            CP("act", Btok[:, i:i + nn, :], pb[:, 0:nn * 128].rearrange("p (a c) -> p a c", a=nn), pb.b, Btok.b)
        for i in range(8):
            MSET("pool", BwT[i][:], 0.0, BwT[i].b)
        for g in range(2):
            load_w(wz[g], l, OFF_Z + g * 256, 256)
            load_w(wx[g], l, OFF_XBC + g * 256, 256)
            for cj in range(2):
                conv_silu(lambda ti, cj=cj: proj_feat(wx[g], cj * 128, ti), g * 2 + cj, xcT)
                for i in range(0, NB, 4):
                    pb = PB()
                    nn = min(4, NB - i)
                    for jj in range(nn):
                        tb = i + jj
                        TR(pb[:, jj * 128:(jj + 1) * 128], xcT[:, tb * 128:(tb + 1) * 128], ident_b, xcT.b + CB, pb.b)
                    CP("act", xtok[:, i:i + nn, cj * 128:(cj + 1) * 128],
                       pb[:, 0:nn * 128].rearrange("p (a c) -> p a c", a=nn), pb.b, xtok.b)
            for tb in range(NB):
                ps = proj_tok(wz[g], 0, 256, tb, tile_of(tb))
                ACT(zs[:, tb, :], ps[:, 0:256], AF.Silu, ps.b, zs.b)
            rows = slice(g * 64, (g + 1) * 64)
            orders = [scan_order(0), scan_order(1)]
            for d in range(2):
                MSET("dve", H32c[d][:], 0.0, H32c[d].b)
                MSET("pool", Hbc[d][0][:], 0.0, Hbc[d][0].b)
            yset = set()

            def ssd_chain(d):
                for it in range(NB):
                    tb = orders[d][it]
                    gt = GT[("s", d, g)]
                    c0 = 8 + d * 8 + g * 4
                    blk = slice(tb * 128, (tb + 1) * 128)
                    want = tb in out_blocks
                    lastit = it == NB - 1
                    bank1, bank2, bankD = ps32[2 * d], ps32[2 * d + 1], ps32[4 + d]
                    S_, B_ = bank1[:, 0:128], bank1[:, 128:384]
                    A_, C_ = bank2[:, 0:256], bank2[:, 256:512]
                    hb_cur, hb_nxt = Hbc[d][it % 2], Hbc[d][(it + 1) % 2]
                    H32 = H32c[d]
                    if want:
                        MM(S_, BT[rows, blk], CT[rows, blk], True, True, BT.b + CT.b, bank1.b)
                        yield
                        MM(B_, CT[rows, blk], hb_cur[rows, :], True, True, CT.b + hb_cur.b, bank1.b)
                        yield
                    if not lastit:
                        for hh in range(4):
                            col = c0 + hh
                            bw = BwT[d * 4 + hh]
                            ACT(bw[:, rows], Btok[:, tb, rows], AF.Identity, Btok.b + wsp.b, bw.b, scale=wsp[:, tb, col:col + 1])
                            yield
                            MM(bank2[:, 256 + hh * 64:256 + (hh + 1) * 64], bw[:], xtok[:, tb, hh * 64:(hh + 1) * 64], True, True,
                               bw.b + xtok.b, bank2.b)
                            yield
                        TT("dve", H32[rows, :].rearrange("p (a c) -> p a c", a=4), H32[rows, :].rearrange("p (a c) -> p a c", a=4),
                           dec[rows, tb, c0:c0 + 4].unsqueeze(2).to_broadcast([64, 4, 64]), ALU.mult, H32.b + dec.b, H32.b)
                        yield
                        TT("dve", H32[rows, :], H32[rows, :], bank2[rows, 256:512], ALU.add, H32.b + bank2.b, H32.b)
                        yield
                        CP("act", hb_nxt[rows, :], H32[rows, :], H32.b, hb_nxt.b)
                        yield
                    if want:
                        gh, gl = gt
                        for hh in range(4):
                            sel = ident_b[0:72, tb * 4 + hh:tb * 4 + hh + 1].to_broadcast([72, 128])
                            o_ = bankD[:, hh * 128:(hh + 1) * 128]
                            MM(o_, sel, gh[:, :], True, False, gh.b + CB, bankD.b)
                            MM(o_, sel, gl[:, :], False, False, gl.b + CB, bankD.b)
                            MM(o_, ident_b, mnegb[:, d * 128:(d + 1) * 128], False, True, CB, bankD.b)
                            yield
                        for hh in range(4):
                            col = c0 + hh
                            i2 = d * 4 + hh
                            ACT(DTs[i2][:, :], bankD[:, hh * 128:(hh + 1) * 128], AF.Exp, bankD.b + cbias.b, DTs[i2].b,
                                bias=cbias[:, tb, col:col + 1], scale=1.0)
                            yield
                        for hh in range(4):
                            i2 = d * 4 + hh
                            TT("dve", PTs[i2][:], S_, DTs[i2][:], ALU.mult, bank1.b + DTs[i2].b, PTs[i2].b)
                            yield
                        for hh in range(4):
                            i2 = d * 4 + hh
                            MM(bank2[:, hh * 64:(hh + 1) * 64], PTs[i2][:], xtok[:, tb, hh * 64:(hh + 1) * 64], True, True,
                               PTs[i2].b + xtok.b, bank2.b)
                            yield
                        yt = ytmp[d]
                        TT("dve", yt[:].rearrange("p (a c) -> p a c", a=4), B_.rearrange("p (a c) -> p a c", a=4),
                           wt[:, tb, c0:c0 + 4].unsqueeze(2).to_broadcast([128, 4, 64]), ALU.mult, bank1.b + wt.b, yt.b)
                        yield
                        yb = [yacc.b[tb]]
                        if tb not in yset:
                            yset.add(tb)
                            TT("dve", yacc[:, tb, :], A_, yt[:], ALU.add, bank2.b + yt.b, yb)
                            yield
                        else:
                            TT("dve", yt[:], A_, yt[:], ALU.add, bank2.b + yt.b, yt.b)
                            yield
                            TT("pool", yacc[:, tb, :], yacc[:, tb, :], yt[:], ALU.add, yb + yt.b, yb)
                            yield

            gens = [ssd_chain(0), ssd_chain(1)]
            while gens:
                for g_ in list(gens):
                    try:
                        next(g_)
                    except StopIteration:
                        gens.remove(g_)
            b0 = out_blocks[0]
            nbk = len(out_blocks)
            yv = yacc[:, b0:NB, :]
            for tb in out_blocks:
                yt = ytmp[tb % 2]
                TT("pool", yt[:].rearrange("p (a c) -> p a c", a=4), xtok[:, tb, :].rearrange("p (a c) -> p a c", a=4),
                   rv(RV_DSK + g * 4, 4).unsqueeze(2).to_broadcast([128, 4, 64]), ALU.mult, xtok.b + rowv.b, yt.b)
                TT("dve", yacc[:, tb, :], yacc[:, tb, :], yt[:], ALU.add, yacc.b + yt.b, yacc.b)
                TT("dve", yacc[:, tb, :], yacc[:, tb, :], zs[:, tb, :], ALU.mult, yacc.b + zs.b, yacc.b)
                CP("pool", G[:, tb, g * 256:(g + 1) * 256], yacc[:, tb, :], yacc.b, G.b)
                ACT(yt[:], yacc[:, tb, :], AF.Square, yacc.b, yt.b)
                RED(ssq2[:, tb, g:g + 1], yt[:], ALU.add, yt.b, ssq2.b)
        b0 = out_blocks[0]
        nbk = len(out_blocks)
        TT("dve", rstd[:, b0:NB], ssq2[:, b0:NB, 0], ssq2[:, b0:NB, 1], ALU.add, ssq2.b, rstd.b)
        ACT(rstd[:, b0:NB], rstd[:, b0:NB], AF.Ln, rstd.b, rstd.b, bias=EPS, scale=1.0 / 512)
        ACT(rstd[:, b0:NB], rstd[:, b0:NB], AF.Exp, rstd.b, rstd.b, scale=-0.5)
        for tb in out_blocks:
            TS("dve", gg[:], G[:, tb, :], rstd[:, tb:tb + 1], ALU.mult, G.b + rstd.b, gg.b)
            TT("pool", G[:, tb, :], gg[:], rv(RV_SSDG, 512), ALU.mult, gg.b + rowv.b, G.b)
        for cch in range(4):
            for tb in out_blocks:
                CP("pool", sout[:, tb, :], G[:, tb, cch * 128:(cch + 1) * 128], G.b, sout.b)
            transpose_to_yT(sout, 4 + cch, out_blocks)
        S.fence()
        RX.cur = gate_mark
        RW.reset()

        if stop_after == 'ssd' and l == 0:
            raise _Stop()
        wa = RW.alloc("wa", [128, 8, 768], BF16)
        load_w(wa, l, OFF_AQ, 768)
        qTa = RX.alloc("qTa", [128, 4, NT], BF16)
        kTa = RX.alloc("kTa", [128, 2, NT], BF16)
        va = RX.alloc("va", [128, NB, 2, 66], BF16)
        qraw = [RX.alloc(f"qraw{i}", [128, 640], F32) for i in range(2)]
        qsq = [RX.alloc(f"qsq{i}", [128, 640], F32) for i in range(2)]
        qn = [RX.alloc(f"qn{i}", [128, 640], F32) for i in range(2)]
        qr = [RX.alloc(f"qr{i}", [128, 640], BF16) for i in range(2)]
        kd = [RX.alloc(f"kd{i}", [128, 2, 128], BF16) for i in range(2)]
        rq = [RX.alloc(f"rq{i}", [128, 10], F32) for i in range(2)]
        t1 = [RX.alloc(f"t1{i}", [128, 10, 2, 16], F32) for i in range(2)]
        t2 = [RX.alloc(f"t2{i}", [128, 10, 2, 16], F32) for i in range(2)]
        PTa = [RW.alloc(f"PTa{i}", [128, 512], BF16) for i in range(10)]
        otok = [RX.alloc(f"otok{i}", [128, 512], BF16) for i in range(2)]
        dna = RX.alloc("dna", [128, 16], F32)
        esink = RX.alloc("esink", [128, 8], F32)
        gqk = RX.alloc("gqk", [128, 640], F32)
        MSET("pool", va[:, :, :, 64:66], 1.0, va.b)
        ACT(esink[:], rv(RV_SINK, 8), AF.Exp, rowv.b, esink.b)
        for hh in range(8):
            CP("dve", gqk[:, hh * 64:(hh + 1) * 64], rv(RV_QG, 64), rowv.b, gqk.b)
        for hh in range(2):
            TS("dve", gqk[:, 512 + hh * 64:512 + (hh + 1) * 64], rv(RV_KG, 64), 8.0, ALU.mult, rowv.b, gqk.b)
        def att_prep(tb):
            i2 = tb % 2
            ps = proj_tok(wa, 0, 512, tb, tile_of(tb), ps=ps32[2 * i2])
            yield
            CP("act", qraw[i2][:, 0:512], ps[:, 0:512], ps.b, qraw[i2].b)
            yield
            ps = proj_tok(wa, 512, 256, tb, tile_of(tb), ps=ps32[2 * i2 + 1])
            yield
            CP("act", qraw[i2][:, 512:640], ps[:, 0:128], ps.b, qraw[i2].b)
            yield
            CP("dve", va[:, tb, :, 0:64], ps[:, 128:256].rearrange("p (g c) -> p g c", g=2), ps.b, va.b)
            yield
            q3 = qraw[i2][:].rearrange("p (h c) -> p h c", h=10)
            TT("dve", qsq[i2][:], qraw[i2][:], qraw[i2][:], ALU.mult, qraw[i2].b, qsq[i2].b)
            yield
            RED(rq[i2][:], qsq[i2][:].rearrange("p (h c) -> p h c", h=10), ALU.add, qsq[i2].b, rq[i2].b)
            yield
            ACT(rq[i2][:], rq[i2][:], AF.Ln, rq[i2].b, rq[i2].b, bias=64 * EPS, scale=1.0)
            yield
            ACT(rq[i2][:], rq[i2][:], AF.Exp, rq[i2].b, rq[i2].b, scale=-0.5)
            yield
            qn3 = qn[i2][:].rearrange("p (h c) -> p h c", h=10)
            TT("dve", qn3, q3, rq[i2][:].unsqueeze(2).to_broadcast([128, 10, 64]), ALU.mult, qraw[i2].b + rq[i2].b, qn[i2].b)
            yield
            if tb >= 2:
                TT("pool", qn[i2][:], qn[i2][:], gqk[:], ALU.mult, qn[i2].b + gqk.b, qn[i2].b)
                yield
                q5 = qn[i2][:].rearrange("p (h a b i) -> p h a b i", h=10, a=2, b=2)
                o5 = qr[i2][:].rearrange("p (h a b i) -> p h a b i", h=10, a=2, b=2)
                cosb = rcos[:, tb - 2, :, :].unsqueeze(1).to_broadcast([128, 10, 2, 16])
                sinb = rsin[:, tb - 2, :, :].unsqueeze(1).to_broadcast([128, 10, 2, 16])
                x1, x2 = q5[:, :, :, 0, :], q5[:, :, :, 1, :]
                TT("dve", t1[i2][:], x1, cosb, ALU.mult, qn[i2].b + rope.b, t1[i2].b)
                yield
                TT("pool", t2[i2][:], x2, sinb, ALU.mult, qn[i2].b + rope.b, t2[i2].b)
                yield
                TT("dve", o5[:, :, :, 0, :], t1[i2][:], t2[i2][:], ALU.subtract, t1[i2].b + t2[i2].b, qr[i2].b)
                yield
                TT("dve", t1[i2][:], x2, cosb, ALU.mult, qn[i2].b + rope.b, t1[i2].b)
                yield
                TT("pool", t2[i2][:], x1, sinb, ALU.mult, qn[i2].b + rope.b, t2[i2].b)
                yield
                TT("dve", o5[:, :, :, 1, :], t1[i2][:], t2[i2][:], ALU.add, t1[i2].b + t2[i2].b, qr[i2].b)
                yield
            else:
                TT("pool", qr[i2][:], qn[i2][:], gqk[:], ALU.mult, qn[i2].b + gqk.b, qr[i2].b)
                yield
            for g in range(2):
                for hf in range(2):
                    CP("pool", kd[i2][:, g, hf * 64:(hf + 1) * 64], qr[i2][:, 512 + g * 64:512 + (g + 1) * 64], qr[i2].b, kd[i2].b)
                    yield
            pb = psb[i2]
            for a in range(4):
                TR(pb[:, a * 128:(a + 1) * 128], qr[i2][:, a * 128:(a + 1) * 128], ident_b, qr[i2].b + CB, pb.b)
                yield
            CP("act", qTa[:, :, tb * 128:(tb + 1) * 128], pb[:, 0:512].rearrange("p (a t) -> p a t", a=4), pb.b, qTa.b)
            yield
            pb = psb[i2]
            for g in range(2):
                TR(pb[:, g * 128:(g + 1) * 128], kd[i2][:, g, :], ident_b, kd[i2].b + CB, pb.b)
                yield
            CP("act", kTa[:, :, tb * 128:(tb + 1) * 128], pb[:, 0:256].rearrange("p (a t) -> p a t", a=2), pb.b, kTa.b)
            yield

        pend, active = list(range(NB)), []
        while pend or active:
            while len(active) < 2 and pend:
                active.append(att_prep(pend.pop(0)))
            for g_ in list(active):
                try:
                    next(g_)
                except StopIteration:
                    active.remove(g_)
        dump("qTa", qTa, [128, 4, NT], BF16); dump("kTa", kTa, [128, 2, NT], BF16)
        pi = 0
        for qi, tb in enumerate(out_blocks):
            keys = [(0, None), (1, None)]
            if tb >= 2:
                if tb - 1 >= 2:
                    keys.append((tb - 1, 0))
                keys.append((tb, None))
                if tb + 1 < NB:
                    keys.append((tb + 1, 1))
            psO = [ps32[0], ps32[1]]
            for ki, (kb, mk) in enumerate(keys):
                for g in range(2):
                    pt = PTa[ki * 2 + g]
                    for hf in range(2):
                        psS = ps32[2 + hf + 2 * (pi % 2)]
                        prow = slice(hf * 64, (hf + 1) * 64)
                        MM(psS[:, 0:256].rearrange("p (a t) -> p a t", a=2),
                           kTa[prow, g, kb * 128:(kb + 1) * 128], qTa[prow, 2 * g:2 * g + 2, tb * 128:(tb + 1) * 128],
                           True, True, kTa.b + qTa.b, psS.b)
                        ACT(pt[:, hf * 256:(hf + 1) * 256], psS[:, 0:256], AF.Exp, psS.b, pt.b)
                    pi += 1
                    if mk is not None:
                        TT("pool", pt[:], pt[:], m01[:, mk * 512:(mk + 1) * 512], ALU.mult, pt.b + m01.b, pt.b)
            for g in range(2):
                for hf in range(2):
                    for al in range(2):
                        cb_ = hf * 2 + al
                        hl = 2 * al + hf
                        for ki, (kb, mk) in enumerate(keys):
                            pt = PTa[ki * 2 + g]
                            MM(psO[g][:, hl * 66:(hl + 1) * 66], pt[:, cb_ * 128:(cb_ + 1) * 128], va[:, kb, g, 0:66],
                               ki == 0, ki == len(keys) - 1, pt.b + va.b, psO[g].b)
            ot = otok[qi % 2]
            for g in range(2):
                o4 = psO[g][:, 0:264].rearrange("p (h c) -> p h c", h=4)
                TT("dve", dna[:, g * 4:(g + 1) * 4], o4[:, :, 64], esink[:, g * 4:(g + 1) * 4], ALU.add, psO[g].b + esink.b, dna.b)
                RECIP(dna[:, 8 + g * 4:8 + (g + 1) * 4], dna[:, g * 4:(g + 1) * 4], dna.b, dna.b)
                TT("dve", ot[:, g * 256:(g + 1) * 256].rearrange("p (h c) -> p h c", h=4), o4[:, :, 0:64],
                   dna[:, 8 + g * 4:8 + (g + 1) * 4].unsqueeze(2).to_broadcast([128, 4, 64]), ALU.mult, psO[g].b + dna.b, ot.b)
            pb = PB()
            for cch in range(4):
                TR(pb[:, cch * 128:(cch + 1) * 128], ot[:, cch * 128:(cch + 1) * 128], ident_b, ot.b + CB, pb.b)
            CP("act", yT[:, 8:12, tb * 128:(tb + 1) * 128], pb[:, 0:512].rearrange("p (a t) -> p a t", a=4), pb.b, yT.b)
        dump("yT", yT, [128, 12, NT], BF16)
        S.fence()
        RX.reset()
        RW.reset()

        if stop_after == 'attn' and l == 0:
            raise _Stop()
        xT = RX.alloc("xT", [128, 8, NT], F32, nb=1)
        DMA("sp", xT[:].rearrange("p k t -> p (k t)"), xs_d, (), xT.b)
        tiles_out = tiles_all if need_ctx else tiles_all[1:]
        wo = RW.alloc("wo", [128, 12, 1024], BF16)
        for hf in range(2):
            DMA("pool", wo[:, :, hf * 512:(hf + 1) * 512],
                wout_d[l].rearrange("(m p) f -> p m f", p=128)[:, :, hf * 512:(hf + 1) * 512], (), wo.b)
        for ti in tiles_out:
            s0, n, j = TILES[ti]
            for c in range(8):
                ps = P32()
                for m in range(12):
                    MM(ps[:, 0:n], wo[:, m, c * 128:(c + 1) * 128], yT[:, m, s0:s0 + n], m == 0, m == 11, wo.b + yT.b, ps.b)
                STT(xT[:, c, s0:s0 + n], ps[:, 0:n], mp[:, 16 + c, j:j + 1], xT[:, c, s0:s0 + n], ALU.mult, ALU.add,
                    ps.b + mp.b + xT.b, xT.b)
        dump("x1", xT, [128, 8, NT])
        S.fence()
        RW.reset()
        RY.reset()
        if stop_after == 'outproj' and l == 0:
            raise _Stop()
        norm_to_h(l, 32, 24, tiles_out)
        wu = [RY.alloc(f"wu{i}", [128, 8, 512], BF16) for i in range(2)]
        wd = [RY.alloc(f"wd{i}", [128, 4, 1024], BF16) for i in range(2)]
        act = [RY.alloc(f"act{i}", [128, 4, 512], BF16) for i in range(2)]
        rl = [RY.alloc(f"rl{i}", [128, 512], F32) for i in range(2)]
        wup_v = wup_d[l].rearrange("(k p) f -> p k f", p=128)
        wdn_v = wdn_d[l].rearrange("(m p) f -> p m f", p=128)
        cnt = 0
        mg = None
        if l + 1 < depth:
            bm1 = RW.alloc("bm", [128, 48], F32)
            g121 = RW.alloc("g12", [128, 16], F32)
            wm1 = [RW.alloc("wm1", [128, 8, 512], BF16), RY.alloc("wm1b", [128, 8, 512], BF16)]
            mg = mod_gen(l + 1, wm1, bm1, g121)

        def load_mlp(sl):
            DMA("pool", wu[sl % 2][:], wup_v[:, :, sl * 512:(sl + 1) * 512], (), wu[sl % 2].b)
            DMA("pool", wd[sl % 2][:], wdn_v[:, sl * 4:(sl + 1) * 4, :], (), wd[sl % 2].b)

        load_mlp(0)
        for sl in range(8):
            u, dd = wu[sl % 2], wd[sl % 2]
            if sl + 1 < 8:
                load_mlp(sl + 1)
            for ti in tiles_out:
                s0, n, j = TILES[ti]
                a = act[cnt % 2]
                cnt += 1
                if mg is not None and cnt % 2 == 0:
                    if next(mg, "done") == "done":
                        mg = None
                for fc in range(4):
                    ps = P32()
                    for k in range(8):
                        MM(ps[:, 0:n], u[:, k, fc * 128:(fc + 1) * 128], hT[:, k, s0:s0 + n], k == 0, k == 7,
                           u.b + [hT.b[ti]], ps.b)
                    r_ = rl[fc % 2]
                    ACT(r_[:, 0:n], ps[:, 0:n], AF.Relu, ps.b, r_.b)
                    TT("pool", a[:, fc, 0:n], r_[:, 0:n], r_[:, 0:n], ALU.mult, r_.b, a.b)
                for c in range(8):
                    ps = P32()
                    for fc in range(4):
                        MM(ps[:, 0:n], dd[:, fc, c * 128:(c + 1) * 128], a[:, fc, 0:n], fc == 0, fc == 3, dd.b + a.b, ps.b)
                    STT(xT[:, c, s0:s0 + n], ps[:, 0:n], mp[:, 40 + c, j:j + 1], xT[:, c, s0:s0 + n], ALU.mult, ALU.add,
                        ps.b + mp.b + xT.b, xT.b)
        if mg is not None:
            for _ in mg:
                pass
        S.fence()
        RW.reset()
        RY.reset()
        yT = RY.alloc("yT", [128, 12, NT], BF16, nb=1)


    try:
        for l in range(depth):
            layer(l)
    except _Stop:
        S.fence()
        RW.reset()

    xo = [RW.alloc(f"xo{i}", [128, D], F32) for i in range(3)]
    finals = []
    for tb in range(2, NB):
        o = xo[tb % 3]
        for hf in range(2):
            ps = P32()
            for j in range(4):
                k = hf * 4 + j
                TR(ps[:, j * 128:(j + 1) * 128], xT[:, k, tb * 128:(tb + 1) * 128], ident_f, xT.b + CB, ps.b)
            CP("act" if hf else "dve", o[:, hf * 512:(hf + 1) * 512], ps[:, 0:512], ps.b, o.b)
        finals.append(DMA("sp", out_d[(tb - 2) * 128:(tb - 1) * 128, :], o[:], o.b, ()))
    finals += list(dbg_out.values())
    S.emit(finals)
    return S


def _host_constants():
    s = np.arange(128)[:, None]
    t = np.arange(128)[None, :]
    ident = np.eye(128, dtype=np.float32)
    triF = (s <= t).astype(np.float32)
    triR = (s >= t).astype(np.float32)
    mnegF = np.where(s <= t, 0.0, NEG).astype(np.float32)
    mnegR = np.where(s >= t, 0.0, NEG).astype(np.float32)
    ones = np.ones((128, 128), np.float32)
    cst = np.concatenate([ident, triF, triR, mnegF, mnegR, ones], axis=1)
    m01 = np.concatenate([np.tile(triR, (1, 4)), np.tile(triF, (1, 4))], axis=1).astype(np.float32)
    pos = np.arange(SEQ)
    rows, cols = pos // 64, pos % 64
    inv = 10000.0 ** (-np.arange(16, dtype=np.float32) / 16)
    ang = np.stack([rows[:, None] * inv[None, :], cols[:, None] * inv[None, :]], axis=1).astype(np.float32)
    ang = ang.reshape(16, 128, 2, 16).transpose(1, 0, 2, 3).reshape(128, 512)
    rope = np.concatenate([np.cos(ang), np.sin(ang)], axis=1).astype(np.float32)
    return cst, m01, rope


_CACHE = {}


def kernel(x, c, ctx, c_ctx, w_mod, b_mod, norm1_g, w_in, ml_b_i, ml_b_f, ml_norm_g,
           ssd_conv_w, ssd_conv_b, ssd_a_log, ssd_dt_bias, ssd_d, ssd_norm_g,
           att_qn_g, att_kn_g, att_sink, w_out, norm2_g, w_up, w_down, _dbg=(), _cores=8, _stop=None):
    f = lambda a: np.ascontiguousarray(np.asarray(a, dtype=np.float32))
    x, c, ctx, c_ctx = f(x), f(c), f(ctx), f(c_ctx)
    depth = w_mod.shape[0]
    cst, m01, rope = _host_constants()
    pk = lambda v: f(v).reshape(-1, 128).T
    bmod = np.stack([pk(b_mod[l]) for l in range(depth)])
    g1 = np.stack([pk(norm1_g[l]) for l in range(depth)])
    g2 = np.stack([pk(norm2_g[l]) for l in range(depth)])
    rowv = np.zeros((depth, 1, 1280), np.float32)
    convp = np.zeros((depth, 128, 24), np.float32)
    for l in range(depth):
        r = rowv[l, 0]
        r[0:8] = f(ml_b_i[l]).reshape(-1)
        r[8:16] = f(ml_b_f[l]).reshape(-1)
        r[16:528] = f(ml_norm_g[l])
        r[528:544] = f(ssd_a_log[l]).reshape(-1)
        r[544:560] = f(ssd_dt_bias[l]).reshape(-1)
        r[560:568] = f(ssd_d[l])
        r[568:1080] = f(ssd_norm_g[l])
        r[1080:1144] = f(att_qn_g[l])
        r[1144:1208] = f(att_kn_g[l])
        r[1208:1216] = f(att_sink[l])
        cw = f(ssd_conv_w[l])
        cb = f(ssd_conv_b[l])
        for ch in range(6):
            convp[l, :, ch * 4 + 0] = cw[0, ch * 128:(ch + 1) * 128]
            convp[l, :, ch * 4 + 1] = cw[1, ch * 128:(ch + 1) * 128]
            convp[l, :, ch * 4 + 2] = cw[2, ch * 128:(ch + 1) * 128]
            convp[l, :, ch * 4 + 3] = cb[ch * 128:(ch + 1) * 128]
    key = (depth, tuple(_dbg))
    nc = bass.Bass("TRN2", target_bir_lowering=False)
    build(nc, depth=depth, dbg=_dbg, stop_after=_stop)
    shared = {"w_mod": f(w_mod), "b_mod": bmod, "norm1_g": g1, "norm2_g": g2, "w_in": f(w_in), "w_out": f(w_out),
              "w_up": f(w_up), "w_down": f(w_down), "rowv": rowv, "convp": convp, "cst": cst, "m01": m01, "rope": rope}
    in_maps = []
    for b in range(_cores):
        cc = np.stack([pk(c[b]), pk(c_ctx)], axis=2).reshape(128, 16)
        m = dict(shared)
        m.update({"x": x[b], "ctx": ctx[b], "cc": np.ascontiguousarray(cc)})
        in_maps.append(m)
    res = run_bass_kernel_spmd(nc, in_maps, core_ids=list(range(_cores)))
    out = np.stack([res.results[b]["out"] for b in range(_cores)], axis=0).astype(np.float32)
    if _dbg:
        return out, res.results
    return out
```

```python
import math
from contextlib import ExitStack

import numpy as np
import ml_dtypes
import concourse.bass as bass
import concourse.mybir as mybir
from concourse.bass_utils import run_bass_kernel_spmd

F32 = mybir.dt.float32
BF16 = mybir.dt.bfloat16
ALU = mybir.AluOpType
AF = mybir.ActivationFunctionType
AX = mybir.AxisListType

D = 1024
SEQ = 2048
CTX = 256
NT = SEQ + CTX
NB = NT // 128
DEPTH = 2
EPS = 1e-6
IN_W = 4128
OFF_MLQ, OFF_MLK, OFF_MLV, OFF_MLO, OFF_MLI, OFF_MLF = 0, 512, 1024, 1536, 2048, 2056
OFF_Z, OFF_XBC, OFF_DT, OFF_AQ, OFF_AK, OFF_AV = 2064, 2576, 3344, 3360, 3872, 4000
TILES = [(0, 256, 1)] + [(256 + 512 * i, 512, 0) for i in range(4)]
NEG = -30000.0


class Buf:
    __slots__ = ("name", "writer", "readers", "dma_readers", "excl")

    def __init__(self, name=""):
        self.excl = False
        self.name = name
        self.writer = None
        self.readers = {}
        self.dma_readers = []


class Rec:
    __slots__ = ("eng", "fn", "deps", "inc", "is_dma", "sem", "count")

    def __init__(self, eng, fn, is_dma):
        self.eng = eng
        self.fn = fn
        self.deps = []
        self.inc = False
        self.is_dma = is_dma
        self.sem = None
        self.count = 0


class Sched:
    ENGS = ("pe", "dve", "act", "pool", "sp")
    NDMA = 24

    def __init__(self, nc):
        self.nc = nc
        self.streams = {e: [] for e in self.ENGS}
        self.dma_rr = 0
        self.dma_rr2 = [0, 0]
        self.dma_last = [None] * self.NDMA
        self.dma_cnt = [0] * self.NDMA
        self.last = {e: None for e in self.ENGS}

    def op(self, eng, fn, R=(), W=(), dma=False, extra=()):
        rec = Rec(eng, fn, dma)
        deps = {}

        def add(d):
            if d is None or d is rec:
                return
            if d.eng == "pe" and eng == "pe" and not d.is_dma and not dma:
                return
            deps[id(d)] = d

        xr = [b for b in R if b.excl and b not in W]
        if xr:
            W = list(W) + xr
        for b in R:
            add(b.writer)
        for b in W:
            add(b.writer)
            for r in b.readers.values():
                add(r)
            for r in b.dma_readers:
                add(r)
        for d in extra:
            add(d)
        if dma:
            half = self.NDMA // 2
            sw = 1 if eng == "pool" else 0
            k = sw * half + self.dma_rr2[sw]
            self.dma_rr2[sw] = (self.dma_rr2[sw] + 1) % half
            add(self.dma_last[k])
            self.dma_last[k] = rec
            self.dma_cnt[k] += 16
            rec.sem = k
            rec.count = self.dma_cnt[k]
        elif fn is not None:
            self.last[eng] = rec
        rec.deps = list(deps.values())
        for d in rec.deps:
            d.inc = True
        for b in R:
            if dma:
                b.dma_readers.append(rec)
            else:
                b.readers[eng] = rec
        for b in W:
            b.writer = rec
            b.readers = {}
            b.dma_readers = []
        self.streams[eng].append(rec)
        return rec

    def fence(self):
        pend = [r for r in self.last.values() if r is not None]
        pend += [r for r in self.dma_last if r is not None]
        for e in self.ENGS:
            self.op(e, None, extra=pend)

    def emit(self, final_recs):
        nc = self.nc
        for r in final_recs:
            r.inc = True
        for e in self.ENGS:
            c = 0
            for r in self.streams[e]:
                if r.is_dma or r.fn is None:
                    continue
                if r.inc:
                    c += 1
                    r.count = c
        with ExitStack() as es:
            esem = {e: es.enter_context(nc.semaphore(f"sem_{e}")) for e in self.ENGS}
            dsem = [es.enter_context(nc.semaphore(f"dsem{k}")) for k in range(self.NDMA)]
            block = es.enter_context(nc.Block())

            def replay(e, engine, extra_final=None):
                waited = {}

                def wait(d):
                    if d.is_dma:
                        key, sem = ("d", d.sem), dsem[d.sem]
                    else:
                        key, sem = ("e", d.eng), esem[d.eng]
                    if waited.get(key, 0) >= d.count:
                        return
                    waited[key] = d.count
                    engine.wait_ge(sem, d.count)

                for r in self.streams[e]:
                    for d in r.deps:
                        wait(d)
                    if r.fn is None:
                        continue
                    ins = r.fn(engine)
                    if r.is_dma:
                        ins.then_inc(dsem[r.sem], 16)
                    elif r.inc:
                        ins.then_inc(esem[e], 1)
                for d in extra_final or ():
                    wait(d)

            @block.tensor
            def _(eng):
                replay("pe", eng)

            @block.vector
            def _(eng):
                replay("dve", eng)

            @block.scalar
            def _(eng):
                replay("act", eng)

            @block.gpsimd
            def _(eng):
                replay("pool", eng)

            @block.sync
            def _(eng):
                replay("sp", eng, extra_final=final_recs)


class Tl:
    def __init__(self, t, nb, name):
        self.t = t
        self.b = [Buf(f"{name}.{i}") for i in range(nb)]

    def __getitem__(self, idx):
        return self.t[idx]


class Region:
    def alias(self, name, shape, dtype, off, bufs):
        self.n += 1
        t = self.nc.alloc_sbuf_tensor_at(f"{self.name}_{name}_{self.n}", list(shape), dtype, offset=self.base + off)
        tl = Tl(t, 0, name)
        tl.b = bufs
        return tl

    def __init__(self, nc, base, size, name):
        self.nc, self.base, self.size, self.cur, self.name, self.n = nc, base, size, 0, name, 0

    def reset(self):
        self.cur = 0

    def alloc(self, name, shape, dtype, nb=1):
        nbytes = int(np.prod(shape[1:])) * (4 if dtype == F32 else 2)
        nbytes = (nbytes + 31) // 32 * 32
        assert self.cur + nbytes <= self.size, f"region {self.name} overflow at {name}: {self.cur}+{nbytes}>{self.size}"
        self.n += 1
        t = self.nc.alloc_sbuf_tensor_at(f"{self.name}_{name}_{self.n}", list(shape), dtype, offset=self.base + self.cur)
        self.cur += nbytes
        return Tl(t, nb, name)


class _Stop(Exception):
    pass


def build(nc, depth=DEPTH, dbg=(), stop_after=None):
    S = Sched(nc)
    dbg_out = {}

    def din(name, shape, dt=F32):
        return nc.dram_tensor(name, list(shape), dt, kind="ExternalInput").ap()

    x_d = din("x", [SEQ, D])
    ctx_d = din("ctx", [CTX, D])
    cc_d = din("cc", [128, 16])
    wmod_d = din("w_mod", [depth, D, 6 * D])
    bmod_d = din("b_mod", [depth, 128, 48])
    g1_d = din("norm1_g", [depth, 128, 8])
    g2_d = din("norm2_g", [depth, 128, 8])
    win_d = din("w_in", [depth, D, IN_W])
    wout_d = din("w_out", [depth, 1536, D])
    wup_d = din("w_up", [depth, D, 4 * D])
    wdn_d = din("w_down", [depth, 4 * D, D])
    rowv_d = din("rowv", [depth, 1, 1280])
    convp_d = din("convp", [depth, 128, 24])
    cst_d = din("cst", [128, 128 * 6])
    m01_d = din("m01", [128, 1024])
    rope_d = din("rope", [128, 2 * 16 * 32])
    out_d = nc.dram_tensor("out", [SEQ, D], F32, kind="ExternalOutput").ap()
    xs_d = nc.dram_tensor("xs_scratch", [128, 8 * NT], F32, kind="Internal").ap()

    base0 = 16512
    slab = nc.alloc_sbuf_tensor("slab", [128, (229344 - base0) // 4 - 8], F32)
    sizes = dict(C=16384, X=73728, H=36864, Y=55296)
    off = base0
    RC = Region(nc, off, sizes["C"], "C"); off += sizes["C"]
    RX = Region(nc, off, sizes["X"], "X"); off += sizes["X"]
    RH = Region(nc, off, sizes["H"], "H"); off += sizes["H"]
    RY = Region(nc, off, sizes["Y"], "Y"); off += sizes["Y"]
    RW = Region(nc, off, 229344 - off - 64, "W")

    ps32 = [Tl(nc.alloc_psum_tensor(f"ps{i}", [128, 512], F32), 1, f"ps{i}") for i in range(6)]
    psb = [Tl(nc.alloc_psum_tensor(f"psb{i}", [128, 1024], BF16), 1, f"psb{i}") for i in range(2)]
    for _t in ps32 + psb:
        _t.b[0].excl = True
    pctr = [0, 0, 0]

    def P32():
        pctr[0] += 1
        return ps32[pctr[0] % 4]

    def PD():
        pctr[2] += 1
        return ps32[4 + pctr[2] % 2]

    def PB():
        pctr[1] += 1
        return psb[pctr[1] % 2]

    def MM(out, lhsT, rhs, start, stop, R, W):
        S.op("pe", lambda e: e.matmul(out, lhsT=lhsT, rhs=rhs, start=start, stop=stop), R, W)

    def TR(out, in_, ident, R, W):
        S.op("pe", lambda e: e.transpose(out=out, in_=in_, identity=ident), R, W)

    def ACT(out, in_, func, R, W, bias=None, scale=None):
        kw = {}
        if bias is not None:
            kw["bias"] = bias
        if scale is not None:
            kw["scale"] = scale
        S.op("act", lambda e: e.activation(out=out, in_=in_, func=func, **kw), R, W)

    import os as _os
    _nopool = _os.environ.get("KPOOL") != "1"

    def _pe(eng):
        return "dve" if (_nopool and eng == "pool") else eng

    def TT(eng, out, in0, in1, op, R, W):
        eng = _pe(eng)
        S.op(eng, lambda e: e.tensor_tensor(out=out, in0=in0, in1=in1, op=op), R, W)

    def TS(eng, out, in0, s1, op0, R, W, s2=None, op1=None):
        eng = _pe(eng)
        if op1 is None:
            S.op(eng, lambda e: e.tensor_scalar(out=out, in0=in0, scalar1=s1, scalar2=None, op0=op0), R, W)
        else:
            S.op(eng, lambda e: e.tensor_scalar(out=out, in0=in0, scalar1=s1, scalar2=s2, op0=op0, op1=op1), R, W)

    def STT(out, in0, scalar, in1, op0, op1, R, W):
        S.op("dve", lambda e: e.scalar_tensor_tensor(out=out, in0=in0, scalar=scalar, in1=in1, op0=op0, op1=op1), R, W)

    def CP(eng, out, in_, R, W):
        eng = _pe(eng)
        if eng == "act":
            S.op("act", lambda e: e.copy(out=out, in_=in_), R, W)
        else:
            S.op(eng, lambda e: e.tensor_copy(out=out, in_=in_), R, W)

    def RED(out, in_, op, R, W):
        S.op("dve", lambda e: e.tensor_reduce(out=out, in_=in_, axis=AX.X, op=op), R, W)

    def RECIP(out, in_, R, W):
        S.op("dve", lambda e: e.reciprocal(out=out, in_=in_), R, W)

    def MSET(eng, ap, val, W):
        eng = _pe(eng)
        S.op(eng, lambda e: e.memset(ap, val), (), W)

    def DMA(eng, out, in_, R, W, slow=False):
        if slow:
            return S.op(eng, lambda e: e.dma_start(out=out, in_=in_, allow_slow_non_contiguous=True), R, W, dma=True)
        return S.op(eng, lambda e: e.dma_start(out=out, in_=in_), R, W, dma=True)

    def dump(name, tl, shape, dt=F32):
        if name in dbg and name not in dbg_out:
            d = nc.dram_tensor("dbg_" + name, list(shape), dt, kind="ExternalOutput").ap()
            dbg_out[name] = DMA("sp", d, tl.t[:], tl.b, ())

    def rsqrt_chain(out, in_, add, R, W):
        ACT(out, in_, AF.Ln, R, W, bias=add, scale=1.0)
        ACT(out, out, AF.Exp, W, W, scale=-0.5)

    cst = RC.alloc("cst", [128, 768], F32)
    DMA("sp", cst[:], cst_d, (), cst.b)
    ident_f, triF, triR = cst[:, 0:128], cst[:, 128:256], cst[:, 256:384]
    mneg = [cst[:, 384:512], cst[:, 512:640]]
    ones_f = cst[:, 640:768]
    cstb = RC.alloc("cstb", [128, 256], BF16)
    DMA("pool", cstb[:, 0:128], cst_d[:, 0:128], (), cstb.b)
    DMA("pool", cstb[:, 128:256], cst_d[:, 640:768], (), cstb.b)
    ident_b, ones_b = cstb[:, 0:128], cstb[:, 128:256]
    m01 = RC.alloc("m01", [128, 1024], BF16)
    DMA("pool", m01[:], m01_d, (), m01.b)
    rope = RC.alloc("rope", [128, 1024], F32)
    DMA("sp", rope[:], rope_d, (), rope.b)
    rcos = rope[:, 0:512].rearrange("p (b h i) -> p b h i", b=16, h=2)
    rsin = rope[:, 512:1024].rearrange("p (b h i) -> p b h i", b=16, h=2)
    CB = cst.b + cstb.b

    modp = [RC.alloc(f"modp{l}", [128, 48, 2], F32) for l in range(depth)]
    rowv = RC.alloc("rowv", [128, 1280], F32)
    convp = RC.alloc("convp", [128, 24], F32)

    cc = RC.alloc("cc", [128, 16], F32)
    DMA("sp", cc[:], cc_d, (), cc.b)
    s2b = RC.alloc("s2b", [128, 8, 2], BF16)
    ACT(s2b[:].rearrange("p k j -> p (k j)"), cc[:], AF.Silu, cc.b, s2b.b)
    mnegb = RC.alloc("mnegb", [128, 256], BF16)
    DMA("pool", mnegb[:], cst_d[:, 384:640], (), mnegb.b)
    CB = CB + mnegb.b

    def mod_gen(l, wm, bm, g12):
        DMA("sp", bm[:], bmod_d[l], (), bm.b)
        DMA("sp", g12[:, 0:8], g1_d[l], (), g12.b)
        DMA("sp", g12[:, 8:16], g2_d[l], (), g12.b)
        mp = modp[l]
        def ld(fg):
            w_ = wm[fg % len(wm)]
            DMA("pool", w_[:], wmod_d[l].rearrange("(k p) f -> p k f", p=128)[:, :, fg * 512:(fg + 1) * 512], (), w_.b)

        ld(0)
        for fg in range(12):
            w = wm[fg % len(wm)]
            if fg + 1 < 12 and len(wm) > 1:
                ld(fg + 1)
            elif fg > 0 and len(wm) == 1:
                ld(fg)
            ps = P32()
            for j in range(4):
                for k in range(8):
                    MM(ps[:, 2 * j:2 * j + 2], w[:, k, j * 128:(j + 1) * 128], s2b[:, k, :], k == 0, k == 7,
                       w.b + s2b.b, ps.b)
            TT("dve", mp[:, fg * 4:(fg + 1) * 4, :], ps[:, 0:8].rearrange("p (a j) -> p a j", j=2),
               bm[:, fg * 4:(fg + 1) * 4].unsqueeze(2).to_broadcast([128, 4, 2]), ALU.add, ps.b + bm.b, mp.b)
            yield
        for (so, go) in ((8, 0), (32, 8)):
            TS("dve", mp[:, so:so + 8, :], mp[:, so:so + 8, :], 1.0, ALU.add, mp.b, mp.b, s2=32.0, op1=ALU.mult)
            TT("dve", mp[:, so:so + 8, :], mp[:, so:so + 8, :],
               g12[:, go:go + 8].unsqueeze(2).to_broadcast([128, 8, 2]), ALU.mult, mp.b + g12.b, mp.b)
        yield

    bm0 = RW.alloc("bm", [128, 48], F32)
    g120 = RW.alloc("g12", [128, 16], F32)
    wm0 = [RW.alloc(f"wm{i}", [128, 8, 512], BF16) for i in range(2)]
    for _ in mod_gen(0, wm0, bm0, g120):
        pass
    S.fence()
    RW.reset()

    xT = RX.alloc("xT", [128, 8, NT], F32, nb=1)
    xin = [RW.alloc(f"xin{i}", [128, D], F32) for i in range(3)]
    for tb in range(NB):
        xi = xin[tb % 3]
        src = ctx_d[tb * 128:(tb + 1) * 128, :] if tb < 2 else x_d[(tb - 2) * 128:(tb - 1) * 128, :]
        DMA("sp", xi[:], src, (), xi.b)
        for hf in range(2):
            ps = P32()
            for j in range(4):
                k = hf * 4 + j
                TR(ps[:, j * 128:(j + 1) * 128], xi[:, k * 128:(k + 1) * 128], ident_f, xi.b + CB, ps.b)
            CP("act" if hf else "dve", xT[:, hf * 4:(hf + 1) * 4, tb * 128:(tb + 1) * 128],
               ps[:, :].rearrange("p (a t) -> p a t", a=4), ps.b, xT.b)
    S.fence()
    RW.reset()

    hT = RH.alloc("hT", [128, 8, NT], BF16, nb=len(TILES))
    yT = RY.alloc("yT", [128, 12, NT], BF16, nb=1)

    def norm_to_h(l, a_off, b_off, tiles):
        mp = modp[l]
        sq = RW.alloc("nsq", [128, 8, 512], BF16)
        rs = RW.alloc("nrs", [128, 512], F32)
        tmp = [RW.alloc(f"ntmp{i}", [128, 512], F32) for i in range(2)]
        for ti, (s0, n, j) in enumerate(TILES):
            if ti not in tiles:
                continue
            ACT(sq[:, :, 0:n], xT[:, :, s0:s0 + n], AF.Square, xT.b, sq.b)
            ps = P32()
            for k in range(8):
                MM(ps[:, 0:n], ones_b, sq[:, k, 0:n], k == 0, k == 7, sq.b + CB, ps.b)
            rsqrt_chain(rs[:, 0:n], ps[:, 0:n], float(D * EPS), ps.b, rs.b)
            for k in range(8):
                t = tmp[k % 2]
                STT(t[:, 0:n], xT[:, k, s0:s0 + n], mp[:, a_off + k, j:j + 1], rs[:, 0:n], ALU.mult, ALU.mult,
                    xT.b + mp.b + rs.b, t.b)
                ACT(hT[:, k, s0:s0 + n], t[:, 0:n], AF.Identity, t.b + mp.b, [hT.b[ti]],
                    bias=mp[:, b_off + k, j:j + 1], scale=1.0)

    win_v = [win_d[l].rearrange("(k p) f -> p k f", p=128) for l in range(depth)]

    def load_w(tl, l, col0, ncols, dst0=0):
        DMA("pool", tl[:, :, dst0:dst0 + ncols], win_v[l][:, :, col0:col0 + ncols], (), tl.b)

    def proj_tok(w, c0, ncols, tb, ti, ps=None):
        ps = ps or P32()
        for k in range(8):
            MM(ps[:, 0:ncols], hT[:, k, tb * 128:(tb + 1) * 128], w[:, k, c0:c0 + ncols], k == 0, k == 7,
               w.b + [hT.b[ti]], ps.b)
        return ps

    def proj_feat(w, c0, ti, ps=None):
        s0, n, j = TILES[ti]
        ps = ps or P32()
        for k in range(8):
            MM(ps[:, 0:n], w[:, k, c0:c0 + 128], hT[:, k, s0:s0 + n], k == 0, k == 7, w.b + [hT.b[ti]], ps.b)
        return ps

    def tile_of(tb):
        return 0 if tb < 2 else 1 + (tb - 2) // 4

    def transpose_to_yT(src, chunk, blocks):
        blocks = list(blocks)
        for i in range(0, len(blocks), 4):
            grp = blocks[i:i + 4]
            pb = PB()
            for jj, tb in enumerate(grp):
                TR(pb[:, jj * 128:(jj + 1) * 128], src[:, tb, :], ident_b, src.b + CB, pb.b)
            if grp[-1] - grp[0] == len(grp) - 1:
                CP("act", yT[:, chunk, grp[0] * 128:(grp[-1] + 1) * 128], pb[:, 0:len(grp) * 128], pb.b, yT.b)
            else:
                for jj, tb in enumerate(grp):
                    CP("act", yT[:, chunk, tb * 128:(tb + 1) * 128], pb[:, jj * 128:(jj + 1) * 128], pb.b, yT.b)

    def layer(l):
        nonlocal xT, yT
        last = l == depth - 1
        need_ctx = not last
        out_blocks = list(range(NB)) if need_ctx else list(range(2, NB))
        tiles_all = list(range(len(TILES)))
        mp = modp[l]
        DMA("sp", rowv[:], rowv_d[l].partition_broadcast(128), (), rowv.b)
        DMA("sp", convp[:], convp_d[l], (), convp.b)
        rv = lambda a, n: rowv[:, a:a + n]
        RV_BI, RV_BF, RV_MLG, RV_ALOG, RV_DTB, RV_DSK, RV_SSDG, RV_QG, RV_KG, RV_SINK = 0, 8, 16, 528, 544, 560, 568, 1080, 1144, 1208

        norm_to_h(l, 8, 0, tiles_all)
        if stop_after == 'norm1' and l == 0:
            raise _Stop()
        DMA("sp", xs_d, xT[:].rearrange("p k t -> p (k t)"), xT.b, ())
        S.fence()
        RW.reset()
        RX.reset()

        wg = RW.alloc("wg", [128, 8, 32], BF16)
        load_w(wg, l, OFF_MLI, 16, 0)
        load_w(wg, l, OFF_DT, 16, 16)
        GW = 24
        wt = RX.alloc("wt", [128, NB, GW], F32)
        wsp = RX.alloc("wsp", [128, NB, GW], F32)
        cbias = RX.alloc("cbias", [128, NB, GW], F32)
        dec = RX.alloc("dec", [128, NB, GW], F32)
        GTt = {k: RW.alloc(f"GT{k}", [72, 128], F32) for k in range(6)}
        GTh = {k: RX.alloc(f"GTh{k}", [72, 128], BF16) for k in range(6)}
        GTl = {k: RX.alloc(f"GTl{k}", [72, 128], BF16) for k in range(6)}
        gate_mark = RX.cur
        graw = RW.alloc("graw", [128, NB, 32], F32)
        for tb in range(NB):
            ps = proj_tok(wg, 0, 32, tb, tile_of(tb))
            CP("dve", graw[:, tb, :], ps[:, 0:32], ps.b, graw.b)
        lnb = RW.alloc("lnb", [128, NB, GW], F32)
        ldec = RW.alloc("ldec", [128, NB, GW], F32)
        fcs = RW.alloc("fcs", [128, NB, GW], F32)
        tot = RW.alloc("tot", [128, NB, GW], F32)
        gtmp = RW.alloc("gtmp", [128, NB, 16], F32)
        gtmp2 = RW.alloc("gtmp2", [128, NB, 16], F32)

        def bc(a, n):
            return rv(a, n).unsqueeze(1).to_broadcast([128, NB, n])

        TT("dve", lnb[:, :, 0:8], graw[:, :, 0:8], bc(RV_BI, 8), ALU.add, graw.b + rowv.b, lnb.b)
        TT("dve", gtmp[:, :, 0:8], graw[:, :, 8:16], bc(RV_BF, 8), ALU.add, graw.b + rowv.b, gtmp.b)
        ACT(gtmp[:, :, 0:8], gtmp[:, :, 0:8], AF.Exp, gtmp.b, gtmp.b, scale=-1.0)
        ACT(gtmp[:, :, 0:8], gtmp[:, :, 0:8], AF.Ln, gtmp.b, gtmp.b, bias=1.0, scale=1.0)
        TS("dve", ldec[:, :, 0:8], gtmp[:, :, 0:8], -1.0, ALU.mult, gtmp.b, ldec.b)
        TT("dve", gtmp[:], graw[:, :, 16:32], bc(RV_DTB, 16), ALU.add, graw.b + rowv.b, gtmp.b)
        STT(gtmp2[:], gtmp[:], -1.0, gtmp[:], ALU.mult, ALU.max, gtmp.b, gtmp2.b)
        ACT(gtmp2[:], gtmp2[:], AF.Exp, gtmp2.b, gtmp2.b, scale=-1.0)
        ACT(gtmp2[:], gtmp2[:], AF.Ln, gtmp2.b, gtmp2.b, bias=1.0, scale=1.0)
        STT(gtmp[:], gtmp[:], 0.0, gtmp2[:], ALU.max, ALU.add, gtmp.b + gtmp2.b, gtmp.b)
        ACT(lnb[:, :, 8:24], gtmp[:], AF.Ln, gtmp.b, lnb.b)
        aexp = RW.alloc("aexp", [128, 16], F32)
        ACT(aexp[:], rv(RV_ALOG, 16), AF.Exp, rowv.b, aexp.b)
        TT("dve", gtmp2[:], gtmp[:], aexp[:].unsqueeze(1).to_broadcast([128, NB, 16]), ALU.mult, gtmp.b + aexp.b, gtmp2.b)
        TS("dve", ldec[:, :, 8:24], gtmp2[:], -1.0, ALU.mult, gtmp2.b, ldec.b)
        colsets = [(0, 4, 0), (4, 8, 1), (8, 16, 0), (16, 24, 1)]
        for (c0, c1, d) in colsets:
            ps = P32()
            nn = (c1 - c0) * NB
            MM(ps[:, 0:nn].rearrange("p (b c) -> p b c", b=NB), triR if d else triF, ldec[:, :, c0:c1], True, True, ldec.b + CB, ps.b)
            CP("dve", fcs[:, :, c0:c1], ps[:, 0:nn].rearrange("p (b c) -> p b c", b=NB), ps.b, fcs.b)
        ps = P32()
        MM(ps[:, 0:NB * GW], ones_f, ldec[:].rearrange("p b c -> p (b c)"), True, True, ldec.b + CB, ps.b)
        CP("dve", tot[:].rearrange("p b c -> p (b c)"), ps[:, 0:NB * GW], ps.b, tot.b)
        ACT(wt[:], fcs[:], AF.Exp, fcs.b, wt.b)
        ACT(dec[:], tot[:], AF.Exp, tot.b, dec.b)
        TT("dve", cbias[:], lnb[:], fcs[:], ALU.subtract, lnb.b + fcs.b, cbias.b)
        TT("dve", wsp[:], cbias[:], tot[:], ALU.add, cbias.b + tot.b, wsp.b)
        ACT(wsp[:], wsp[:], AF.Exp, wsp.b, wsp.b)
        fd = RW.alloc("fd", [128, NB, 4], F32)
        GT = {}
        gsets = [("m", 0, 0, 0), ("m", 1, 0, 4), ("s", 0, 0, 8), ("s", 0, 1, 12), ("s", 1, 0, 16), ("s", 1, 1, 20)]
        for gi, (fam, d, g, c0) in enumerate(gsets):
            CP("dve", fd[:], fcs[:, :, c0:c0 + 4], fcs.b, fd.b)
            ps = P32()
            TR(ps[0:72, 0:128], fd[:].rearrange("p b c -> p (b c)"), ident_f, fd.b + CB, ps.b)
            gt = GTt[gi]
            CP("dve", gt[:], ps[0:72, 0:128], ps.b, gt.b)
            CP("act", GTh[gi][:], gt[:], gt.b, GTh[gi].b)
            TT("dve", GTl[gi][:], gt[:], GTh[gi][:], ALU.subtract, gt.b + GTh[gi].b, GTl[gi].b)
            GT[(fam, d, g)] = (GTh[gi], GTl[gi])
        dump("cbias", cbias, [128, NB, GW]); dump("wt", wt, [128, NB, GW]); dump("wsp", wsp, [128, NB, GW])
        S.fence()
        RW.reset()

        def scan_order(d):
            return list(range(NB)) if d == 0 else [1, 0] + list(range(NB - 1, 1, -1))

        def dt_tile(gt, r, d, tb, col, dst, ps=None, c0=0):
            ps = ps or PD()
            gh, gl = gt
            sel = ident_b[0:72, r:r + 1].to_broadcast([72, 128])
            MM(ps[:, c0:c0 + 128], sel, gh[:, :], True, False, gh.b + CB, ps.b)
            MM(ps[:, c0:c0 + 128], sel, gl[:, :], False, False, gl.b + CB, ps.b)
            MM(ps[:, c0:c0 + 128], ident_b, mnegb[:, d * 128:(d + 1) * 128], False, True, CB, ps.b)
            ACT(dst[:, :], ps[:, c0:c0 + 128], AF.Exp, ps.b + cbias.b, dst.b, bias=cbias[:, tb, col:col + 1], scale=1.0)

        if stop_after == 'gates' and l == 0:
            raise _Stop()
        wq = [RW.alloc(f"wq{i}", [128, 8, 512], BF16) for i in range(2)]
        hbuf = []
        for si in range(2):
            if si == 0:
                al = lambda nm, shp, dt: RX.alloc(nm, shp, dt)
            else:
                yoff = [4 * NT * 2]

                def al(nm, shp, dt):
                    nbytes = (int(np.prod(shp[1:])) * (4 if dt == F32 else 2) + 31) // 32 * 32
                    t_ = RY.alias(nm, shp, dt, yoff[0], [Buf(nm)])
                    yoff[0] += nbytes
                    assert yoff[0] <= 12 * NT * 2
                    return t_
            hbuf.append([al(f"qT{si}", [128, NT], BF16), al(f"kT{si}", [128, NT], BF16),
                         al(f"ktok{si}", [128, NB, 128], BF16), al(f"vaug{si}", [128, NB, 130], BF16),
                         al(f"osig{si}", [128, NB, 128], F32)])
        hacc = RX.alloc("hacc", [128, NB, 128], F32, nb=NB)
        C32c = [RX.alloc(f"C32{i}", [128, 132], F32) for i in range(2)]
        Cbc = [[RX.alloc(f"Cb{i}{j}", [128, 132], BF16) for j in range(2)] for i in range(2)]
        DTs = [RX.alloc(f"DTs{i}", [128, 128], F32) for i in range(2)]
        PTs = [RX.alloc(f"PTs{i}", [128, 128], BF16) for i in range(2)]
        kws = [RX.alloc(f"kws{i}", [128, 128], BF16) for i in range(2)]
        tA = [RX.alloc(f"tA{i}", [128, 132], F32) for i in range(2)]
        tB = [RX.alloc(f"tB{i}", [128, 132], F32) for i in range(2)]
        dn = [RX.alloc(f"dn{i}", [128, 2], F32) for i in range(2)]
        mout = RX.alloc("mout", [128, NB, 128], BF16)
        ssq = RX.alloc("ssq", [128, NB], F32)
        sqt = RX.alloc("sqt", [128, NB, 128], F32)
        for si in range(2):
            MSET("dve", hbuf[si][3][:, :, 128:130], 1.0, hbuf[si][3].b)
        kscale = 128.0 ** -0.5
        orders = [scan_order(0), scan_order(1)]
        hset = set()

        def ml_proj(h):
            qT, kT, ktok, vaug, osig = hbuf[h % 2]
            w = wq[h % 2]
            for gq in range(4):
                load_w(w, l, gq * 512 + h * 128, 128, gq * 128)
            pc = 0
            for ti in tiles_all:
                s0, n, j = TILES[ti]
                ps = proj_feat(w, 0, ti, ps=ps32[4 + pc % 2]); pc += 1
                CP("act", qT[:, s0:s0 + n], ps[:, 0:n], ps.b, qT.b)
                yield
                ps = proj_feat(w, 128, ti, ps=ps32[4 + pc % 2]); pc += 1
                ACT(kT[:, s0:s0 + n], ps[:, 0:n], AF.Copy, ps.b, kT.b, scale=kscale)
                yield
            for tb in range(NB):
                ps = proj_tok(w, 128, 384, tb, tile_of(tb), ps=ps32[4 + pc % 2]); pc += 1
                ACT(ktok[:, tb, :], ps[:, 0:128], AF.Copy, ps.b, ktok.b, scale=kscale)
                CP("dve", vaug[:, tb, 0:128], ps[:, 128:256], ps.b, vaug.b)
                ACT(osig[:, tb, :], ps[:, 256:384], AF.Sigmoid, ps.b, osig.b)
                yield

        def ml_chain(d, h, hb_):
            for it in range(NB):
                tb = orders[d][it]
                gt = GT[("m", d, 0)]
                col = d * 4 + h
                blk = slice(tb * 128, (tb + 1) * 128)
                want = tb in out_blocks
                bank1, bank2 = ps32[2 * d], ps32[2 * d + 1]
                qT, kT, ktok, vaug = hb_[0], hb_[1], hb_[2], hb_[3]
                S_, B_ = bank1[:, 0:128], bank1[:, 128:258]
                A_, C_ = bank2[:, 0:130], bank2[:, 130:260]
                cb_cur, cb_nxt = Cbc[d][it % 2], Cbc[d][(it + 1) % 2]
                if want:
                    MM(S_, kT[:, blk], qT[:, blk], True, True, kT.b + qT.b, bank1.b)
                    yield
                    MM(B_, qT[:, blk], cb_cur[:, 0:130], True, True, qT.b + cb_cur.b, bank1.b)
                    yield
                if it < NB - 1:
                    ACT(kws[d][:], ktok[:, tb, :], AF.Identity, ktok.b + wsp.b, kws[d].b, scale=wsp[:, tb, col:col + 1])
                    yield
                    MM(C_, kws[d][:], vaug[:, tb, 0:130], True, True, kws[d].b + vaug.b, bank2.b)
                    yield
                    STT(C32c[d][:, 0:130], C32c[d][:, 0:130], dec[:, tb, col:col + 1], C_, ALU.mult, ALU.add,
                        C32c[d].b + dec.b + bank2.b, C32c[d].b)
                    yield
                    CP("act", cb_nxt[:, 0:130], C32c[d][:, 0:130], C32c[d].b, cb_nxt.b)
                    yield
                if want:
                    dt_tile(gt, tb * 4 + h, d, tb, col, DTs[d], ps=bank1, c0=260)
                    yield
                    TT("dve", PTs[d][:], S_, DTs[d][:], ALU.mult, bank1.b + DTs[d].b, PTs[d].b)
                    yield
                    MM(A_, PTs[d][:], vaug[:, tb, 0:130], True, True, PTs[d].b + vaug.b, bank2.b)
                    yield
                    ACT(tB[d][:, 0:130], B_, AF.Identity, bank1.b + wt.b, tB[d].b, scale=wt[:, tb, col:col + 1])
                    yield
                    TT("dve", tA[d][:, 0:130], A_, tB[d][:, 0:130], ALU.add, bank2.b + tB[d].b, tA[d].b)
                    yield
                    TS("dve", dn[d][:, 0:1], tA[d][:, 128:129], 1.0, ALU.max, tA[d].b, dn[d].b)
                    yield
                    STT(dn[d][:, 0:1], tA[d][:, 128:129], -1.0, dn[d][:, 0:1], ALU.mult, ALU.max, tA[d].b + dn[d].b, dn[d].b)
                    yield
                    RECIP(dn[d][:, 1:2], dn[d][:, 0:1], dn[d].b, dn[d].b)
                    yield
                    hb = [hacc.b[tb]]
                    if tb not in hset:
                        hset.add(tb)
                        TS("dve", hacc[:, tb, :], tA[d][:, 0:128], dn[d][:, 1:2], ALU.mult, tA[d].b + dn[d].b, hb)
                        yield
                    else:
                        STT(hacc[:, tb, :], tA[d][:, 0:128], dn[d][:, 1:2], hacc[:, tb, :], ALU.mult, ALU.add,
                            tA[d].b + dn[d].b + hb, hb)
                        yield


        def run_rr(fast, slow=(), ratio=8):
            fast, slow = list(fast), list(slow)
            rnd = 0
            while fast or slow:
                for g_ in list(fast):
                    try:
                        next(g_)
                    except StopIteration:
                        fast.remove(g_)
                rnd += 1
                if slow and (rnd % ratio == 0 or not fast):
                    for g_ in list(slow):
                        try:
                            next(g_)
                        except StopIteration:
                            slow.remove(g_)

        run_rr([ml_proj(0)])
        for h in range(4):
            osig = hbuf[h % 2][4]
            for d in range(2):
                MSET("dve", C32c[d][:], 0.0, C32c[d].b)
                MSET("dve", Cbc[d][0][:], 0.0, Cbc[d][0].b)
            hset.clear()
            gl_ = [ml_chain(0, h, hbuf[h % 2]), ml_chain(1, h, hbuf[h % 2])]
            run_rr(gl_, [ml_proj(h + 1)] if h + 1 < 4 else [], ratio=8)
            b0 = out_blocks[0]
            nbk = len(out_blocks)
            hv = hacc[:, b0:NB, :]
            TT("dve", osig[:, b0:NB, :], osig[:, b0:NB, :],
               rv(RV_MLG + h * 128, 128).unsqueeze(1).to_broadcast([128, nbk, 128]), ALU.mult, osig.b + rowv.b, osig.b)
            TT("dve", sqt[:, b0:NB, :], hv, hv, ALU.mult, hacc.b, sqt.b)
            RED(ssq[:, b0:NB], sqt[:, b0:NB, :], ALU.add, sqt.b, ssq.b)
            ACT(ssq[:, b0:NB], ssq[:, b0:NB], AF.Ln, ssq.b, ssq.b, bias=EPS, scale=1.0 / 128)
            ACT(ssq[:, b0:NB], ssq[:, b0:NB], AF.Exp, ssq.b, ssq.b, scale=-0.5)
            TT("dve", hv, hv, ssq[:, b0:NB].unsqueeze(2).to_broadcast([128, nbk, 128]), ALU.mult, hacc.b + ssq.b, hacc.b)
            TT("dve", mout[:, b0:NB, :], hv, osig[:, b0:NB, :], ALU.mult, hacc.b + osig.b, mout.b)
            transpose_to_yT(mout, h, out_blocks)
        S.fence()
        RX.cur = gate_mark
        RW.reset()

        if stop_after == 'mlstm' and l == 0:
            raise _Stop()
        wz = [RW.alloc(f"wz{i}", [128, 8, 256], BF16) for i in range(2)]
        wx = [RW.alloc(f"wx{i}", [128, 8, 256], BF16) for i in range(2)]
        wbc = RW.alloc("wbc", [128, 8, 256], BF16)
        DTs = [RW.alloc(f"DTs{i}", [128, 128], F32) for i in range(8)]
        PTs = [RW.alloc(f"PTs{i}", [128, 128], BF16) for i in range(8)]
        BT = RX.alloc("BT", [128, NT], BF16)
        CT = RX.alloc("CT", [128, NT], BF16)
        Btok = RX.alloc("Btok", [128, NB, 128], BF16)
        raw0_off = RX.cur
        raw = [RX.alloc(f"raw{i}", [128, NT], F32) for i in range(2)]
        raw[1].b = raw[0].b
        zs = RX.alias("zs", [128, NB, 256], BF16, raw0_off, raw[0].b)
        gg = RX.alias("gg", [128, 512], F32, raw0_off + 9216, raw[0].b)
        sout = RX.alias("sout", [128, NB, 128], BF16, raw0_off + 9216 + 2048, raw[0].b)
        xcT = RX.alloc("xcT", [128, NT], BF16)
        yacc = RX.alloc("yacc", [128, NB, 256], F32, nb=NB)
        H32c = [RX.alloc(f"H32{i}", [128, 256], F32) for i in range(2)]
        Hbc = [[RX.alloc(f"Hb{i}{j}", [128, 256], BF16) for j in range(2)] for i in range(2)]
        BwT = [RX.alloc(f"BwT{i}", [128, 128], BF16) for i in range(8)]
        ytmp = [RX.alloc(f"ytmp{i}", [128, 256], F32) for i in range(2)]
        ssq2 = RX.alloc("ssq2", [128, NB, 2], F32)
        rstd = RX.alloc("rstd", [128, NB], F32)
        G = RY.alias("G", [128, NB, 512], BF16, 8 * NT * 2, yT.b)
        xtok = RY.alias("xtok", [128, NB, 256], BF16, 4 * NT * 2, yT.b)

        def conv_silu(ps_src_fn, chunk, dst):
            r = raw[chunk % 2]
            for ti in tiles_all:
                s0, n, j = TILES[ti]
                ps = ps_src_fn(ti)
                CP("dve", r[:, s0:s0 + n], ps[:, 0:n], ps.b, r.b)
            acc = raw[(chunk + 1) % 2]
            cw = lambda i: convp[:, chunk * 4 + i:chunk * 4 + i + 1]
            ACT(acc[:, :], r[:, :], AF.Identity, r.b + convp.b, acc.b, bias=cw(3), scale=cw(1))
            for (a, b) in ((0, CTX), (CTX, NT)):
                STT(acc[:, a + 1:b], r[:, a:b - 1], cw(0), acc[:, a + 1:b], ALU.mult, ALU.add, r.b + acc.b + convp.b, acc.b)
                STT(acc[:, a:b - 1], r[:, a + 1:b], cw(2), acc[:, a:b - 1], ALU.mult, ALU.add, r.b + acc.b + convp.b, acc.b)
            ACT(dst[:, :], acc[:, :], AF.Silu, acc.b, dst.b)

        load_w(wbc, l, OFF_XBC + 512, 256)
        conv_silu(lambda ti: proj_feat(wbc, 0, ti), 4, BT)
        conv_silu(lambda ti: proj_feat(wbc, 128, ti), 5, CT)
        for i in range(0, NB, 4):
            pb = PB()
            for jj in range(4):
                tb = i + jj
                if tb < NB:
                    TR(pb[:, jj * 128:(jj + 1) * 128], BT[:, tb * 128:(tb + 1) * 128], ident_b, BT.b + CB, pb.b)
            nn = min(4, NB - i)
            CP("act", Btok[:, i:i + nn, :], pb[:, 0:nn * 128].rearrange("p (a c) -> p a c", a=nn), pb.b, Btok.b)
        for i in range(8):
            MSET("pool", BwT[i][:], 0.0, BwT[i].b)
        for g in range(2):
            load_w(wz[g], l, OFF_Z + g * 256, 256)
            load_w(wx[g], l, OFF_XBC + g * 256, 256)
            for cj in range(2):
                conv_silu(lambda ti, cj=cj: proj_feat(wx[g], cj * 128, ti), g * 2 + cj, xcT)
                for i in range(0, NB, 4):
                    pb = PB()
                    nn = min(4, NB - i)
                    for jj in range(nn):
                        tb = i + jj
                        TR(pb[:, jj * 128:(jj + 1) * 128], xcT[:, tb * 128:(tb + 1) * 128], ident_b, xcT.b + CB, pb.b)
                    CP("act", xtok[:, i:i + nn, cj * 128:(cj + 1) * 128],
                       pb[:, 0:nn * 128].rearrange("p (a c) -> p a c", a=nn), pb.b, xtok.b)
            for tb in range(NB):
                ps = proj_tok(wz[g], 0, 256, tb, tile_of(tb))
                ACT(zs[:, tb, :], ps[:, 0:256], AF.Silu, ps.b, zs.b)
            rows = slice(g * 64, (g + 1) * 64)
            orders = [scan_order(0), scan_order(1)]
            for d in range(2):
                MSET("dve", H32c[d][:], 0.0, H32c[d].b)
                MSET("pool", Hbc[d][0][:], 0.0, Hbc[d][0].b)
            yset = set()

            def ssd_chain(d):
                for it in range(NB):
                    tb = orders[d][it]
                    gt = GT[("s", d, g)]
                    c0 = 8 + d * 8 + g * 4
                    blk = slice(tb * 128, (tb + 1) * 128)
                    want = tb in out_blocks
                    lastit = it == NB - 1
                    bank1, bank2, bankD = ps32[2 * d], ps32[2 * d + 1], ps32[4 + d]
                    S_, B_ = bank1[:, 0:128], bank1[:, 128:384]
                    A_, C_ = bank2[:, 0:256], bank2[:, 256:512]
                    hb_cur, hb_nxt = Hbc[d][it % 2], Hbc[d][(it + 1) % 2]
                    H32 = H32c[d]
                    if want:
                        MM(S_, BT[rows, blk], CT[rows, blk], True, True, BT.b + CT.b, bank1.b)
                        yield
                        MM(B_, CT[rows, blk], hb_cur[rows, :], True, True, CT.b + hb_cur.b, bank1.b)
                        yield
                    if not lastit:
                        for hh in range(4):
                            col = c0 + hh
                            bw = BwT[d * 4 + hh]
                            ACT(bw[:, rows], Btok[:, tb, rows], AF.Identity, Btok.b + wsp.b, bw.b, scale=wsp[:, tb, col:col + 1])
                            yield
                            MM(bank2[:, 256 + hh * 64:256 + (hh + 1) * 64], bw[:], xtok[:, tb, hh * 64:(hh + 1) * 64], True, True,
                               bw.b + xtok.b, bank2.b)
                            yield
                        TT("dve", H32[rows, :].rearrange("p (a c) -> p a c", a=4), H32[rows, :].rearrange("p (a c) -> p a c", a=4),
                           dec[rows, tb, c0:c0 + 4].unsqueeze(2).to_broadcast([64, 4, 64]), ALU.mult, H32.b + dec.b, H32.b)
                        yield
                        TT("dve", H32[rows, :], H32[rows, :], bank2[rows, 256:512], ALU.add, H32.b + bank2.b, H32.b)
                        yield
                        CP("act", hb_nxt[rows, :], H32[rows, :], H32.b, hb_nxt.b)
                        yield
                    if want:
                        gh, gl = gt
                        for hh in range(4):
                            sel = ident_b[0:72, tb * 4 + hh:tb * 4 + hh + 1].to_broadcast([72, 128])
                            o_ = bankD[:, hh * 128:(hh + 1) * 128]
                            MM(o_, sel, gh[:, :], True, False, gh.b + CB, bankD.b)
                            MM(o_, sel, gl[:, :], False, False, gl.b + CB, bankD.b)
                            MM(o_, ident_b, mnegb[:, d * 128:(d + 1) * 128], False, True, CB, bankD.b)
                            yield
                        for hh in range(4):
                            col = c0 + hh
                            i2 = d * 4 + hh
                            ACT(DTs[i2][:, :], bankD[:, hh * 128:(hh + 1) * 128], AF.Exp, bankD.b + cbias.b, DTs[i2].b,
                                bias=cbias[:, tb, col:col + 1], scale=1.0)
                            yield
                        for hh in range(4):
                            i2 = d * 4 + hh
                            TT("dve", PTs[i2][:], S_, DTs[i2][:], ALU.mult, bank1.b + DTs[i2].b, PTs[i2].b)
                            yield
                        for hh in range(4):
                            i2 = d * 4 + hh
                            MM(bank2[:, hh * 64:(hh + 1) * 64], PTs[i2][:], xtok[:, tb, hh * 64:(hh + 1) * 64], True, True,
                               PTs[i2].b + xtok.b, bank2.b)
                            yield
                        yt = ytmp[d]
                        TT("dve", yt[:].rearrange("p (a c) -> p a c", a=4), B_.rearrange("p (a c) -> p a c", a=4),
                           wt[:, tb, c0:c0 + 4].unsqueeze(2).to_broadcast([128, 4, 64]), ALU.mult, bank1.b + wt.b, yt.b)
                        yield
                        yb = [yacc.b[tb]]
                        if tb not in yset:
                            yset.add(tb)
                            TT("dve", yacc[:, tb, :], A_, yt[:], ALU.add, bank2.b + yt.b, yb)
                            yield
                        else:
                            TT("dve", yt[:], A_, yt[:], ALU.add, bank2.b + yt.b, yt.b)
                            yield
                            TT("pool", yacc[:, tb, :], yacc[:, tb, :], yt[:], ALU.add, yb + yt.b, yb)
                            yield

            gens = [ssd_chain(0), ssd_chain(1)]
            while gens:
                for g_ in list(gens):
                    try:
                        next(g_)
                    except StopIteration:
                        gens.remove(g_)
            b0 = out_blocks[0]
            nbk = len(out_blocks)
            yv = yacc[:, b0:NB, :]
            for tb in out_blocks:
                yt = ytmp[tb % 2]
                TT("pool", yt[:].rearrange("p (a c) -> p a c", a=4), xtok[:, tb, :].rearrange("p (a c) -> p a c", a=4),
                   rv(RV_DSK + g * 4, 4).unsqueeze(2).to_broadcast([128, 4, 64]), ALU.mult, xtok.b + rowv.b, yt.b)
                TT("dve", yacc[:, tb, :], yacc[:, tb, :], yt[:], ALU.add, yacc.b + yt.b, yacc.b)
                TT("dve", yacc[:, tb, :], yacc[:, tb, :], zs[:, tb, :], ALU.mult, yacc.b + zs.b, yacc.b)
                CP("pool", G[:, tb, g * 256:(g + 1) * 256], yacc[:, tb, :], yacc.b, G.b)
                ACT(yt[:], yacc[:, tb, :], AF.Square, yacc.b, yt.b)
                RED(ssq2[:, tb, g:g + 1], yt[:], ALU.add, yt.b, ssq2.b)
        b0 = out_blocks[0]
        nbk = len(out_blocks)
        TT("dve", rstd[:, b0:NB], ssq2[:, b0:NB, 0], ssq2[:, b0:NB, 1], ALU.add, ssq2.b, rstd.b)
        ACT(rstd[:, b0:NB], rstd[:, b0:NB], AF.Ln, rstd.b, rstd.b, bias=EPS, scale=1.0 / 512)
        ACT(rstd[:, b0:NB], rstd[:, b0:NB], AF.Exp, rstd.b, rstd.b, scale=-0.5)
        for tb in out_blocks:
            TS("dve", gg[:], G[:, tb, :], rstd[:, tb:tb + 1], ALU.mult, G.b + rstd.b, gg.b)
            TT("pool", G[:, tb, :], gg[:], rv(RV_SSDG, 512), ALU.mult, gg.b + rowv.b, G.b)
        for cch in range(4):
            for tb in out_blocks:
                CP("pool", sout[:, tb, :], G[:, tb, cch * 128:(cch + 1) * 128], G.b, sout.b)
            transpose_to_yT(sout, 4 + cch, out_blocks)
        S.fence()
        RX.cur = gate_mark
        RW.reset()

        if stop_after == 'ssd' and l == 0:
            raise _Stop()
        wa = RW.alloc("wa", [128, 8, 768], BF16)
        load_w(wa, l, OFF_AQ, 768)
        qTa = RX.alloc("qTa", [128, 4, NT], BF16)
        kTa = RX.alloc("kTa", [128, 2, NT], BF16)
        va = RX.alloc("va", [128, NB, 2, 66], BF16)
        qraw = [RX.alloc(f"qraw{i}", [128, 640], F32) for i in range(2)]
        qsq = [RX.alloc(f"qsq{i}", [128, 640], F32) for i in range(2)]
        qn = [RX.alloc(f"qn{i}", [128, 640], F32) for i in range(2)]
        qr = [RX.alloc(f"qr{i}", [128, 640], BF16) for i in range(2)]
        kd = [RX.alloc(f"kd{i}", [128, 2, 128], BF16) for i in range(2)]
        rq = [RX.alloc(f"rq{i}", [128, 10], F32) for i in range(2)]
        t1 = [RX.alloc(f"t1{i}", [128, 10, 2, 16], F32) for i in range(2)]
        t2 = [RX.alloc(f"t2{i}", [128, 10, 2, 16], F32) for i in range(2)]
        PTa = [RW.alloc(f"PTa{i}", [128, 512], BF16) for i in range(10)]
        otok = [RX.alloc(f"otok{i}", [128, 512], BF16) for i in range(2)]
        dna = RX.alloc("dna", [128, 16], F32)
        esink = RX.alloc("esink", [128, 8], F32)
        gqk = RX.alloc("gqk", [128, 640], F32)
        MSET("pool", va[:, :, :, 64:66], 1.0, va.b)
        ACT(esink[:], rv(RV_SINK, 8), AF.Exp, rowv.b, esink.b)
        for hh in range(8):
            CP("dve", gqk[:, hh * 64:(hh + 1) * 64], rv(RV_QG, 64), rowv.b, gqk.b)
        for hh in range(2):
            TS("dve", gqk[:, 512 + hh * 64:512 + (hh + 1) * 64], rv(RV_KG, 64), 8.0, ALU.mult, rowv.b, gqk.b)
        def att_prep(tb):
            i2 = tb % 2
            ps = proj_tok(wa, 0, 512, tb, tile_of(tb), ps=ps32[2 * i2])
            yield
            CP("act", qraw[i2][:, 0:512], ps[:, 0:512], ps.b, qraw[i2].b)
            yield
            ps = proj_tok(wa, 512, 256, tb, tile_of(tb), ps=ps32[2 * i2 + 1])
            yield
            CP("act", qraw[i2][:, 512:640], ps[:, 0:128], ps.b, qraw[i2].b)
            yield
            CP("dve", va[:, tb, :, 0:64], ps[:, 128:256].rearrange("p (g c) -> p g c", g=2), ps.b, va.b)
            yield
            q3 = qraw[i2][:].rearrange("p (h c) -> p h c", h=10)
            TT("dve", qsq[i2][:], qraw[i2][:], qraw[i2][:], ALU.mult, qraw[i2].b, qsq[i2].b)
            yield
            RED(rq[i2][:], qsq[i2][:].rearrange("p (h c) -> p h c", h=10), ALU.add, qsq[i2].b, rq[i2].b)
            yield
            ACT(rq[i2][:], rq[i2][:], AF.Ln, rq[i2].b, rq[i2].b, bias=64 * EPS, scale=1.0)
            yield
            ACT(rq[i2][:], rq[i2][:], AF.Exp, rq[i2].b, rq[i2].b, scale=-0.5)
            yield
            qn3 = qn[i2][:].rearrange("p (h c) -> p h c", h=10)
            TT("dve", qn3, q3, rq[i2][:].unsqueeze(2).to_broadcast([128, 10, 64]), ALU.mult, qraw[i2].b + rq[i2].b, qn[i2].b)
            yield
            if tb >= 2:
                TT("pool", qn[i2][:], qn[i2][:], gqk[:], ALU.mult, qn[i2].b + gqk.b, qn[i2].b)
                yield
                q5 = qn[i2][:].rearrange("p (h a b i) -> p h a b i", h=10, a=2, b=2)
                o5 = qr[i2][:].rearrange("p (h a b i) -> p h a b i", h=10, a=2, b=2)
                cosb = rcos[:, tb - 2, :, :].unsqueeze(1).to_broadcast([128, 10, 2, 16])
                sinb = rsin[:, tb - 2, :, :].unsqueeze(1).to_broadcast([128, 10, 2, 16])
                x1, x2 = q5[:, :, :, 0, :], q5[:, :, :, 1, :]
                TT("dve", t1[i2][:], x1, cosb, ALU.mult, qn[i2].b + rope.b, t1[i2].b)
                yield
                TT("pool", t2[i2][:], x2, sinb, ALU.mult, qn[i2].b + rope.b, t2[i2].b)
                yield
                TT("dve", o5[:, :, :, 0, :], t1[i2][:], t2[i2][:], ALU.subtract, t1[i2].b + t2[i2].b, qr[i2].b)
                yield
                TT("dve", t1[i2][:], x2, cosb, ALU.mult, qn[i2].b + rope.b, t1[i2].b)
                yield
                TT("pool", t2[i2][:], x1, sinb, ALU.mult, qn[i2].b + rope.b, t2[i2].b)
                yield
                TT("dve", o5[:, :, :, 1, :], t1[i2][:], t2[i2][:], ALU.add, t1[i2].b + t2[i2].b, qr[i2].b)
                yield
            else:
                TT("pool", qr[i2][:], qn[i2][:], gqk[:], ALU.mult, qn[i2].b + gqk.b, qr[i2].b)
                yield
            for g in range(2):
                for hf in range(2):
                    CP("pool", kd[i2][:, g, hf * 64:(hf + 1) * 64], qr[i2][:, 512 + g * 64:512 + (g + 1) * 64], qr[i2].b, kd[i2].b)
                    yield
            pb = psb[i2]
            for a in range(4):
                TR(pb[:, a * 128:(a + 1) * 128], qr[i2][:, a * 128:(a + 1) * 128], ident_b, qr[i2].b + CB, pb.b)
                yield
            CP("act", qTa[:, :, tb * 128:(tb + 1) * 128], pb[:, 0:512].rearrange("p (a t) -> p a t", a=4), pb.b, qTa.b)
            yield
            pb = psb[i2]
            for g in range(2):
                TR(pb[:, g * 128:(g + 1) * 128], kd[i2][:, g, :], ident_b, kd[i2].b + CB, pb.b)
                yield
            CP("act", kTa[:, :, tb * 128:(tb + 1) * 128], pb[:, 0:256].rearrange("p (a t) -> p a t", a=2), pb.b, kTa.b)
            yield

        pend, active = list(range(NB)), []
        while pend or active:
            while len(active) < 2 and pend:
                active.append(att_prep(pend.pop(0)))
            for g_ in list(active):
                try:
                    next(g_)
                except StopIteration:
                    active.remove(g_)
        dump("qTa", qTa, [128, 4, NT], BF16); dump("kTa", kTa, [128, 2, NT], BF16)
        pi = 0
        for qi, tb in enumerate(out_blocks):
            keys = [(0, None), (1, None)]
            if tb >= 2:
                if tb - 1 >= 2:
                    keys.append((tb - 1, 0))
                keys.append((tb, None))
                if tb + 1 < NB:
                    keys.append((tb + 1, 1))
            psO = [ps32[0], ps32[1]]
            for ki, (kb, mk) in enumerate(keys):
                for g in range(2):
                    pt = PTa[ki * 2 + g]
                    for hf in range(2):
                        psS = ps32[2 + hf + 2 * (pi % 2)]
                        prow = slice(hf * 64, (hf + 1) * 64)
                        MM(psS[:, 0:256].rearrange("p (a t) -> p a t", a=2),
                           kTa[prow, g, kb * 128:(kb + 1) * 128], qTa[prow, 2 * g:2 * g + 2, tb * 128:(tb + 1) * 128],
                           True, True, kTa.b + qTa.b, psS.b)
                        ACT(pt[:, hf * 256:(hf + 1) * 256], psS[:, 0:256], AF.Exp, psS.b, pt.b)
                    pi += 1
                    if mk is not None:
                        TT("pool", pt[:], pt[:], m01[:, mk * 512:(mk + 1) * 512], ALU.mult, pt.b + m01.b, pt.b)
            for g in range(2):
                for hf in range(2):
                    for al in range(2):
                        cb_ = hf * 2 + al
                        hl = 2 * al + hf
                        for ki, (kb, mk) in enumerate(keys):
                            pt = PTa[ki * 2 + g]
                            MM(psO[g][:, hl * 66:(hl + 1) * 66], pt[:, cb_ * 128:(cb_ + 1) * 128], va[:, kb, g, 0:66],
                               ki == 0, ki == len(keys) - 1, pt.b + va.b, psO[g].b)
            ot = otok[qi % 2]
            for g in range(2):
                o4 = psO[g][:, 0:264].rearrange("p (h c) -> p h c", h=4)
                TT("dve", dna[:, g * 4:(g + 1) * 4], o4[:, :, 64], esink[:, g * 4:(g + 1) * 4], ALU.add, psO[g].b + esink.b, dna.b)
                RECIP(dna[:, 8 + g * 4:8 + (g + 1) * 4], dna[:, g * 4:(g + 1) * 4], dna.b, dna.b)
                TT("dve", ot[:, g * 256:(g + 1) * 256].rearrange("p (h c) -> p h c", h=4), o4[:, :, 0:64],
                   dna[:, 8 + g * 4:8 + (g + 1) * 4].unsqueeze(2).to_broadcast([128, 4, 64]), ALU.mult, psO[g].b + dna.b, ot.b)
            pb = PB()
            for cch in range(4):
                TR(pb[:, cch * 128:(cch + 1) * 128], ot[:, cch * 128:(cch + 1) * 128], ident_b, ot.b + CB, pb.b)
            CP("act", yT[:, 8:12, tb * 128:(tb + 1) * 128], pb[:, 0:512].rearrange("p (a t) -> p a t", a=4), pb.b, yT.b)
        dump("yT", yT, [128, 12, NT], BF16)
        S.fence()
        RX.reset()
        RW.reset()

        if stop_after == 'attn' and l == 0:
            raise _Stop()
        xT = RX.alloc("xT", [128, 8, NT], F32, nb=1)
        DMA("sp", xT[:].rearrange("p k t -> p (k t)"), xs_d, (), xT.b)
        tiles_out = tiles_all if need_ctx else tiles_all[1:]
        wo = RW.alloc("wo", [128, 12, 1024], BF16)
        for hf in range(2):
            DMA("pool", wo[:, :, hf * 512:(hf + 1) * 512],
                wout_d[l].rearrange("(m p) f -> p m f", p=128)[:, :, hf * 512:(hf + 1) * 512], (), wo.b)
        for ti in tiles_out:
            s0, n, j = TILES[ti]
            for c in range(8):
                ps = P32()
                for m in range(12):
                    MM(ps[:, 0:n], wo[:, m, c * 128:(c + 1) * 128], yT[:, m, s0:s0 + n], m == 0, m == 11, wo.b + yT.b, ps.b)
                STT(xT[:, c, s0:s0 + n], ps[:, 0:n], mp[:, 16 + c, j:j + 1], xT[:, c, s0:s0 + n], ALU.mult, ALU.add,
                    ps.b + mp.b + xT.b, xT.b)
        dump("x1", xT, [128, 8, NT])
        S.fence()
        RW.reset()
        RY.reset()
        if stop_after == 'outproj' and l == 0:
            raise _Stop()
        norm_to_h(l, 32, 24, tiles_out)
        wu = [RY.alloc(f"wu{i}", [128, 8, 512], BF16) for i in range(2)]
        wd = [RY.alloc(f"wd{i}", [128, 4, 1024], BF16) for i in range(2)]
        act = [RY.alloc(f"act{i}", [128, 4, 512], BF16) for i in range(2)]
        rl = [RY.alloc(f"rl{i}", [128, 512], F32) for i in range(2)]
        wup_v = wup_d[l].rearrange("(k p) f -> p k f", p=128)
        wdn_v = wdn_d[l].rearrange("(m p) f -> p m f", p=128)
        cnt = 0
        mg = None
        if l + 1 < depth:
            bm1 = RW.alloc("bm", [128, 48], F32)
            g121 = RW.alloc("g12", [128, 16], F32)
            wm1 = [RW.alloc("wm1", [128, 8, 512], BF16), RY.alloc("wm1b", [128, 8, 512], BF16)]
            mg = mod_gen(l + 1, wm1, bm1, g121)

        def load_mlp(sl):
            DMA("pool", wu[sl % 2][:], wup_v[:, :, sl * 512:(sl + 1) * 512], (), wu[sl % 2].b)
            DMA("pool", wd[sl % 2][:], wdn_v[:, sl * 4:(sl + 1) * 4, :], (), wd[sl % 2].b)

        load_mlp(0)
        for sl in range(8):
            u, dd = wu[sl % 2], wd[sl % 2]
            if sl + 1 < 8:
                load_mlp(sl + 1)
            for ti in tiles_out:
                s0, n, j = TILES[ti]
                a = act[cnt % 2]
                cnt += 1
                if mg is not None and cnt % 2 == 0:
                    if next(mg, "done") == "done":
                        mg = None
                for fc in range(4):
                    ps = P32()
                    for k in range(8):
                        MM(ps[:, 0:n], u[:, k, fc * 128:(fc + 1) * 128], hT[:, k, s0:s0 + n], k == 0, k == 7,
                           u.b + [hT.b[ti]], ps.b)
                    r_ = rl[fc % 2]
                    ACT(r_[:, 0:n], ps[:, 0:n], AF.Relu, ps.b, r_.b)
                    TT("pool", a[:, fc, 0:n], r_[:, 0:n], r_[:, 0:n], ALU.mult, r_.b, a.b)
                for c in range(8):
                    ps = P32()
                    for fc in range(4):
                        MM(ps[:, 0:n], dd[:, fc, c * 128:(c + 1) * 128], a[:, fc, 0:n], fc == 0, fc == 3, dd.b + a.b, ps.b)
                    STT(xT[:, c, s0:s0 + n], ps[:, 0:n], mp[:, 40 + c, j:j + 1], xT[:, c, s0:s0 + n], ALU.mult, ALU.add,
                        ps.b + mp.b + xT.b, xT.b)
        if mg is not None:
            for _ in mg:
                pass
        S.fence()
        RW.reset()
        RY.reset()
        yT = RY.alloc("yT", [128, 12, NT], BF16, nb=1)


    try:
        for l in range(depth):
            layer(l)
    except _Stop:
        S.fence()
        RW.reset()

    xo = [RW.alloc(f"xo{i}", [128, D], F32) for i in range(3)]
    finals = []
    for tb in range(2, NB):
        o = xo[tb % 3]
        for hf in range(2):
            ps = P32()
            for j in range(4):
                k = hf * 4 + j
                TR(ps[:, j * 128:(j + 1) * 128], xT[:, k, tb * 128:(tb + 1) * 128], ident_f, xT.b + CB, ps.b)
            CP("act" if hf else "dve", o[:, hf * 512:(hf + 1) * 512], ps[:, 0:512], ps.b, o.b)
        finals.append(DMA("sp", out_d[(tb - 2) * 128:(tb - 1) * 128, :], o[:], o.b, ()))
    finals += list(dbg_out.values())
    S.emit(finals)
    return S


def _host_constants():
    s = np.arange(128)[:, None]
    t = np.arange(128)[None, :]
    ident = np.eye(128, dtype=np.float32)
    triF = (s <= t).astype(np.float32)
    triR = (s >= t).astype(np.float32)
    mnegF = np.where(s <= t, 0.0, NEG).astype(np.float32)
    mnegR = np.where(s >= t, 0.0, NEG).astype(np.float32)
    ones = np.ones((128, 128), np.float32)
    cst = np.concatenate([ident, triF, triR, mnegF, mnegR, ones], axis=1)
    m01 = np.concatenate([np.tile(triR, (1, 4)), np.tile(triF, (1, 4))], axis=1).astype(np.float32)
    pos = np.arange(SEQ)
    rows, cols = pos // 64, pos % 64
    inv = 10000.0 ** (-np.arange(16, dtype=np.float32) / 16)
    ang = np.stack([rows[:, None] * inv[None, :], cols[:, None] * inv[None, :]], axis=1).astype(np.float32)
    ang = ang.reshape(16, 128, 2, 16).transpose(1, 0, 2, 3).reshape(128, 512)
    rope = np.concatenate([np.cos(ang), np.sin(ang)], axis=1).astype(np.float32)
    return cst, m01, rope


_CACHE = {}


def kernel(x, c, ctx, c_ctx, w_mod, b_mod, norm1_g, w_in, ml_b_i, ml_b_f, ml_norm_g,
           ssd_conv_w, ssd_conv_b, ssd_a_log, ssd_dt_bias, ssd_d, ssd_norm_g,
           att_qn_g, att_kn_g, att_sink, w_out, norm2_g, w_up, w_down, _dbg=(), _cores=8, _stop=None):
    f = lambda a: np.ascontiguousarray(np.asarray(a, dtype=np.float32))
    x, c, ctx, c_ctx = f(x), f(c), f(ctx), f(c_ctx)
    depth = w_mod.shape[0]
    cst, m01, rope = _host_constants()
    pk = lambda v: f(v).reshape(-1, 128).T
    bmod = np.stack([pk(b_mod[l]) for l in range(depth)])
    g1 = np.stack([pk(norm1_g[l]) for l in range(depth)])
    g2 = np.stack([pk(norm2_g[l]) for l in range(depth)])
    rowv = np.zeros((depth, 1, 1280), np.float32)
    convp = np.zeros((depth, 128, 24), np.float32)
    for l in range(depth):
        r = rowv[l, 0]
        r[0:8] = f(ml_b_i[l]).reshape(-1)
        r[8:16] = f(ml_b_f[l]).reshape(-1)
        r[16:528] = f(ml_norm_g[l])
        r[528:544] = f(ssd_a_log[l]).reshape(-1)
        r[544:560] = f(ssd_dt_bias[l]).reshape(-1)
        r[560:568] = f(ssd_d[l])
        r[568:1080] = f(ssd_norm_g[l])
        r[1080:1144] = f(att_qn_g[l])
        r[1144:1208] = f(att_kn_g[l])
        r[1208:1216] = f(att_sink[l])
        cw = f(ssd_conv_w[l])
        cb = f(ssd_conv_b[l])
        for ch in range(6):
            convp[l, :, ch * 4 + 0] = cw[0, ch * 128:(ch + 1) * 128]
            convp[l, :, ch * 4 + 1] = cw[1, ch * 128:(ch + 1) * 128]
            convp[l, :, ch * 4 + 2] = cw[2, ch * 128:(ch + 1) * 128]
            convp[l, :, ch * 4 + 3] = cb[ch * 128:(ch + 1) * 128]
    key = (depth, tuple(_dbg))
    nc = bass.Bass("TRN2", target_bir_lowering=False)
    build(nc, depth=depth, dbg=_dbg, stop_after=_stop)
    shared = {"w_mod": f(w_mod), "b_mod": bmod, "norm1_g": g1, "norm2_g": g2, "w_in": f(w_in), "w_out": f(w_out),
              "w_up": f(w_up), "w_down": f(w_down), "rowv": rowv, "convp": convp, "cst": cst, "m01": m01, "rope": rope}
    in_maps = []
    for b in range(_cores):
        cc = np.stack([pk(c[b]), pk(c_ctx)], axis=2).reshape(128, 16)
        m = dict(shared)
        m.update({"x": x[b], "ctx": ctx[b], "cc": np.ascontiguousarray(cc)})
        in_maps.append(m)
    res = run_bass_kernel_spmd(nc, in_maps, core_ids=list(range(_cores)))
    out = np.stack([res.results[b]["out"] for b in range(_cores)], axis=0).astype(np.float32)
    if _dbg:
        return out, res.results
    return out
```

```python
import math
from contextlib import ExitStack

import numpy as np
import ml_dtypes
import concourse.bass as bass
import concourse.mybir as mybir
from concourse.bass_utils import run_bass_kernel_spmd

F32 = mybir.dt.float32
BF16 = mybir.dt.bfloat16
ALU = mybir.AluOpType
AF = mybir.ActivationFunctionType
AX = mybir.AxisListType

D = 1024
SEQ = 2048
CTX = 256
NT = SEQ + CTX
NB = NT // 128
DEPTH = 2
EPS = 1e-6
IN_W = 4128
OFF_MLQ, OFF_MLK, OFF_MLV, OFF_MLO, OFF_MLI, OFF_MLF = 0, 512, 1024, 1536, 2048, 2056
OFF_Z, OFF_XBC, OFF_DT, OFF_AQ, OFF_AK, OFF_AV = 2064, 2576, 3344, 3360, 3872, 4000
TILES = [(0, 256, 1)] + [(256 + 512 * i, 512, 0) for i in range(4)]
NEG = -30000.0


class Buf:
    __slots__ = ("name", "writer", "readers", "dma_readers", "excl")

    def __init__(self, name=""):
        self.excl = False
        self.name = name
        self.writer = None
        self.readers = {}
        self.dma_readers = []


class Rec:
    __slots__ = ("eng", "fn", "deps", "inc", "is_dma", "sem", "count")

    def __init__(self, eng, fn, is_dma):
        self.eng = eng
        self.fn = fn
        self.deps = []
        self.inc = False
        self.is_dma = is_dma
        self.sem = None
        self.count = 0


class Sched:
    ENGS = ("pe", "dve", "act", "pool", "sp")
    NDMA = 24

    def __init__(self, nc):
        self.nc = nc
        self.streams = {e: [] for e in self.ENGS}
        self.dma_rr = 0
        self.dma_rr2 = [0, 0]
        self.dma_last = [None] * self.NDMA
        self.dma_cnt = [0] * self.NDMA
        self.last = {e: None for e in self.ENGS}

    def op(self, eng, fn, R=(), W=(), dma=False, extra=()):
        rec = Rec(eng, fn, dma)
        deps = {}

        def add(d):
            if d is None or d is rec:
                return
            if d.eng == "pe" and eng == "pe" and not d.is_dma and not dma:
                return
            deps[id(d)] = d

        for b in R:
            add(b.writer)
            if b.excl:
                for r in b.readers.values():
                    if r.eng != eng:
                        add(r)
        for b in W:
            add(b.writer)
            for r in b.readers.values():
                add(r)
            for r in b.dma_readers:
                add(r)
        for d in extra:
            add(d)
        if dma:
            half = self.NDMA // 2
            sw = 1 if eng == "pool" else 0
            k = sw * half + self.dma_rr2[sw]
            self.dma_rr2[sw] = (self.dma_rr2[sw] + 1) % half
            add(self.dma_last[k])
            self.dma_last[k] = rec
            self.dma_cnt[k] += 16
            rec.sem = k
            rec.count = self.dma_cnt[k]
        elif fn is not None:
            self.last[eng] = rec
        rec.deps = list(deps.values())
        for d in rec.deps:
            d.inc = True
        for b in R:
            if dma:
                b.dma_readers.append(rec)
            else:
                b.readers[eng] = rec
        for b in W:
            b.writer = rec
            b.readers = {}
            b.dma_readers = []
        self.streams[eng].append(rec)
        return rec

    def fence(self):
        pend = [r for r in self.last.values() if r is not None]
        pend += [r for r in self.dma_last if r is not None]
        for e in self.ENGS:
            self.op(e, None, extra=pend)

    def emit(self, final_recs):
        nc = self.nc
        for r in final_recs:
            r.inc = True
        for e in self.ENGS:
            c = 0
            for r in self.streams[e]:
                if r.is_dma or r.fn is None:
                    continue
                if r.inc:
                    c += 1
                    r.count = c
        with ExitStack() as es:
            esem = {e: es.enter_context(nc.semaphore(f"sem_{e}")) for e in self.ENGS}
            dsem = [es.enter_context(nc.semaphore(f"dsem{k}")) for k in range(self.NDMA)]
            block = es.enter_context(nc.Block())

            def replay(e, engine, extra_final=None):
                waited = {}

                def wait(d):
                    if d.is_dma:
                        key, sem = ("d", d.sem), dsem[d.sem]
                    else:
                        key, sem = ("e", d.eng), esem[d.eng]
                    if waited.get(key, 0) >= d.count:
                        return
                    waited[key] = d.count
                    engine.wait_ge(sem, d.count)

                for r in self.streams[e]:
                    for d in r.deps:
                        wait(d)
                    if r.fn is None:
                        continue
                    ins = r.fn(engine)
                    if r.is_dma:
                        ins.then_inc(dsem[r.sem], 16)
                    elif r.inc:
                        ins.then_inc(esem[e], 1)
                for d in extra_final or ():
                    wait(d)

            @block.tensor
            def _(eng):
                replay("pe", eng)

            @block.vector
            def _(eng):
                replay("dve", eng)

            @block.scalar
            def _(eng):
                replay("act", eng)

            @block.gpsimd
            def _(eng):
                replay("pool", eng)

            @block.sync
            def _(eng):
                replay("sp", eng, extra_final=final_recs)


class Tl:
    def __init__(self, t, nb, name):
        self.t = t
        self.b = [Buf(f"{name}.{i}") for i in range(nb)]

    def __getitem__(self, idx):
        return self.t[idx]


class Region:
    def alias(self, name, shape, dtype, off, bufs):
        self.n += 1
        t = self.nc.alloc_sbuf_tensor_at(f"{self.name}_{name}_{self.n}", list(shape), dtype, offset=self.base + off)
        tl = Tl(t, 0, name)
        tl.b = bufs
        return tl

    def __init__(self, nc, base, size, name):
        self.nc, self.base, self.size, self.cur, self.name, self.n = nc, base, size, 0, name, 0

    def reset(self):
        self.cur = 0

    def alloc(self, name, shape, dtype, nb=1):
        nbytes = int(np.prod(shape[1:])) * (4 if dtype == F32 else 2)
        nbytes = (nbytes + 31) // 32 * 32
        assert self.cur + nbytes <= self.size, f"region {self.name} overflow at {name}: {self.cur}+{nbytes}>{self.size}"
        self.n += 1
        t = self.nc.alloc_sbuf_tensor_at(f"{self.name}_{name}_{self.n}", list(shape), dtype, offset=self.base + self.cur)
        self.cur += nbytes
        return Tl(t, nb, name)


class _Stop(Exception):
    pass


def build(nc, depth=DEPTH, dbg=(), stop_after=None):
    S = Sched(nc)
    dbg_out = {}

    def din(name, shape, dt=F32):
        return nc.dram_tensor(name, list(shape), dt, kind="ExternalInput").ap()

    x_d = din("x", [SEQ, D])
    ctx_d = din("ctx", [CTX, D])
    cc_d = din("cc", [128, 16])
    wmod_d = din("w_mod", [depth, D, 6 * D])
    bmod_d = din("b_mod", [depth, 128, 48])
    g1_d = din("norm1_g", [depth, 128, 8])
    g2_d = din("norm2_g", [depth, 128, 8])
    win_d = din("w_in", [depth, D, IN_W])
    wout_d = din("w_out", [depth, 1536, D])
    wup_d = din("w_up", [depth, D, 4 * D])
    wdn_d = din("w_down", [depth, 4 * D, D])
    rowv_d = din("rowv", [depth, 1, 1280])
    convp_d = din("convp", [depth, 128, 24])
    cst_d = din("cst", [128, 128 * 6])
    m01_d = din("m01", [128, 1024])
    rope_d = din("rope", [128, 2 * 16 * 32])
    out_d = nc.dram_tensor("out", [SEQ, D], F32, kind="ExternalOutput").ap()
    xs_d = nc.dram_tensor("xs_scratch", [128, 8 * NT], F32, kind="Internal").ap()

    base0 = 16512
    slab = nc.alloc_sbuf_tensor("slab", [128, (229344 - base0) // 4 - 8], F32)
    sizes = dict(C=16384, X=73728, H=36864, Y=55296)
    off = base0
    RC = Region(nc, off, sizes["C"], "C"); off += sizes["C"]
    RX = Region(nc, off, sizes["X"], "X"); off += sizes["X"]
    RH = Region(nc, off, sizes["H"], "H"); off += sizes["H"]
    RY = Region(nc, off, sizes["Y"], "Y"); off += sizes["Y"]
    RW = Region(nc, off, 229344 - off - 64, "W")

    ps32 = [Tl(nc.alloc_psum_tensor(f"ps{i}", [128, 512], F32), 1, f"ps{i}") for i in range(6)]
    psb = [Tl(nc.alloc_psum_tensor(f"psb{i}", [128, 1024], BF16), 1, f"psb{i}") for i in range(2)]
    for _t in ps32 + psb:
        _t.b[0].excl = True
    pctr = [0, 0, 0]

    def P32():
        pctr[0] += 1
        return ps32[pctr[0] % 4]

    def PD():
        pctr[2] += 1
        return ps32[4 + pctr[2] % 2]

    def PB():
        pctr[1] += 1
        return psb[pctr[1] % 2]

    def MM(out, lhsT, rhs, start, stop, R, W):
        S.op("pe", lambda e: e.matmul(out, lhsT=lhsT, rhs=rhs, start=start, stop=stop), R, W)

    def TR(out, in_, ident, R, W):
        S.op("pe", lambda e: e.transpose(out=out, in_=in_, identity=ident), R, W)

    def ACT(out, in_, func, R, W, bias=None, scale=None):
        kw = {}
        if bias is not None:
            kw["bias"] = bias
        if scale is not None:
            kw["scale"] = scale
        S.op("act", lambda e: e.activation(out=out, in_=in_, func=func, **kw), R, W)

    import os as _os
    _nopool = _os.environ.get("KPOOL") != "1"

    def _pe(eng):
        return "dve" if (_nopool and eng == "pool") else eng

    def TT(eng, out, in0, in1, op, R, W):
        eng = _pe(eng)
        S.op(eng, lambda e: e.tensor_tensor(out=out, in0=in0, in1=in1, op=op), R, W)

    def TS(eng, out, in0, s1, op0, R, W, s2=None, op1=None):
        eng = _pe(eng)
        if op1 is None:
            S.op(eng, lambda e: e.tensor_scalar(out=out, in0=in0, scalar1=s1, scalar2=None, op0=op0), R, W)
        else:
            S.op(eng, lambda e: e.tensor_scalar(out=out, in0=in0, scalar1=s1, scalar2=s2, op0=op0, op1=op1), R, W)

    def STT(out, in0, scalar, in1, op0, op1, R, W):
        S.op("dve", lambda e: e.scalar_tensor_tensor(out=out, in0=in0, scalar=scalar, in1=in1, op0=op0, op1=op1), R, W)

    def CP(eng, out, in_, R, W):
        eng = _pe(eng)
        if eng == "act":
            S.op("act", lambda e: e.copy(out=out, in_=in_), R, W)
        else:
            S.op(eng, lambda e: e.tensor_copy(out=out, in_=in_), R, W)

    def RED(out, in_, op, R, W):
        S.op("dve", lambda e: e.tensor_reduce(out=out, in_=in_, axis=AX.X, op=op), R, W)

    def RECIP(out, in_, R, W):
        S.op("dve", lambda e: e.reciprocal(out=out, in_=in_), R, W)

    def MSET(eng, ap, val, W):
        eng = _pe(eng)
        S.op(eng, lambda e: e.memset(ap, val), (), W)

    def DMA(eng, out, in_, R, W, slow=False):
        if slow:
            return S.op(eng, lambda e: e.dma_start(out=out, in_=in_, allow_slow_non_contiguous=True), R, W, dma=True)
        return S.op(eng, lambda e: e.dma_start(out=out, in_=in_), R, W, dma=True)

    def dump(name, tl, shape, dt=F32):
        if name in dbg and name not in dbg_out:
            d = nc.dram_tensor("dbg_" + name, list(shape), dt, kind="ExternalOutput").ap()
            dbg_out[name] = DMA("sp", d, tl.t[:], tl.b, ())

    def rsqrt_chain(out, in_, add, R, W):
        ACT(out, in_, AF.Ln, R, W, bias=add, scale=1.0)
        ACT(out, out, AF.Exp, W, W, scale=-0.5)

    cst = RC.alloc("cst", [128, 768], F32)
    DMA("sp", cst[:], cst_d, (), cst.b)
    ident_f, triF, triR = cst[:, 0:128], cst[:, 128:256], cst[:, 256:384]
    mneg = [cst[:, 384:512], cst[:, 512:640]]
    ones_f = cst[:, 640:768]
    cstb = RC.alloc("cstb", [128, 256], BF16)
    DMA("pool", cstb[:, 0:128], cst_d[:, 0:128], (), cstb.b)
    DMA("pool", cstb[:, 128:256], cst_d[:, 640:768], (), cstb.b)
    ident_b, ones_b = cstb[:, 0:128], cstb[:, 128:256]
    m01 = RC.alloc("m01", [128, 1024], BF16)
    DMA("pool", m01[:], m01_d, (), m01.b)
    rope = RC.alloc("rope", [128, 1024], F32)
    DMA("sp", rope[:], rope_d, (), rope.b)
    rcos = rope[:, 0:512].rearrange("p (b h i) -> p b h i", b=16, h=2)
    rsin = rope[:, 512:1024].rearrange("p (b h i) -> p b h i", b=16, h=2)
    CB = cst.b + cstb.b

    modp = [RC.alloc(f"modp{l}", [128, 48, 2], F32) for l in range(depth)]
    rowv = RC.alloc("rowv", [128, 1280], F32)
    convp = RC.alloc("convp", [128, 24], F32)

    cc = RC.alloc("cc", [128, 16], F32)
    DMA("sp", cc[:], cc_d, (), cc.b)
    s2b = RC.alloc("s2b", [128, 8, 2], BF16)
    ACT(s2b[:].rearrange("p k j -> p (k j)"), cc[:], AF.Silu, cc.b, s2b.b)
    mnegb = RC.alloc("mnegb", [128, 256], BF16)
    DMA("pool", mnegb[:], cst_d[:, 384:640], (), mnegb.b)
    CB = CB + mnegb.b

    def mod_gen(l, wm, bm, g12):
        DMA("sp", bm[:], bmod_d[l], (), bm.b)
        DMA("sp", g12[:, 0:8], g1_d[l], (), g12.b)
        DMA("sp", g12[:, 8:16], g2_d[l], (), g12.b)
        mp = modp[l]
        def ld(fg):
            w_ = wm[fg % len(wm)]
            DMA("pool", w_[:], wmod_d[l].rearrange("(k p) f -> p k f", p=128)[:, :, fg * 512:(fg + 1) * 512], (), w_.b)

        ld(0)
        for fg in range(12):
            w = wm[fg % len(wm)]
            if fg + 1 < 12 and len(wm) > 1:
                ld(fg + 1)
            elif fg > 0 and len(wm) == 1:
                ld(fg)
            ps = P32()
            for j in range(4):
                for k in range(8):
                    MM(ps[:, 2 * j:2 * j + 2], w[:, k, j * 128:(j + 1) * 128], s2b[:, k, :], k == 0, k == 7,
                       w.b + s2b.b, ps.b)
            TT("dve", mp[:, fg * 4:(fg + 1) * 4, :], ps[:, 0:8].rearrange("p (a j) -> p a j", j=2),
               bm[:, fg * 4:(fg + 1) * 4].unsqueeze(2).to_broadcast([128, 4, 2]), ALU.add, ps.b + bm.b, mp.b)
            yield
        for (so, go) in ((8, 0), (32, 8)):
            TS("dve", mp[:, so:so + 8, :], mp[:, so:so + 8, :], 1.0, ALU.add, mp.b, mp.b, s2=32.0, op1=ALU.mult)
            TT("dve", mp[:, so:so + 8, :], mp[:, so:so + 8, :],
               g12[:, go:go + 8].unsqueeze(2).to_broadcast([128, 8, 2]), ALU.mult, mp.b + g12.b, mp.b)
        yield

    bm0 = RW.alloc("bm", [128, 48], F32)
    g120 = RW.alloc("g12", [128, 16], F32)
    wm0 = [RW.alloc(f"wm{i}", [128, 8, 512], BF16) for i in range(2)]
    for _ in mod_gen(0, wm0, bm0, g120):
        pass
    S.fence()
    RW.reset()

    xT = RX.alloc("xT", [128, 8, NT], F32, nb=1)
    xin = [RW.alloc(f"xin{i}", [128, D], F32) for i in range(3)]
    for tb in range(NB):
        xi = xin[tb % 3]
        src = ctx_d[tb * 128:(tb + 1) * 128, :] if tb < 2 else x_d[(tb - 2) * 128:(tb - 1) * 128, :]
        DMA("sp", xi[:], src, (), xi.b)
        for hf in range(2):
            ps = P32()
            for j in range(4):
                k = hf * 4 + j
                TR(ps[:, j * 128:(j + 1) * 128], xi[:, k * 128:(k + 1) * 128], ident_f, xi.b + CB, ps.b)
            CP("act" if hf else "dve", xT[:, hf * 4:(hf + 1) * 4, tb * 128:(tb + 1) * 128],
               ps[:, :].rearrange("p (a t) -> p a t", a=4), ps.b, xT.b)
    S.fence()
    RW.reset()

    hT = RH.alloc("hT", [128, 8, NT], BF16, nb=len(TILES))
    yT = RY.alloc("yT", [128, 12, NT], BF16, nb=1)

    def norm_to_h(l, a_off, b_off, tiles):
        mp = modp[l]
        sq = RW.alloc("nsq", [128, 8, 512], BF16)
        rs = RW.alloc("nrs", [128, 512], F32)
        tmp = [RW.alloc(f"ntmp{i}", [128, 512], F32) for i in range(2)]
        for ti, (s0, n, j) in enumerate(TILES):
            if ti not in tiles:
                continue
            ACT(sq[:, :, 0:n], xT[:, :, s0:s0 + n], AF.Square, xT.b, sq.b)
            ps = P32()
            for k in range(8):
                MM(ps[:, 0:n], ones_b, sq[:, k, 0:n], k == 0, k == 7, sq.b + CB, ps.b)
            rsqrt_chain(rs[:, 0:n], ps[:, 0:n], float(D * EPS), ps.b, rs.b)
            for k in range(8):
                t = tmp[k % 2]
                STT(t[:, 0:n], xT[:, k, s0:s0 + n], mp[:, a_off + k, j:j + 1], rs[:, 0:n], ALU.mult, ALU.mult,
                    xT.b + mp.b + rs.b, t.b)
                ACT(hT[:, k, s0:s0 + n], t[:, 0:n], AF.Identity, t.b + mp.b, [hT.b[ti]],
                    bias=mp[:, b_off + k, j:j + 1], scale=1.0)

    win_v = [win_d[l].rearrange("(k p) f -> p k f", p=128) for l in range(depth)]

    def load_w(tl, l, col0, ncols, dst0=0):
        DMA("pool", tl[:, :, dst0:dst0 + ncols], win_v[l][:, :, col0:col0 + ncols], (), tl.b)

    def proj_tok(w, c0, ncols, tb, ti, ps=None):
        ps = ps or P32()
        for k in range(8):
            MM(ps[:, 0:ncols], hT[:, k, tb * 128:(tb + 1) * 128], w[:, k, c0:c0 + ncols], k == 0, k == 7,
               w.b + [hT.b[ti]], ps.b)
        return ps

    def proj_feat(w, c0, ti, ps=None):
        s0, n, j = TILES[ti]
        ps = ps or P32()
        for k in range(8):
            MM(ps[:, 0:n], w[:, k, c0:c0 + 128], hT[:, k, s0:s0 + n], k == 0, k == 7, w.b + [hT.b[ti]], ps.b)
        return ps

    def tile_of(tb):
        return 0 if tb < 2 else 1 + (tb - 2) // 4

    def transpose_to_yT(src, chunk, blocks):
        blocks = list(blocks)
        for i in range(0, len(blocks), 4):
            grp = blocks[i:i + 4]
            pb = PB()
            for jj, tb in enumerate(grp):
                TR(pb[:, jj * 128:(jj + 1) * 128], src[:, tb, :], ident_b, src.b + CB, pb.b)
            if grp[-1] - grp[0] == len(grp) - 1:
                CP("act", yT[:, chunk, grp[0] * 128:(grp[-1] + 1) * 128], pb[:, 0:len(grp) * 128], pb.b, yT.b)
            else:
                for jj, tb in enumerate(grp):
                    CP("act", yT[:, chunk, tb * 128:(tb + 1) * 128], pb[:, jj * 128:(jj + 1) * 128], pb.b, yT.b)

    def layer(l):
        nonlocal xT, yT
        last = l == depth - 1
        need_ctx = not last
        out_blocks = list(range(NB)) if need_ctx else list(range(2, NB))
        tiles_all = list(range(len(TILES)))
        mp = modp[l]
        DMA("sp", rowv[:], rowv_d[l].partition_broadcast(128), (), rowv.b)
        DMA("sp", convp[:], convp_d[l], (), convp.b)
        rv = lambda a, n: rowv[:, a:a + n]
        RV_BI, RV_BF, RV_MLG, RV_ALOG, RV_DTB, RV_DSK, RV_SSDG, RV_QG, RV_KG, RV_SINK = 0, 8, 16, 528, 544, 560, 568, 1080, 1144, 1208

        norm_to_h(l, 8, 0, tiles_all)
        if stop_after == 'norm1' and l == 0:
            raise _Stop()
        DMA("sp", xs_d, xT[:].rearrange("p k t -> p (k t)"), xT.b, ())
        S.fence()
        RW.reset()
        RX.reset()

        wg = RW.alloc("wg", [128, 8, 32], BF16)
        load_w(wg, l, OFF_MLI, 16, 0)
        load_w(wg, l, OFF_DT, 16, 16)
        GW = 24
        wt = RX.alloc("wt", [128, NB, GW], F32)
        wsp = RX.alloc("wsp", [128, NB, GW], F32)
        cbias = RX.alloc("cbias", [128, NB, GW], F32)
        dec = RX.alloc("dec", [128, NB, GW], F32)
        GTt = {k: RW.alloc(f"GT{k}", [72, 128], F32) for k in range(6)}
        GTh = {k: RX.alloc(f"GTh{k}", [72, 128], BF16) for k in range(6)}
        GTl = {k: RX.alloc(f"GTl{k}", [72, 128], BF16) for k in range(6)}
        gate_mark = RX.cur
        graw = RW.alloc("graw", [128, NB, 32], F32)
        for tb in range(NB):
            ps = proj_tok(wg, 0, 32, tb, tile_of(tb))
            CP("dve", graw[:, tb, :], ps[:, 0:32], ps.b, graw.b)
        lnb = RW.alloc("lnb", [128, NB, GW], F32)
        ldec = RW.alloc("ldec", [128, NB, GW], F32)
        fcs = RW.alloc("fcs", [128, NB, GW], F32)
        tot = RW.alloc("tot", [128, NB, GW], F32)
        gtmp = RW.alloc("gtmp", [128, NB, 16], F32)
        gtmp2 = RW.alloc("gtmp2", [128, NB, 16], F32)

        def bc(a, n):
            return rv(a, n).unsqueeze(1).to_broadcast([128, NB, n])

        TT("dve", lnb[:, :, 0:8], graw[:, :, 0:8], bc(RV_BI, 8), ALU.add, graw.b + rowv.b, lnb.b)
        TT("dve", gtmp[:, :, 0:8], graw[:, :, 8:16], bc(RV_BF, 8), ALU.add, graw.b + rowv.b, gtmp.b)
        ACT(gtmp[:, :, 0:8], gtmp[:, :, 0:8], AF.Exp, gtmp.b, gtmp.b, scale=-1.0)
        ACT(gtmp[:, :, 0:8], gtmp[:, :, 0:8], AF.Ln, gtmp.b, gtmp.b, bias=1.0, scale=1.0)
        TS("dve", ldec[:, :, 0:8], gtmp[:, :, 0:8], -1.0, ALU.mult, gtmp.b, ldec.b)
        TT("dve", gtmp[:], graw[:, :, 16:32], bc(RV_DTB, 16), ALU.add, graw.b + rowv.b, gtmp.b)
        STT(gtmp2[:], gtmp[:], -1.0, gtmp[:], ALU.mult, ALU.max, gtmp.b, gtmp2.b)
        ACT(gtmp2[:], gtmp2[:], AF.Exp, gtmp2.b, gtmp2.b, scale=-1.0)
        ACT(gtmp2[:], gtmp2[:], AF.Ln, gtmp2.b, gtmp2.b, bias=1.0, scale=1.0)
        STT(gtmp[:], gtmp[:], 0.0, gtmp2[:], ALU.max, ALU.add, gtmp.b + gtmp2.b, gtmp.b)
        ACT(lnb[:, :, 8:24], gtmp[:], AF.Ln, gtmp.b, lnb.b)
        aexp = RW.alloc("aexp", [128, 16], F32)
        ACT(aexp[:], rv(RV_ALOG, 16), AF.Exp, rowv.b, aexp.b)
        TT("dve", gtmp2[:], gtmp[:], aexp[:].unsqueeze(1).to_broadcast([128, NB, 16]), ALU.mult, gtmp.b + aexp.b, gtmp2.b)
        TS("dve", ldec[:, :, 8:24], gtmp2[:], -1.0, ALU.mult, gtmp2.b, ldec.b)
        colsets = [(0, 4, 0), (4, 8, 1), (8, 16, 0), (16, 24, 1)]
        for (c0, c1, d) in colsets:
            ps = P32()
            nn = (c1 - c0) * NB
            MM(ps[:, 0:nn].rearrange("p (b c) -> p b c", b=NB), triR if d else triF, ldec[:, :, c0:c1], True, True, ldec.b + CB, ps.b)
            CP("dve", fcs[:, :, c0:c1], ps[:, 0:nn].rearrange("p (b c) -> p b c", b=NB), ps.b, fcs.b)
        ps = P32()
        MM(ps[:, 0:NB * GW], ones_f, ldec[:].rearrange("p b c -> p (b c)"), True, True, ldec.b + CB, ps.b)
        CP("dve", tot[:].rearrange("p b c -> p (b c)"), ps[:, 0:NB * GW], ps.b, tot.b)
        ACT(wt[:], fcs[:], AF.Exp, fcs.b, wt.b)
        ACT(dec[:], tot[:], AF.Exp, tot.b, dec.b)
        TT("dve", cbias[:], lnb[:], fcs[:], ALU.subtract, lnb.b + fcs.b, cbias.b)
        TT("dve", wsp[:], cbias[:], tot[:], ALU.add, cbias.b + tot.b, wsp.b)
        ACT(wsp[:], wsp[:], AF.Exp, wsp.b, wsp.b)
        fd = RW.alloc("fd", [128, NB, 4], F32)
        GT = {}
        gsets = [("m", 0, 0, 0), ("m", 1, 0, 4), ("s", 0, 0, 8), ("s", 0, 1, 12), ("s", 1, 0, 16), ("s", 1, 1, 20)]
        for gi, (fam, d, g, c0) in enumerate(gsets):
            CP("dve", fd[:], fcs[:, :, c0:c0 + 4], fcs.b, fd.b)
            ps = P32()
            TR(ps[0:72, 0:128], fd[:].rearrange("p b c -> p (b c)"), ident_f, fd.b + CB, ps.b)
            gt = GTt[gi]
            CP("dve", gt[:], ps[0:72, 0:128], ps.b, gt.b)
            CP("act", GTh[gi][:], gt[:], gt.b, GTh[gi].b)
            TT("dve", GTl[gi][:], gt[:], GTh[gi][:], ALU.subtract, gt.b + GTh[gi].b, GTl[gi].b)
            GT[(fam, d, g)] = (GTh[gi], GTl[gi])
        dump("cbias", cbias, [128, NB, GW]); dump("wt", wt, [128, NB, GW]); dump("wsp", wsp, [128, NB, GW])
        S.fence()
        RW.reset()

        def scan_order(d):
            return list(range(NB)) if d == 0 else [1, 0] + list(range(NB - 1, 1, -1))

        def dt_tile(gt, r, d, tb, col, dst, ps=None, c0=0):
            ps = ps or PD()
            gh, gl = gt
            sel = ident_b[0:72, r:r + 1].to_broadcast([72, 128])
            MM(ps[:, c0:c0 + 128], sel, gh[:, :], True, False, gh.b + CB, ps.b)
            MM(ps[:, c0:c0 + 128], sel, gl[:, :], False, False, gl.b + CB, ps.b)
            MM(ps[:, c0:c0 + 128], ident_b, mnegb[:, d * 128:(d + 1) * 128], False, True, CB, ps.b)
            ACT(dst[:, :], ps[:, c0:c0 + 128], AF.Exp, ps.b + cbias.b, dst.b, bias=cbias[:, tb, col:col + 1], scale=1.0)

        if stop_after == 'gates' and l == 0:
            raise _Stop()
        wq = [RW.alloc(f"wq{i}", [128, 8, 512], BF16) for i in range(2)]
        hbuf = []
        for si in range(2):
            if si == 0:
                al = lambda nm, shp, dt: RX.alloc(nm, shp, dt)
            else:
                yoff = [4 * NT * 2]

                def al(nm, shp, dt):
                    nbytes = (int(np.prod(shp[1:])) * (4 if dt == F32 else 2) + 31) // 32 * 32
                    t_ = RY.alias(nm, shp, dt, yoff[0], [Buf(nm)])
                    yoff[0] += nbytes
                    assert yoff[0] <= 12 * NT * 2
                    return t_
            hbuf.append([al(f"qT{si}", [128, NT], BF16), al(f"kT{si}", [128, NT], BF16),
                         al(f"ktok{si}", [128, NB, 128], BF16), al(f"vaug{si}", [128, NB, 130], BF16),
                         al(f"osig{si}", [128, NB, 128], F32)])
        hacc = RX.alloc("hacc", [128, NB, 128], F32, nb=NB)
        C32c = [RX.alloc(f"C32{i}", [128, 132], F32) for i in range(2)]
        Cbc = [[RX.alloc(f"Cb{i}{j}", [128, 132], BF16) for j in range(2)] for i in range(2)]
        DTs = [RX.alloc(f"DTs{i}", [128, 128], F32) for i in range(2)]
        PTs = [RX.alloc(f"PTs{i}", [128, 128], BF16) for i in range(2)]
        kws = [RX.alloc(f"kws{i}", [128, 128], BF16) for i in range(2)]
        tA = [RX.alloc(f"tA{i}", [128, 132], F32) for i in range(2)]
        tB = [RX.alloc(f"tB{i}", [128, 132], F32) for i in range(2)]
        dn = [RX.alloc(f"dn{i}", [128, 2], F32) for i in range(2)]
        mout = RX.alloc("mout", [128, NB, 128], BF16)
        ssq = RX.alloc("ssq", [128, NB], F32)
        sqt = RX.alloc("sqt", [128, NB, 128], F32)
        for si in range(2):
            MSET("dve", hbuf[si][3][:, :, 128:130], 1.0, hbuf[si][3].b)
        kscale = 128.0 ** -0.5
        orders = [scan_order(0), scan_order(1)]
        hset = set()

        def ml_proj(h):
            qT, kT, ktok, vaug, osig = hbuf[h % 2]
            w = wq[h % 2]
            for gq in range(4):
                load_w(w, l, gq * 512 + h * 128, 128, gq * 128)
            pc = 0
            for ti in tiles_all:
                s0, n, j = TILES[ti]
                ps = proj_feat(w, 0, ti, ps=ps32[4 + pc % 2]); pc += 1
                CP("act", qT[:, s0:s0 + n], ps[:, 0:n], ps.b, qT.b)
                yield
                ps = proj_feat(w, 128, ti, ps=ps32[4 + pc % 2]); pc += 1
                ACT(kT[:, s0:s0 + n], ps[:, 0:n], AF.Copy, ps.b, kT.b, scale=kscale)
                yield
            for tb in range(NB):
                ps = proj_tok(w, 128, 384, tb, tile_of(tb), ps=ps32[4 + pc % 2]); pc += 1
                ACT(ktok[:, tb, :], ps[:, 0:128], AF.Copy, ps.b, ktok.b, scale=kscale)
                CP("dve", vaug[:, tb, 0:128], ps[:, 128:256], ps.b, vaug.b)
                ACT(osig[:, tb, :], ps[:, 256:384], AF.Sigmoid, ps.b, osig.b)
                yield

        def ml_chain(d, h, hb_):
            for it in range(NB):
                tb = orders[d][it]
                gt = GT[("m", d, 0)]
                col = d * 4 + h
                blk = slice(tb * 128, (tb + 1) * 128)
                want = tb in out_blocks
                bank1, bank2 = ps32[2 * d], ps32[2 * d + 1]
                qT, kT, ktok, vaug = hb_[0], hb_[1], hb_[2], hb_[3]
                S_, B_ = bank1[:, 0:128], bank1[:, 128:258]
                A_, C_ = bank2[:, 0:130], bank2[:, 130:260]
                cb_cur, cb_nxt = Cbc[d][it % 2], Cbc[d][(it + 1) % 2]
                if want:
                    MM(S_, kT[:, blk], qT[:, blk], True, True, kT.b + qT.b, bank1.b)
                    yield
                    MM(B_, qT[:, blk], cb_cur[:, 0:130], True, True, qT.b + cb_cur.b, bank1.b)
                    yield
                if it < NB - 1:
                    ACT(kws[d][:], ktok[:, tb, :], AF.Identity, ktok.b + wsp.b, kws[d].b, scale=wsp[:, tb, col:col + 1])
                    yield
                    MM(C_, kws[d][:], vaug[:, tb, 0:130], True, True, kws[d].b + vaug.b, bank2.b)
                    yield
                    STT(C32c[d][:, 0:130], C32c[d][:, 0:130], dec[:, tb, col:col + 1], C_, ALU.mult, ALU.add,
                        C32c[d].b + dec.b + bank2.b, C32c[d].b)
                    yield
                    CP("act", cb_nxt[:, 0:130], C32c[d][:, 0:130], C32c[d].b, cb_nxt.b)
                    yield
                if want:
                    dt_tile(gt, tb * 4 + h, d, tb, col, DTs[d], ps=bank1, c0=260)
                    yield
                    TT("dve", PTs[d][:], S_, DTs[d][:], ALU.mult, bank1.b + DTs[d].b, PTs[d].b)
                    yield
                    MM(A_, PTs[d][:], vaug[:, tb, 0:130], True, True, PTs[d].b + vaug.b, bank2.b)
                    yield
                    ACT(tB[d][:, 0:130], B_, AF.Identity, bank1.b + wt.b, tB[d].b, scale=wt[:, tb, col:col + 1])
                    yield
                    TT("dve", tA[d][:, 0:130], A_, tB[d][:, 0:130], ALU.add, bank2.b + tB[d].b, tA[d].b)
                    yield
                    TS("dve", dn[d][:, 0:1], tA[d][:, 128:129], 1.0, ALU.max, tA[d].b, dn[d].b)
                    yield
                    STT(dn[d][:, 0:1], tA[d][:, 128:129], -1.0, dn[d][:, 0:1], ALU.mult, ALU.max, tA[d].b + dn[d].b, dn[d].b)
                    yield
                    RECIP(dn[d][:, 1:2], dn[d][:, 0:1], dn[d].b, dn[d].b)
                    yield
                    hb = [hacc.b[tb]]
                    if tb not in hset:
                        hset.add(tb)
                        TS("dve", hacc[:, tb, :], tA[d][:, 0:128], dn[d][:, 1:2], ALU.mult, tA[d].b + dn[d].b, hb)
                        yield
                    else:
                        STT(hacc[:, tb, :], tA[d][:, 0:128], dn[d][:, 1:2], hacc[:, tb, :], ALU.mult, ALU.add,
                            tA[d].b + dn[d].b + hb, hb)
                        yield


        def run_rr(gens):
            gens = list(gens)
            while gens:
                for g_ in list(gens):
                    try:
                        next(g_)
                    except StopIteration:
                        gens.remove(g_)

        run_rr([ml_proj(0)])
        for h in range(4):
            osig = hbuf[h % 2][4]
            for d in range(2):
                MSET("dve", C32c[d][:], 0.0, C32c[d].b)
                MSET("dve", Cbc[d][0][:], 0.0, Cbc[d][0].b)
            hset.clear()
            gl_ = [ml_chain(0, h, hbuf[h % 2]), ml_chain(1, h, hbuf[h % 2])]
            if h + 1 < 4:
                gl_.append(ml_proj(h + 1))
            run_rr(gl_)
            b0 = out_blocks[0]
            nbk = len(out_blocks)
            hv = hacc[:, b0:NB, :]
            TT("dve", osig[:, b0:NB, :], osig[:, b0:NB, :],
               rv(RV_MLG + h * 128, 128).unsqueeze(1).to_broadcast([128, nbk, 128]), ALU.mult, osig.b + rowv.b, osig.b)
            TT("dve", sqt[:, b0:NB, :], hv, hv, ALU.mult, hacc.b, sqt.b)
            RED(ssq[:, b0:NB], sqt[:, b0:NB, :], ALU.add, sqt.b, ssq.b)
            ACT(ssq[:, b0:NB], ssq[:, b0:NB], AF.Ln, ssq.b, ssq.b, bias=EPS, scale=1.0 / 128)
            ACT(ssq[:, b0:NB], ssq[:, b0:NB], AF.Exp, ssq.b, ssq.b, scale=-0.5)
            TT("dve", hv, hv, ssq[:, b0:NB].unsqueeze(2).to_broadcast([128, nbk, 128]), ALU.mult, hacc.b + ssq.b, hacc.b)
            TT("dve", mout[:, b0:NB, :], hv, osig[:, b0:NB, :], ALU.mult, hacc.b + osig.b, mout.b)
            transpose_to_yT(mout, h, out_blocks)
        S.fence()
        RX.cur = gate_mark
        RW.reset()

        if stop_after == 'mlstm' and l == 0:
            raise _Stop()
        wz = [RW.alloc(f"wz{i}", [128, 8, 256], BF16) for i in range(2)]
        wx = [RW.alloc(f"wx{i}", [128, 8, 256], BF16) for i in range(2)]
        wbc = RW.alloc("wbc", [128, 8, 256], BF16)
        DTs = [RW.alloc(f"DTs{i}", [128, 128], F32) for i in range(8)]
        PTs = [RW.alloc(f"PTs{i}", [128, 128], BF16) for i in range(8)]
        BT = RX.alloc("BT", [128, NT], BF16)
        CT = RX.alloc("CT", [128, NT], BF16)
        Btok = RX.alloc("Btok", [128, NB, 128], BF16)
        raw0_off = RX.cur
        raw = [RX.alloc(f"raw{i}", [128, NT], F32) for i in range(2)]
        raw[1].b = raw[0].b
        zs = RX.alias("zs", [128, NB, 256], BF16, raw0_off, raw[0].b)
        gg = RX.alias("gg", [128, 512], F32, raw0_off + 9216, raw[0].b)
        sout = RX.alias("sout", [128, NB, 128], BF16, raw0_off + 9216 + 2048, raw[0].b)
        xcT = RX.alloc("xcT", [128, NT], BF16)
        yacc = RX.alloc("yacc", [128, NB, 256], F32, nb=NB)
        H32c = [RX.alloc(f"H32{i}", [128, 256], F32) for i in range(2)]
        Hbc = [[RX.alloc(f"Hb{i}{j}", [128, 256], BF16) for j in range(2)] for i in range(2)]
        BwT = [RX.alloc(f"BwT{i}", [128, 128], BF16) for i in range(8)]
        ytmp = [RX.alloc(f"ytmp{i}", [128, 256], F32) for i in range(2)]
        ssq2 = RX.alloc("ssq2", [128, NB, 2], F32)
        rstd = RX.alloc("rstd", [128, NB], F32)
        G = RY.alias("G", [128, NB, 512], BF16, 8 * NT * 2, yT.b)
        xtok = RY.alias("xtok", [128, NB, 256], BF16, 4 * NT * 2, yT.b)

        def conv_silu(ps_src_fn, chunk, dst):
            r = raw[chunk % 2]
            for ti in tiles_all:
                s0, n, j = TILES[ti]
                ps = ps_src_fn(ti)
                CP("dve", r[:, s0:s0 + n], ps[:, 0:n], ps.b, r.b)
            acc = raw[(chunk + 1) % 2]
            cw = lambda i: convp[:, chunk * 4 + i:chunk * 4 + i + 1]
            ACT(acc[:, :], r[:, :], AF.Identity, r.b + convp.b, acc.b, bias=cw(3), scale=cw(1))
            for (a, b) in ((0, CTX), (CTX, NT)):
                STT(acc[:, a + 1:b], r[:, a:b - 1], cw(0), acc[:, a + 1:b], ALU.mult, ALU.add, r.b + acc.b + convp.b, acc.b)
                STT(acc[:, a:b - 1], r[:, a + 1:b], cw(2), acc[:, a:b - 1], ALU.mult, ALU.add, r.b + acc.b + convp.b, acc.b)
            ACT(dst[:, :], acc[:, :], AF.Silu, acc.b, dst.b)

        load_w(wbc, l, OFF_XBC + 512, 256)
        conv_silu(lambda ti: proj_feat(wbc, 0, ti), 4, BT)
        conv_silu(lambda ti: proj_feat(wbc, 128, ti), 5, CT)
        for i in range(0, NB, 4):
            pb = PB()
            for jj in range(4):
                tb = i + jj
                if tb < NB:
                    TR(pb[:, jj * 128:(jj + 1) * 128], BT[:, tb * 128:(tb + 1) * 128], ident_b, BT.b + CB, pb.b)
            nn = min(4, NB - i)
            CP("act", Btok[:, i:i + nn, :], pb[:, 0:nn * 128].rearrange("p (a c) -> p a c", a=nn), pb.b, Btok.b)
        for i in range(8):
            MSET("pool", BwT[i][:], 0.0, BwT[i].b)
        for g in range(2):
            load_w(wz[g], l, OFF_Z + g * 256, 256)
            load_w(wx[g], l, OFF_XBC + g * 256, 256)
            for cj in range(2):
                conv_silu(lambda ti, cj=cj: proj_feat(wx[g], cj * 128, ti), g * 2 + cj, xcT)
                for i in range(0, NB, 4):
                    pb = PB()
                    nn = min(4, NB - i)
                    for jj in range(nn):
                        tb = i + jj
                        TR(pb[:, jj * 128:(jj + 1) * 128], xcT[:, tb * 128:(tb + 1) * 128], ident_b, xcT.b + CB, pb.b)
                    CP("act", xtok[:, i:i + nn, cj * 128:(cj + 1) * 128],
                       pb[:, 0:nn * 128].rearrange("p (a c) -> p a c", a=nn), pb.b, xtok.b)
            for tb in range(NB):
                ps = proj_tok(wz[g], 0, 256, tb, tile_of(tb))
                ACT(zs[:, tb, :], ps[:, 0:256], AF.Silu, ps.b, zs.b)
            rows = slice(g * 64, (g + 1) * 64)
            orders = [scan_order(0), scan_order(1)]
            for d in range(2):
                MSET("dve", H32c[d][:], 0.0, H32c[d].b)
                MSET("pool", Hbc[d][0][:], 0.0, Hbc[d][0].b)
            yset = set()

            def ssd_chain(d):
                for it in range(NB):
                    tb = orders[d][it]
                    gt = GT[("s", d, g)]
                    c0 = 8 + d * 8 + g * 4
                    blk = slice(tb * 128, (tb + 1) * 128)
                    want = tb in out_blocks
                    lastit = it == NB - 1
                    bank1, bank2, bankD = ps32[2 * d], ps32[2 * d + 1], ps32[4 + d]
                    S_, B_ = bank1[:, 0:128], bank1[:, 128:384]
                    A_, C_ = bank2[:, 0:256], bank2[:, 256:512]
                    hb_cur, hb_nxt = Hbc[d][it % 2], Hbc[d][(it + 1) % 2]
                    H32 = H32c[d]
                    if want:
                        MM(S_, BT[rows, blk], CT[rows, blk], True, True, BT.b + CT.b, bank1.b)
                        yield
                        MM(B_, CT[rows, blk], hb_cur[rows, :], True, True, CT.b + hb_cur.b, bank1.b)
                        yield
                    if not lastit:
                        for hh in range(4):
                            col = c0 + hh
                            bw = BwT[d * 4 + hh]
                            ACT(bw[:, rows], Btok[:, tb, rows], AF.Identity, Btok.b + wsp.b, bw.b, scale=wsp[:, tb, col:col + 1])
                            yield
                            MM(bank2[:, 256 + hh * 64:256 + (hh + 1) * 64], bw[:], xtok[:, tb, hh * 64:(hh + 1) * 64], True, True,
                               bw.b + xtok.b, bank2.b)
                            yield
                        TT("dve", H32[rows, :].rearrange("p (a c) -> p a c", a=4), H32[rows, :].rearrange("p (a c) -> p a c", a=4),
                           dec[rows, tb, c0:c0 + 4].unsqueeze(2).to_broadcast([64, 4, 64]), ALU.mult, H32.b + dec.b, H32.b)
                        yield
                        TT("dve", H32[rows, :], H32[rows, :], bank2[rows, 256:512], ALU.add, H32.b + bank2.b, H32.b)
                        yield
                        CP("act", hb_nxt[rows, :], H32[rows, :], H32.b, hb_nxt.b)
                        yield
                    if want:
                        gh, gl = gt
                        for hh in range(4):
                            sel = ident_b[0:72, tb * 4 + hh:tb * 4 + hh + 1].to_broadcast([72, 128])
                            o_ = bankD[:, hh * 128:(hh + 1) * 128]
                            MM(o_, sel, gh[:, :], True, False, gh.b + CB, bankD.b)
                            MM(o_, sel, gl[:, :], False, False, gl.b + CB, bankD.b)
                            MM(o_, ident_b, mnegb[:, d * 128:(d + 1) * 128], False, True, CB, bankD.b)
                            yield
                        for hh in range(4):
                            col = c0 + hh
                            i2 = d * 4 + hh
                            ACT(DTs[i2][:, :], bankD[:, hh * 128:(hh + 1) * 128], AF.Exp, bankD.b + cbias.b, DTs[i2].b,
                                bias=cbias[:, tb, col:col + 1], scale=1.0)
                            yield
                        for hh in range(4):
                            i2 = d * 4 + hh
                            TT("dve", PTs[i2][:], S_, DTs[i2][:], ALU.mult, bank1.b + DTs[i2].b, PTs[i2].b)
                            yield
                        for hh in range(4):
                            i2 = d * 4 + hh
                            MM(bank2[:, hh * 64:(hh + 1) * 64], PTs[i2][:], xtok[:, tb, hh * 64:(hh + 1) * 64], True, True,
                               PTs[i2].b + xtok.b, bank2.b)
                            yield
                        yt = ytmp[d]
                        TT("dve", yt[:].rearrange("p (a c) -> p a c", a=4), B_.rearrange("p (a c) -> p a c", a=4),
                           wt[:, tb, c0:c0 + 4].unsqueeze(2).to_broadcast([128, 4, 64]), ALU.mult, bank1.b + wt.b, yt.b)
                        yield
                        yb = [yacc.b[tb]]
                        if tb not in yset:
                            yset.add(tb)
                            TT("dve", yacc[:, tb, :], A_, yt[:], ALU.add, bank2.b + yt.b, yb)
                            yield
                        else:
                            TT("dve", yt[:], A_, yt[:], ALU.add, bank2.b + yt.b, yt.b)
                            yield
                            TT("pool", yacc[:, tb, :], yacc[:, tb, :], yt[:], ALU.add, yb + yt.b, yb)
                            yield

            gens = [ssd_chain(0), ssd_chain(1)]
            while gens:
                for g_ in list(gens):
                    try:
                        next(g_)
                    except StopIteration:
                        gens.remove(g_)
            b0 = out_blocks[0]
            nbk = len(out_blocks)
            yv = yacc[:, b0:NB, :]
            for tb in out_blocks:
                yt = ytmp[tb % 2]
                TT("pool", yt[:].rearrange("p (a c) -> p a c", a=4), xtok[:, tb, :].rearrange("p (a c) -> p a c", a=4),
                   rv(RV_DSK + g * 4, 4).unsqueeze(2).to_broadcast([128, 4, 64]), ALU.mult, xtok.b + rowv.b, yt.b)
                TT("dve", yacc[:, tb, :], yacc[:, tb, :], yt[:], ALU.add, yacc.b + yt.b, yacc.b)
                TT("dve", yacc[:, tb, :], yacc[:, tb, :], zs[:, tb, :], ALU.mult, yacc.b + zs.b, yacc.b)
                CP("pool", G[:, tb, g * 256:(g + 1) * 256], yacc[:, tb, :], yacc.b, G.b)
                ACT(yt[:], yacc[:, tb, :], AF.Square, yacc.b, yt.b)
                RED(ssq2[:, tb, g:g + 1], yt[:], ALU.add, yt.b, ssq2.b)
        b0 = out_blocks[0]
        nbk = len(out_blocks)
        TT("dve", rstd[:, b0:NB], ssq2[:, b0:NB, 0], ssq2[:, b0:NB, 1], ALU.add, ssq2.b, rstd.b)
        ACT(rstd[:, b0:NB], rstd[:, b0:NB], AF.Ln, rstd.b, rstd.b, bias=EPS, scale=1.0 / 512)
        ACT(rstd[:, b0:NB], rstd[:, b0:NB], AF.Exp, rstd.b, rstd.b, scale=-0.5)
        for tb in out_blocks:
            TS("dve", gg[:], G[:, tb, :], rstd[:, tb:tb + 1], ALU.mult, G.b + rstd.b, gg.b)
            TT("pool", G[:, tb, :], gg[:], rv(RV_SSDG, 512), ALU.mult, gg.b + rowv.b, G.b)
        for cch in range(4):
            for tb in out_blocks:
                CP("pool", sout[:, tb, :], G[:, tb, cch * 128:(cch + 1) * 128], G.b, sout.b)
            transpose_to_yT(sout, 4 + cch, out_blocks)
        S.fence()
        RX.cur = gate_mark
        RW.reset()

        if stop_after == 'ssd' and l == 0:
            raise _Stop()
        wa = RW.alloc("wa", [128, 8, 768], BF16)
        load_w(wa, l, OFF_AQ, 768)
        qTa = RX.alloc("qTa", [128, 4, NT], BF16)
        kTa = RX.alloc("kTa", [128, 2, NT], BF16)
        va = RX.alloc("va", [128, NB, 2, 66], BF16)
        qraw = [RX.alloc(f"qraw{i}", [128, 640], F32) for i in range(2)]
        qsq = [RX.alloc(f"qsq{i}", [128, 640], F32) for i in range(2)]
        qn = [RX.alloc(f"qn{i}", [128, 640], F32) for i in range(2)]
        qr = [RX.alloc(f"qr{i}", [128, 640], BF16) for i in range(2)]
        kd = [RX.alloc(f"kd{i}", [128, 2, 128], BF16) for i in range(2)]
        rq = [RX.alloc(f"rq{i}", [128, 10], F32) for i in range(2)]
        t1 = [RX.alloc(f"t1{i}", [128, 10, 2, 16], F32) for i in range(2)]
        t2 = [RX.alloc(f"t2{i}", [128, 10, 2, 16], F32) for i in range(2)]
        PTa = [RW.alloc(f"PTa{i}", [128, 512], BF16) for i in range(10)]
        otok = [RX.alloc(f"otok{i}", [128, 512], BF16) for i in range(2)]
        dna = RX.alloc("dna", [128, 16], F32)
        esink = RX.alloc("esink", [128, 8], F32)
        gqk = RX.alloc("gqk", [128, 640], F32)
        MSET("pool", va[:, :, :, 64:66], 1.0, va.b)
        ACT(esink[:], rv(RV_SINK, 8), AF.Exp, rowv.b, esink.b)
        for hh in range(8):
            CP("dve", gqk[:, hh * 64:(hh + 1) * 64], rv(RV_QG, 64), rowv.b, gqk.b)
        for hh in range(2):
            TS("dve", gqk[:, 512 + hh * 64:512 + (hh + 1) * 64], rv(RV_KG, 64), 8.0, ALU.mult, rowv.b, gqk.b)
        def att_prep(tb):
            i2 = tb % 2
            ps = proj_tok(wa, 0, 512, tb, tile_of(tb), ps=ps32[2 * i2])
            yield
            CP("act", qraw[i2][:, 0:512], ps[:, 0:512], ps.b, qraw[i2].b)
            yield
            ps = proj_tok(wa, 512, 256, tb, tile_of(tb), ps=ps32[2 * i2 + 1])
            yield
            CP("act", qraw[i2][:, 512:640], ps[:, 0:128], ps.b, qraw[i2].b)
            yield
            CP("dve", va[:, tb, :, 0:64], ps[:, 128:256].rearrange("p (g c) -> p g c", g=2), ps.b, va.b)
            yield
            q3 = qraw[i2][:].rearrange("p (h c) -> p h c", h=10)
            TT("dve", qsq[i2][:], qraw[i2][:], qraw[i2][:], ALU.mult, qraw[i2].b, qsq[i2].b)
            yield
            RED(rq[i2][:], qsq[i2][:].rearrange("p (h c) -> p h c", h=10), ALU.add, qsq[i2].b, rq[i2].b)
            yield
            ACT(rq[i2][:], rq[i2][:], AF.Ln, rq[i2].b, rq[i2].b, bias=64 * EPS, scale=1.0)
            yield
            ACT(rq[i2][:], rq[i2][:], AF.Exp, rq[i2].b, rq[i2].b, scale=-0.5)
            yield
            qn3 = qn[i2][:].rearrange("p (h c) -> p h c", h=10)
            TT("dve", qn3, q3, rq[i2][:].unsqueeze(2).to_broadcast([128, 10, 64]), ALU.mult, qraw[i2].b + rq[i2].b, qn[i2].b)
            yield
            if tb >= 2:
                TT("pool", qn[i2][:], qn[i2][:], gqk[:], ALU.mult, qn[i2].b + gqk.b, qn[i2].b)
                yield
                q5 = qn[i2][:].rearrange("p (h a b i) -> p h a b i", h=10, a=2, b=2)
                o5 = qr[i2][:].rearrange("p (h a b i) -> p h a b i", h=10, a=2, b=2)
                cosb = rcos[:, tb - 2, :, :].unsqueeze(1).to_broadcast([128, 10, 2, 16])
                sinb = rsin[:, tb - 2, :, :].unsqueeze(1).to_broadcast([128, 10, 2, 16])
                x1, x2 = q5[:, :, :, 0, :], q5[:, :, :, 1, :]
                TT("dve", t1[i2][:], x1, cosb, ALU.mult, qn[i2].b + rope.b, t1[i2].b)
                yield
                TT("pool", t2[i2][:], x2, sinb, ALU.mult, qn[i2].b + rope.b, t2[i2].b)
                yield
                TT("dve", o5[:, :, :, 0, :], t1[i2][:], t2[i2][:], ALU.subtract, t1[i2].b + t2[i2].b, qr[i2].b)
                yield
                TT("dve", t1[i2][:], x2, cosb, ALU.mult, qn[i2].b + rope.b, t1[i2].b)
                yield
                TT("pool", t2[i2][:], x1, sinb, ALU.mult, qn[i2].b + rope.b, t2[i2].b)
                yield
                TT("dve", o5[:, :, :, 1, :], t1[i2][:], t2[i2][:], ALU.add, t1[i2].b + t2[i2].b, qr[i2].b)
                yield
            else:
                TT("pool", qr[i2][:], qn[i2][:], gqk[:], ALU.mult, qn[i2].b + gqk.b, qr[i2].b)
                yield
            for g in range(2):
                for hf in range(2):
                    CP("pool", kd[i2][:, g, hf * 64:(hf + 1) * 64], qr[i2][:, 512 + g * 64:512 + (g + 1) * 64], qr[i2].b, kd[i2].b)
                    yield
            pb = psb[i2]
            for a in range(4):
                TR(pb[:, a * 128:(a + 1) * 128], qr[i2][:, a * 128:(a + 1) * 128], ident_b, qr[i2].b + CB, pb.b)
                yield
            CP("act", qTa[:, :, tb * 128:(tb + 1) * 128], pb[:, 0:512].rearrange("p (a t) -> p a t", a=4), pb.b, qTa.b)
            yield
            pb = psb[i2]
            for g in range(2):
                TR(pb[:, g * 128:(g + 1) * 128], kd[i2][:, g, :], ident_b, kd[i2].b + CB, pb.b)
                yield
            CP("act", kTa[:, :, tb * 128:(tb + 1) * 128], pb[:, 0:256].rearrange("p (a t) -> p a t", a=2), pb.b, kTa.b)
            yield

        pend, active = list(range(NB)), []
        while pend or active:
            while len(active) < 2 and pend:
                active.append(att_prep(pend.pop(0)))
            for g_ in list(active):
                try:
                    next(g_)
                except StopIteration:
                    active.remove(g_)
        dump("qTa", qTa, [128, 4, NT], BF16); dump("kTa", kTa, [128, 2, NT], BF16)
        pi = 0
        for qi, tb in enumerate(out_blocks):
            keys = [(0, None), (1, None)]
            if tb >= 2:
                if tb - 1 >= 2:
                    keys.append((tb - 1, 0))
                keys.append((tb, None))
                if tb + 1 < NB:
                    keys.append((tb + 1, 1))
            psO = [ps32[0], ps32[1]]
            for ki, (kb, mk) in enumerate(keys):
                for g in range(2):
                    pt = PTa[ki * 2 + g]
                    for hf in range(2):
                        psS = ps32[2 + hf + 2 * (pi % 2)]
                        prow = slice(hf * 64, (hf + 1) * 64)
                        MM(psS[:, 0:256].rearrange("p (a t) -> p a t", a=2),
                           kTa[prow, g, kb * 128:(kb + 1) * 128], qTa[prow, 2 * g:2 * g + 2, tb * 128:(tb + 1) * 128],
                           True, True, kTa.b + qTa.b, psS.b)
                        ACT(pt[:, hf * 256:(hf + 1) * 256], psS[:, 0:256], AF.Exp, psS.b, pt.b)
                    pi += 1
                    if mk is not None:
                        TT("pool", pt[:], pt[:], m01[:, mk * 512:(mk + 1) * 512], ALU.mult, pt.b + m01.b, pt.b)
            for g in range(2):
                for hf in range(2):
                    for al in range(2):
                        cb_ = hf * 2 + al
                        hl = 2 * al + hf
                        for ki, (kb, mk) in enumerate(keys):
                            pt = PTa[ki * 2 + g]
                            MM(psO[g][:, hl * 66:(hl + 1) * 66], pt[:, cb_ * 128:(cb_ + 1) * 128], va[:, kb, g, 0:66],
                               ki == 0, ki == len(keys) - 1, pt.b + va.b, psO[g].b)
            ot = otok[qi % 2]
            for g in range(2):
                o4 = psO[g][:, 0:264].rearrange("p (h c) -> p h c", h=4)
                TT("dve", dna[:, g * 4:(g + 1) * 4], o4[:, :, 64], esink[:, g * 4:(g + 1) * 4], ALU.add, psO[g].b + esink.b, dna.b)
                RECIP(dna[:, 8 + g * 4:8 + (g + 1) * 4], dna[:, g * 4:(g + 1) * 4], dna.b, dna.b)
                TT("dve", ot[:, g * 256:(g + 1) * 256].rearrange("p (h c) -> p h c", h=4), o4[:, :, 0:64],
                   dna[:, 8 + g * 4:8 + (g + 1) * 4].unsqueeze(2).to_broadcast([128, 4, 64]), ALU.mult, psO[g].b + dna.b, ot.b)
            pb = PB()
            for cch in range(4):
                TR(pb[:, cch * 128:(cch + 1) * 128], ot[:, cch * 128:(cch + 1) * 128], ident_b, ot.b + CB, pb.b)
            CP("act", yT[:, 8:12, tb * 128:(tb + 1) * 128], pb[:, 0:512].rearrange("p (a t) -> p a t", a=4), pb.b, yT.b)
        dump("yT", yT, [128, 12, NT], BF16)
        S.fence()
        RX.reset()
        RW.reset()

        if stop_after == 'attn' and l == 0:
            raise _Stop()
        xT = RX.alloc("xT", [128, 8, NT], F32, nb=1)
        DMA("sp", xT[:].rearrange("p k t -> p (k t)"), xs_d, (), xT.b)
        tiles_out = tiles_all if need_ctx else tiles_all[1:]
        wo = RW.alloc("wo", [128, 12, 1024], BF16)
        for hf in range(2):
            DMA("pool", wo[:, :, hf * 512:(hf + 1) * 512],
                wout_d[l].rearrange("(m p) f -> p m f", p=128)[:, :, hf * 512:(hf + 1) * 512], (), wo.b)
        for ti in tiles_out:
            s0, n, j = TILES[ti]
            for c in range(8):
                ps = P32()
                for m in range(12):
                    MM(ps[:, 0:n], wo[:, m, c * 128:(c + 1) * 128], yT[:, m, s0:s0 + n], m == 0, m == 11, wo.b + yT.b, ps.b)
                STT(xT[:, c, s0:s0 + n], ps[:, 0:n], mp[:, 16 + c, j:j + 1], xT[:, c, s0:s0 + n], ALU.mult, ALU.add,
                    ps.b + mp.b + xT.b, xT.b)
        dump("x1", xT, [128, 8, NT])
        S.fence()
        RW.reset()
        RY.reset()
        if stop_after == 'outproj' and l == 0:
            raise _Stop()
        norm_to_h(l, 32, 24, tiles_out)
        wu = [RY.alloc(f"wu{i}", [128, 8, 512], BF16) for i in range(2)]
        wd = [RY.alloc(f"wd{i}", [128, 4, 1024], BF16) for i in range(2)]
        act = [RY.alloc(f"act{i}", [128, 4, 512], BF16) for i in range(2)]
        rl = [RY.alloc(f"rl{i}", [128, 512], F32) for i in range(2)]
        wup_v = wup_d[l].rearrange("(k p) f -> p k f", p=128)
        wdn_v = wdn_d[l].rearrange("(m p) f -> p m f", p=128)
        cnt = 0
        mg = None
        if l + 1 < depth:
            bm1 = RW.alloc("bm", [128, 48], F32)
            g121 = RW.alloc("g12", [128, 16], F32)
            wm1 = [RW.alloc("wm1", [128, 8, 512], BF16), RY.alloc("wm1b", [128, 8, 512], BF16)]
            mg = mod_gen(l + 1, wm1, bm1, g121)

        def load_mlp(sl):
            DMA("pool", wu[sl % 2][:], wup_v[:, :, sl * 512:(sl + 1) * 512], (), wu[sl % 2].b)
            DMA("pool", wd[sl % 2][:], wdn_v[:, sl * 4:(sl + 1) * 4, :], (), wd[sl % 2].b)

        load_mlp(0)
        for sl in range(8):
            u, dd = wu[sl % 2], wd[sl % 2]
            if sl + 1 < 8:
                load_mlp(sl + 1)
            for ti in tiles_out:
                s0, n, j = TILES[ti]
                a = act[cnt % 2]
                cnt += 1
                if mg is not None and cnt % 2 == 0:
                    if next(mg, "done") == "done":
                        mg = None
                for fc in range(4):
                    ps = P32()
                    for k in range(8):
                        MM(ps[:, 0:n], u[:, k, fc * 128:(fc + 1) * 128], hT[:, k, s0:s0 + n], k == 0, k == 7,
                           u.b + [hT.b[ti]], ps.b)
                    r_ = rl[fc % 2]
                    ACT(r_[:, 0:n], ps[:, 0:n], AF.Relu, ps.b, r_.b)
                    TT("pool", a[:, fc, 0:n], r_[:, 0:n], r_[:, 0:n], ALU.mult, r_.b, a.b)
                for c in range(8):
                    ps = P32()
                    for fc in range(4):
                        MM(ps[:, 0:n], dd[:, fc, c * 128:(c + 1) * 128], a[:, fc, 0:n], fc == 0, fc == 3, dd.b + a.b, ps.b)
                    STT(xT[:, c, s0:s0 + n], ps[:, 0:n], mp[:, 40 + c, j:j + 1], xT[:, c, s0:s0 + n], ALU.mult, ALU.add,
                        ps.b + mp.b + xT.b, xT.b)
        if mg is not None:
            for _ in mg:
                pass
        S.fence()
        RW.reset()
        RY.reset()
        yT = RY.alloc("yT", [128, 12, NT], BF16, nb=1)


    try:
        for l in range(depth):
            layer(l)
    except _Stop:
        S.fence()
        RW.reset()

    xo = [RW.alloc(f"xo{i}", [128, D], F32) for i in range(3)]
    finals = []
    for tb in range(2, NB):
        o = xo[tb % 3]
        for hf in range(2):
            ps = P32()
            for j in range(4):
                k = hf * 4 + j
                TR(ps[:, j * 128:(j + 1) * 128], xT[:, k, tb * 128:(tb + 1) * 128], ident_f, xT.b + CB, ps.b)
            CP("act" if hf else "dve", o[:, hf * 512:(hf + 1) * 512], ps[:, 0:512], ps.b, o.b)
        finals.append(DMA("sp", out_d[(tb - 2) * 128:(tb - 1) * 128, :], o[:], o.b, ()))
    finals += list(dbg_out.values())
    S.emit(finals)
    return S


def _host_constants():
    s = np.arange(128)[:, None]
    t = np.arange(128)[None, :]
    ident = np.eye(128, dtype=np.float32)
    triF = (s <= t).astype(np.float32)
    triR = (s >= t).astype(np.float32)
    mnegF = np.where(s <= t, 0.0, NEG).astype(np.float32)
    mnegR = np.where(s >= t, 0.0, NEG).astype(np.float32)
    ones = np.ones((128, 128), np.float32)
    cst = np.concatenate([ident, triF, triR, mnegF, mnegR, ones], axis=1)
    m01 = np.concatenate([np.tile(triR, (1, 4)), np.tile(triF, (1, 4))], axis=1).astype(np.float32)
    pos = np.arange(SEQ)
    rows, cols = pos // 64, pos % 64
    inv = 10000.0 ** (-np.arange(16, dtype=np.float32) / 16)
    ang = np.stack([rows[:, None] * inv[None, :], cols[:, None] * inv[None, :]], axis=1).astype(np.float32)
    ang = ang.reshape(16, 128, 2, 16).transpose(1, 0, 2, 3).reshape(128, 512)
    rope = np.concatenate([np.cos(ang), np.sin(ang)], axis=1).astype(np.float32)
    return cst, m01, rope


_CACHE = {}


def kernel(x, c, ctx, c_ctx, w_mod, b_mod, norm1_g, w_in, ml_b_i, ml_b_f, ml_norm_g,
           ssd_conv_w, ssd_conv_b, ssd_a_log, ssd_dt_bias, ssd_d, ssd_norm_g,
           att_qn_g, att_kn_g, att_sink, w_out, norm2_g, w_up, w_down, _dbg=(), _cores=8, _stop=None):
    f = lambda a: np.ascontiguousarray(np.asarray(a, dtype=np.float32))
    x, c, ctx, c_ctx = f(x), f(c), f(ctx), f(c_ctx)
    depth = w_mod.shape[0]
    cst, m01, rope = _host_constants()
    pk = lambda v: f(v).reshape(-1, 128).T
    bmod = np.stack([pk(b_mod[l]) for l in range(depth)])
    g1 = np.stack([pk(norm1_g[l]) for l in range(depth)])
    g2 = np.stack([pk(norm2_g[l]) for l in range(depth)])
    rowv = np.zeros((depth, 1, 1280), np.float32)
    convp = np.zeros((depth, 128, 24), np.float32)
    for l in range(depth):
        r = rowv[l, 0]
        r[0:8] = f(ml_b_i[l]).reshape(-1)
        r[8:16] = f(ml_b_f[l]).reshape(-1)
        r[16:528] = f(ml_norm_g[l])
        r[528:544] = f(ssd_a_log[l]).reshape(-1)
        r[544:560] = f(ssd_dt_bias[l]).reshape(-1)
        r[560:568] = f(ssd_d[l])
        r[568:1080] = f(ssd_norm_g[l])
        r[1080:1144] = f(att_qn_g[l])
        r[1144:1208] = f(att_kn_g[l])
        r[1208:1216] = f(att_sink[l])
        cw = f(ssd_conv_w[l])
        cb = f(ssd_conv_b[l])
        for ch in range(6):
            convp[l, :, ch * 4 + 0] = cw[0, ch * 128:(ch + 1) * 128]
            convp[l, :, ch * 4 + 1] = cw[1, ch * 128:(ch + 1) * 128]
            convp[l, :, ch * 4 + 2] = cw[2, ch * 128:(ch + 1) * 128]
            convp[l, :, ch * 4 + 3] = cb[ch * 128:(ch + 1) * 128]
    key = (depth, tuple(_dbg))
    nc = bass.Bass("TRN2", target_bir_lowering=False)
    build(nc, depth=depth, dbg=_dbg, stop_after=_stop)
    shared = {"w_mod": f(w_mod), "b_mod": bmod, "norm1_g": g1, "norm2_g": g2, "w_in": f(w_in), "w_out": f(w_out),
              "w_up": f(w_up), "w_down": f(w_down), "rowv": rowv, "convp": convp, "cst": cst, "m01": m01, "rope": rope}
    in_maps = []
    for b in range(_cores):
        cc = np.stack([pk(c[b]), pk(c_ctx)], axis=2).reshape(128, 16)
        m = dict(shared)
        m.update({"x": x[b], "ctx": ctx[b], "cc": np.ascontiguousarray(cc)})
        in_maps.append(m)
    res = run_bass_kernel_spmd(nc, in_maps, core_ids=list(range(_cores)))
    out = np.stack([res.results[b]["out"] for b in range(_cores)], axis=0).astype(np.float32)
    if _dbg:
        return out, res.results
    return out
```

```python
import math
from contextlib import ExitStack

import numpy as np
import ml_dtypes
import concourse.bass as bass
import concourse.mybir as mybir
from concourse.bass_utils import run_bass_kernel_spmd

F32 = mybir.dt.float32
BF16 = mybir.dt.bfloat16
ALU = mybir.AluOpType
AF = mybir.ActivationFunctionType
AX = mybir.AxisListType

D = 1024
SEQ = 2048
CTX = 256
NT = SEQ + CTX
NB = NT // 128
DEPTH = 2
EPS = 1e-6
IN_W = 4128
OFF_MLQ, OFF_MLK, OFF_MLV, OFF_MLO, OFF_MLI, OFF_MLF = 0, 512, 1024, 1536, 2048, 2056
OFF_Z, OFF_XBC, OFF_DT, OFF_AQ, OFF_AK, OFF_AV = 2064, 2576, 3344, 3360, 3872, 4000
TILES = [(0, 256, 1)] + [(256 + 512 * i, 512, 0) for i in range(4)]
NEG = -30000.0


class Buf:
    __slots__ = ("name", "writer", "readers", "dma_readers", "excl")

    def __init__(self, name=""):
        self.excl = False
        self.name = name
        self.writer = None
        self.readers = {}
        self.dma_readers = []


class Rec:
    __slots__ = ("eng", "fn", "deps", "inc", "is_dma", "sem", "count")

    def __init__(self, eng, fn, is_dma):
        self.eng = eng
        self.fn = fn
        self.deps = []
        self.inc = False
        self.is_dma = is_dma
        self.sem = None
        self.count = 0


class Sched:
    ENGS = ("pe", "dve", "act", "pool", "sp")
    NDMA = 24

    def __init__(self, nc):
        self.nc = nc
        self.streams = {e: [] for e in self.ENGS}
        self.dma_rr = 0
        self.dma_rr2 = [0, 0]
        self.dma_last = [None] * self.NDMA
        self.dma_cnt = [0] * self.NDMA
        self.last = {e: None for e in self.ENGS}

    def op(self, eng, fn, R=(), W=(), dma=False, extra=()):
        rec = Rec(eng, fn, dma)
        deps = {}

        def add(d):
            if d is None or d is rec:
                return
            if d.eng == "pe" and eng == "pe" and not d.is_dma and not dma:
                return
            deps[id(d)] = d

        for b in R:
            add(b.writer)
            if b.excl:
                for r in b.readers.values():
                    if r.eng != eng:
                        add(r)
        for b in W:
            add(b.writer)
            for r in b.readers.values():
                add(r)
            for r in b.dma_readers:
                add(r)
        for d in extra:
            add(d)
        if dma:
            half = self.NDMA // 2
            sw = 1 if eng == "pool" else 0
            k = sw * half + self.dma_rr2[sw]
            self.dma_rr2[sw] = (self.dma_rr2[sw] + 1) % half
            add(self.dma_last[k])
            self.dma_last[k] = rec
            self.dma_cnt[k] += 16
            rec.sem = k
            rec.count = self.dma_cnt[k]
        elif fn is not None:
            self.last[eng] = rec
        rec.deps = list(deps.values())
        for d in rec.deps:
            d.inc = True
        for b in R:
            if dma:
                b.dma_readers.append(rec)
            else:
                b.readers[eng] = rec
        for b in W:
            b.writer = rec
            b.readers = {}
            b.dma_readers = []
        self.streams[eng].append(rec)
        return rec

    def fence(self):
        pend = [r for r in self.last.values() if r is not None]
        pend += [r for r in self.dma_last if r is not None]
        for e in self.ENGS:
            self.op(e, None, extra=pend)

    def emit(self, final_recs):
        nc = self.nc
        for r in final_recs:
            r.inc = True
        for e in self.ENGS:
            c = 0
            for r in self.streams[e]:
                if r.is_dma or r.fn is None:
                    continue
                if r.inc:
                    c += 1
                    r.count = c
        with ExitStack() as es:
            esem = {e: es.enter_context(nc.semaphore(f"sem_{e}")) for e in self.ENGS}
            dsem = [es.enter_context(nc.semaphore(f"dsem{k}")) for k in range(self.NDMA)]
            block = es.enter_context(nc.Block())

            def replay(e, engine, extra_final=None):
                waited = {}

                def wait(d):
                    if d.is_dma:
                        key, sem = ("d", d.sem), dsem[d.sem]
                    else:
                        key, sem = ("e", d.eng), esem[d.eng]
                    if waited.get(key, 0) >= d.count:
                        return
                    waited[key] = d.count
                    engine.wait_ge(sem, d.count)

                for r in self.streams[e]:
                    for d in r.deps:
                        wait(d)
                    if r.fn is None:
                        continue
                    ins = r.fn(engine)
                    if r.is_dma:
                        ins.then_inc(dsem[r.sem], 16)
                    elif r.inc:
                        ins.then_inc(esem[e], 1)
                for d in extra_final or ():
                    wait(d)

            @block.tensor
            def _(eng):
                replay("pe", eng)

            @block.vector
            def _(eng):
                replay("dve", eng)

            @block.scalar
            def _(eng):
                replay("act", eng)

            @block.gpsimd
            def _(eng):
                replay("pool", eng)

            @block.sync
            def _(eng):
                replay("sp", eng, extra_final=final_recs)


class Tl:
    def __init__(self, t, nb, name):
        self.t = t
        self.b = [Buf(f"{name}.{i}") for i in range(nb)]

    def __getitem__(self, idx):
        return self.t[idx]


class Region:
    def alias(self, name, shape, dtype, off, bufs):
        self.n += 1
        t = self.nc.alloc_sbuf_tensor_at(f"{self.name}_{name}_{self.n}", list(shape), dtype, offset=self.base + off)
        tl = Tl(t, 0, name)
        tl.b = bufs
        return tl

    def __init__(self, nc, base, size, name):
        self.nc, self.base, self.size, self.cur, self.name, self.n = nc, base, size, 0, name, 0

    def reset(self):
        self.cur = 0

    def alloc(self, name, shape, dtype, nb=1):
        nbytes = int(np.prod(shape[1:])) * (4 if dtype == F32 else 2)
        nbytes = (nbytes + 31) // 32 * 32
        assert self.cur + nbytes <= self.size, f"region {self.name} overflow at {name}: {self.cur}+{nbytes}>{self.size}"
        self.n += 1
        t = self.nc.alloc_sbuf_tensor_at(f"{self.name}_{name}_{self.n}", list(shape), dtype, offset=self.base + self.cur)
        self.cur += nbytes
        return Tl(t, nb, name)


class _Stop(Exception):
    pass


def build(nc, depth=DEPTH, dbg=(), stop_after=None):
    S = Sched(nc)
    dbg_out = {}

    def din(name, shape, dt=F32):
        return nc.dram_tensor(name, list(shape), dt, kind="ExternalInput").ap()

    x_d = din("x", [SEQ, D])
    ctx_d = din("ctx", [CTX, D])
    cc_d = din("cc", [128, 16])
    wmod_d = din("w_mod", [depth, D, 6 * D])
    bmod_d = din("b_mod", [depth, 128, 48])
    g1_d = din("norm1_g", [depth, 128, 8])
    g2_d = din("norm2_g", [depth, 128, 8])
    win_d = din("w_in", [depth, D, IN_W])
    wout_d = din("w_out", [depth, 1536, D])
    wup_d = din("w_up", [depth, D, 4 * D])
    wdn_d = din("w_down", [depth, 4 * D, D])
    rowv_d = din("rowv", [depth, 1, 1280])
    convp_d = din("convp", [depth, 128, 24])
    cst_d = din("cst", [128, 128 * 6])
    m01_d = din("m01", [128, 1024])
    rope_d = din("rope", [128, 2 * 16 * 32])
    out_d = nc.dram_tensor("out", [SEQ, D], F32, kind="ExternalOutput").ap()
    xs_d = nc.dram_tensor("xs_scratch", [128, 8 * NT], F32, kind="Internal").ap()

    base0 = 16512
    slab = nc.alloc_sbuf_tensor("slab", [128, (229344 - base0) // 4 - 8], F32)
    sizes = dict(C=16384, X=73728, H=36864, Y=55296)
    off = base0
    RC = Region(nc, off, sizes["C"], "C"); off += sizes["C"]
    RX = Region(nc, off, sizes["X"], "X"); off += sizes["X"]
    RH = Region(nc, off, sizes["H"], "H"); off += sizes["H"]
    RY = Region(nc, off, sizes["Y"], "Y"); off += sizes["Y"]
    RW = Region(nc, off, 229344 - off - 64, "W")

    ps32 = [Tl(nc.alloc_psum_tensor(f"ps{i}", [128, 512], F32), 1, f"ps{i}") for i in range(6)]
    psb = [Tl(nc.alloc_psum_tensor(f"psb{i}", [128, 1024], BF16), 1, f"psb{i}") for i in range(2)]
    for _t in ps32 + psb:
        _t.b[0].excl = True
    pctr = [0, 0, 0]

    def P32():
        pctr[0] += 1
        return ps32[pctr[0] % 4]

    def PD():
        pctr[2] += 1
        return ps32[4 + pctr[2] % 2]

    def PB():
        pctr[1] += 1
        return psb[pctr[1] % 2]

    def MM(out, lhsT, rhs, start, stop, R, W):
        S.op("pe", lambda e: e.matmul(out, lhsT=lhsT, rhs=rhs, start=start, stop=stop), R, W)

    def TR(out, in_, ident, R, W):
        S.op("pe", lambda e: e.transpose(out=out, in_=in_, identity=ident), R, W)

    def ACT(out, in_, func, R, W, bias=None, scale=None):
        kw = {}
        if bias is not None:
            kw["bias"] = bias
        if scale is not None:
            kw["scale"] = scale
        S.op("act", lambda e: e.activation(out=out, in_=in_, func=func, **kw), R, W)

    import os as _os
    _nopool = _os.environ.get("KPOOL") != "1"

    def _pe(eng):
        return "dve" if (_nopool and eng == "pool") else eng

    def TT(eng, out, in0, in1, op, R, W):
        eng = _pe(eng)
        S.op(eng, lambda e: e.tensor_tensor(out=out, in0=in0, in1=in1, op=op), R, W)

    def TS(eng, out, in0, s1, op0, R, W, s2=None, op1=None):
        eng = _pe(eng)
        if op1 is None:
            S.op(eng, lambda e: e.tensor_scalar(out=out, in0=in0, scalar1=s1, scalar2=None, op0=op0), R, W)
        else:
            S.op(eng, lambda e: e.tensor_scalar(out=out, in0=in0, scalar1=s1, scalar2=s2, op0=op0, op1=op1), R, W)

    def STT(out, in0, scalar, in1, op0, op1, R, W):
        S.op("dve", lambda e: e.scalar_tensor_tensor(out=out, in0=in0, scalar=scalar, in1=in1, op0=op0, op1=op1), R, W)

    def CP(eng, out, in_, R, W):
        eng = _pe(eng)
        if eng == "act":
            S.op("act", lambda e: e.copy(out=out, in_=in_), R, W)
        else:
            S.op(eng, lambda e: e.tensor_copy(out=out, in_=in_), R, W)

    def RED(out, in_, op, R, W):
        S.op("dve", lambda e: e.tensor_reduce(out=out, in_=in_, axis=AX.X, op=op), R, W)

    def RECIP(out, in_, R, W):
        S.op("dve", lambda e: e.reciprocal(out=out, in_=in_), R, W)

    def MSET(eng, ap, val, W):
        eng = _pe(eng)
        S.op(eng, lambda e: e.memset(ap, val), (), W)

    def DMA(eng, out, in_, R, W, slow=False):
        if slow:
            return S.op(eng, lambda e: e.dma_start(out=out, in_=in_, allow_slow_non_contiguous=True), R, W, dma=True)
        return S.op(eng, lambda e: e.dma_start(out=out, in_=in_), R, W, dma=True)

    def dump(name, tl, shape, dt=F32):
        if name in dbg and name not in dbg_out:
            d = nc.dram_tensor("dbg_" + name, list(shape), dt, kind="ExternalOutput").ap()
            dbg_out[name] = DMA("sp", d, tl.t[:], tl.b, ())

    def rsqrt_chain(out, in_, add, R, W):
        ACT(out, in_, AF.Ln, R, W, bias=add, scale=1.0)
        ACT(out, out, AF.Exp, W, W, scale=-0.5)

    cst = RC.alloc("cst", [128, 768], F32)
    DMA("sp", cst[:], cst_d, (), cst.b)
    ident_f, triF, triR = cst[:, 0:128], cst[:, 128:256], cst[:, 256:384]
    mneg = [cst[:, 384:512], cst[:, 512:640]]
    ones_f = cst[:, 640:768]
    cstb = RC.alloc("cstb", [128, 256], BF16)
    DMA("pool", cstb[:, 0:128], cst_d[:, 0:128], (), cstb.b)
    DMA("pool", cstb[:, 128:256], cst_d[:, 640:768], (), cstb.b)
    ident_b, ones_b = cstb[:, 0:128], cstb[:, 128:256]
    m01 = RC.alloc("m01", [128, 1024], BF16)
    DMA("pool", m01[:], m01_d, (), m01.b)
    rope = RC.alloc("rope", [128, 1024], F32)
    DMA("sp", rope[:], rope_d, (), rope.b)
    rcos = rope[:, 0:512].rearrange("p (b h i) -> p b h i", b=16, h=2)
    rsin = rope[:, 512:1024].rearrange("p (b h i) -> p b h i", b=16, h=2)
    CB = cst.b + cstb.b

    modp = [RC.alloc(f"modp{l}", [128, 48, 2], F32) for l in range(depth)]
    rowv = RC.alloc("rowv", [128, 1280], F32)
    convp = RC.alloc("convp", [128, 24], F32)

    cc = RC.alloc("cc", [128, 16], F32)
    DMA("sp", cc[:], cc_d, (), cc.b)
    s2b = RC.alloc("s2b", [128, 8, 2], BF16)
    ACT(s2b[:].rearrange("p k j -> p (k j)"), cc[:], AF.Silu, cc.b, s2b.b)
    mnegb = RC.alloc("mnegb", [128, 256], BF16)
    DMA("pool", mnegb[:], cst_d[:, 384:640], (), mnegb.b)
    CB = CB + mnegb.b

    def mod_gen(l, wm, bm, g12):
        DMA("sp", bm[:], bmod_d[l], (), bm.b)
        DMA("sp", g12[:, 0:8], g1_d[l], (), g12.b)
        DMA("sp", g12[:, 8:16], g2_d[l], (), g12.b)
        mp = modp[l]
        def ld(fg):
            w_ = wm[fg % len(wm)]
            DMA("pool", w_[:], wmod_d[l].rearrange("(k p) f -> p k f", p=128)[:, :, fg * 512:(fg + 1) * 512], (), w_.b)

        ld(0)
        for fg in range(12):
            w = wm[fg % len(wm)]
            if fg + 1 < 12 and len(wm) > 1:
                ld(fg + 1)
            elif fg > 0 and len(wm) == 1:
                ld(fg)
            ps = P32()
            for j in range(4):
                for k in range(8):
                    MM(ps[:, 2 * j:2 * j + 2], w[:, k, j * 128:(j + 1) * 128], s2b[:, k, :], k == 0, k == 7,
                       w.b + s2b.b, ps.b)
            TT("dve", mp[:, fg * 4:(fg + 1) * 4, :], ps[:, 0:8].rearrange("p (a j) -> p a j", j=2),
               bm[:, fg * 4:(fg + 1) * 4].unsqueeze(2).to_broadcast([128, 4, 2]), ALU.add, ps.b + bm.b, mp.b)
            yield
        for (so, go) in ((8, 0), (32, 8)):
            TS("dve", mp[:, so:so + 8, :], mp[:, so:so + 8, :], 1.0, ALU.add, mp.b, mp.b, s2=32.0, op1=ALU.mult)
            TT("dve", mp[:, so:so + 8, :], mp[:, so:so + 8, :],
               g12[:, go:go + 8].unsqueeze(2).to_broadcast([128, 8, 2]), ALU.mult, mp.b + g12.b, mp.b)
        yield

    bm0 = RW.alloc("bm", [128, 48], F32)
    g120 = RW.alloc("g12", [128, 16], F32)
    wm0 = [RW.alloc(f"wm{i}", [128, 8, 512], BF16) for i in range(2)]
    xT = RX.alloc("xT", [128, 8, NT], F32, nb=1)
    xin = [RW.alloc(f"xin{i}", [128, D], F32) for i in range(3)]
    def xload_gen():
      for tb in range(NB):
          xi = xin[tb % 3]
          src = ctx_d[tb * 128:(tb + 1) * 128, :] if tb < 2 else x_d[(tb - 2) * 128:(tb - 1) * 128, :]
          DMA("sp", xi[:], src, (), xi.b)
          for hf in range(2):
              ps = P32()
              for j in range(4):
                  k = hf * 4 + j
                  TR(ps[:, j * 128:(j + 1) * 128], xi[:, k * 128:(k + 1) * 128], ident_f, xi.b + CB, ps.b)
              CP("act" if hf else "dve", xT[:, hf * 4:(hf + 1) * 4, tb * 128:(tb + 1) * 128],
                 ps[:, :].rearrange("p (a t) -> p a t", a=4), ps.b, xT.b)
          yield

    g_a, g_b = xload_gen(), mod_gen(0, wm0, bm0, g120)
    live = [g_a, g_b]
    while live:
        for g_ in list(live):
            try:
                next(g_)
            except StopIteration:
                live.remove(g_)
    S.fence()
    RW.reset()

    hT = RH.alloc("hT", [128, 8, NT], BF16, nb=len(TILES))
    yT = RY.alloc("yT", [128, 12, NT], BF16, nb=1)

    def norm_to_h(l, a_off, b_off, tiles):
        mp = modp[l]
        sq = RW.alloc("nsq", [128, 8, 512], BF16)
        rs = RW.alloc("nrs", [128, 512], F32)
        tmp = [RW.alloc(f"ntmp{i}", [128, 512], F32) for i in range(2)]
        for ti, (s0, n, j) in enumerate(TILES):
            if ti not in tiles:
                continue
            ACT(sq[:, :, 0:n], xT[:, :, s0:s0 + n], AF.Square, xT.b, sq.b)
            ps = P32()
            for k in range(8):
                MM(ps[:, 0:n], ones_b, sq[:, k, 0:n], k == 0, k == 7, sq.b + CB, ps.b)
            rsqrt_chain(rs[:, 0:n], ps[:, 0:n], float(D * EPS), ps.b, rs.b)
            for k in range(8):
                t = tmp[k % 2]
                STT(t[:, 0:n], xT[:, k, s0:s0 + n], mp[:, a_off + k, j:j + 1], rs[:, 0:n], ALU.mult, ALU.mult,
                    xT.b + mp.b + rs.b, t.b)
                ACT(hT[:, k, s0:s0 + n], t[:, 0:n], AF.Identity, t.b + mp.b, [hT.b[ti]],
                    bias=mp[:, b_off + k, j:j + 1], scale=1.0)

    win_v = [win_d[l].rearrange("(k p) f -> p k f", p=128) for l in range(depth)]

    def load_w(tl, l, col0, ncols, dst0=0):
        DMA("pool", tl[:, :, dst0:dst0 + ncols], win_v[l][:, :, col0:col0 + ncols], (), tl.b)

    def proj_tok(w, c0, ncols, tb, ti, ps=None):
        ps = ps or P32()
        for k in range(8):
            MM(ps[:, 0:ncols], hT[:, k, tb * 128:(tb + 1) * 128], w[:, k, c0:c0 + ncols], k == 0, k == 7,
               w.b + [hT.b[ti]], ps.b)
        return ps

    def proj_feat(w, c0, ti, ps=None):
        s0, n, j = TILES[ti]
        ps = ps or P32()
        for k in range(8):
            MM(ps[:, 0:n], w[:, k, c0:c0 + 128], hT[:, k, s0:s0 + n], k == 0, k == 7, w.b + [hT.b[ti]], ps.b)
        return ps

    def tile_of(tb):
        return 0 if tb < 2 else 1 + (tb - 2) // 4

    def transpose_to_yT(src, chunk, blocks):
        blocks = list(blocks)
        for i in range(0, len(blocks), 4):
            grp = blocks[i:i + 4]
            pb = PB()
            for jj, tb in enumerate(grp):
                TR(pb[:, jj * 128:(jj + 1) * 128], src[:, tb, :], ident_b, src.b + CB, pb.b)
            if grp[-1] - grp[0] == len(grp) - 1:
                CP("act", yT[:, chunk, grp[0] * 128:(grp[-1] + 1) * 128], pb[:, 0:len(grp) * 128], pb.b, yT.b)
            else:
                for jj, tb in enumerate(grp):
                    CP("act", yT[:, chunk, tb * 128:(tb + 1) * 128], pb[:, jj * 128:(jj + 1) * 128], pb.b, yT.b)

    def layer(l):
        nonlocal xT, yT
        last = l == depth - 1
        need_ctx = not last
        out_blocks = list(range(NB)) if need_ctx else list(range(2, NB))
        tiles_all = list(range(len(TILES)))
        mp = modp[l]
        DMA("sp", rowv[:], rowv_d[l].partition_broadcast(128), (), rowv.b)
        DMA("sp", convp[:], convp_d[l], (), convp.b)
        rv = lambda a, n: rowv[:, a:a + n]
        RV_BI, RV_BF, RV_MLG, RV_ALOG, RV_DTB, RV_DSK, RV_SSDG, RV_QG, RV_KG, RV_SINK = 0, 8, 16, 528, 544, 560, 568, 1080, 1144, 1208

        norm_to_h(l, 8, 0, tiles_all)
        if stop_after == 'norm1' and l == 0:
            raise _Stop()
        DMA("sp", xs_d, xT[:].rearrange("p k t -> p (k t)"), xT.b, ())
        S.fence()
        RW.reset()
        RX.reset()

        wg = RW.alloc("wg", [128, 8, 32], BF16)
        load_w(wg, l, OFF_MLI, 16, 0)
        load_w(wg, l, OFF_DT, 16, 16)
        GW = 24
        wt = RX.alloc("wt", [128, NB, GW], F32)
        wsp = RX.alloc("wsp", [128, NB, GW], F32)
        cbias = RX.alloc("cbias", [128, NB, GW], F32)
        dec = RX.alloc("dec", [128, NB, GW], F32)
        GTt = {k: RW.alloc(f"GT{k}", [72, 128], F32) for k in range(6)}
        GTh = {k: RX.alloc(f"GTh{k}", [72, 128], BF16) for k in range(6)}
        GTl = {k: RX.alloc(f"GTl{k}", [72, 128], BF16) for k in range(6)}
        gate_mark = RX.cur
        graw = RW.alloc("graw", [128, NB, 32], F32)
        for tb in range(NB):
            ps = proj_tok(wg, 0, 32, tb, tile_of(tb))
            CP("dve", graw[:, tb, :], ps[:, 0:32], ps.b, graw.b)
        lnb = RW.alloc("lnb", [128, NB, GW], F32)
        ldec = RW.alloc("ldec", [128, NB, GW], F32)
        fcs = RW.alloc("fcs", [128, NB, GW], F32)
        tot = RW.alloc("tot", [128, NB, GW], F32)
        gtmp = RW.alloc("gtmp", [128, NB, 16], F32)
        gtmp2 = RW.alloc("gtmp2", [128, NB, 16], F32)

        def bc(a, n):
            return rv(a, n).unsqueeze(1).to_broadcast([128, NB, n])

        TT("dve", lnb[:, :, 0:8], graw[:, :, 0:8], bc(RV_BI, 8), ALU.add, graw.b + rowv.b, lnb.b)
        TT("dve", gtmp[:, :, 0:8], graw[:, :, 8:16], bc(RV_BF, 8), ALU.add, graw.b + rowv.b, gtmp.b)
        ACT(gtmp[:, :, 0:8], gtmp[:, :, 0:8], AF.Exp, gtmp.b, gtmp.b, scale=-1.0)
        ACT(gtmp[:, :, 0:8], gtmp[:, :, 0:8], AF.Ln, gtmp.b, gtmp.b, bias=1.0, scale=1.0)
        TS("dve", ldec[:, :, 0:8], gtmp[:, :, 0:8], -1.0, ALU.mult, gtmp.b, ldec.b)
        TT("dve", gtmp[:], graw[:, :, 16:32], bc(RV_DTB, 16), ALU.add, graw.b + rowv.b, gtmp.b)
        STT(gtmp2[:], gtmp[:], -1.0, gtmp[:], ALU.mult, ALU.max, gtmp.b, gtmp2.b)
        ACT(gtmp2[:], gtmp2[:], AF.Exp, gtmp2.b, gtmp2.b, scale=-1.0)
        ACT(gtmp2[:], gtmp2[:], AF.Ln, gtmp2.b, gtmp2.b, bias=1.0, scale=1.0)
        STT(gtmp[:], gtmp[:], 0.0, gtmp2[:], ALU.max, ALU.add, gtmp.b + gtmp2.b, gtmp.b)
        ACT(lnb[:, :, 8:24], gtmp[:], AF.Ln, gtmp.b, lnb.b)
        aexp = RW.alloc("aexp", [128, 16], F32)
        ACT(aexp[:], rv(RV_ALOG, 16), AF.Exp, rowv.b, aexp.b)
        TT("dve", gtmp2[:], gtmp[:], aexp[:].unsqueeze(1).to_broadcast([128, NB, 16]), ALU.mult, gtmp.b + aexp.b, gtmp2.b)
        TS("dve", ldec[:, :, 8:24], gtmp2[:], -1.0, ALU.mult, gtmp2.b, ldec.b)
        colsets = [(0, 4, 0), (4, 8, 1), (8, 16, 0), (16, 24, 1)]
        for (c0, c1, d) in colsets:
            ps = P32()
            nn = (c1 - c0) * NB
            MM(ps[:, 0:nn].rearrange("p (b c) -> p b c", b=NB), triR if d else triF, ldec[:, :, c0:c1], True, True, ldec.b + CB, ps.b)
            CP("dve", fcs[:, :, c0:c1], ps[:, 0:nn].rearrange("p (b c) -> p b c", b=NB), ps.b, fcs.b)
        ps = P32()
        MM(ps[:, 0:NB * GW], ones_f, ldec[:].rearrange("p b c -> p (b c)"), True, True, ldec.b + CB, ps.b)
        CP("dve", tot[:].rearrange("p b c -> p (b c)"), ps[:, 0:NB * GW], ps.b, tot.b)
        ACT(wt[:], fcs[:], AF.Exp, fcs.b, wt.b)
        ACT(dec[:], tot[:], AF.Exp, tot.b, dec.b)
        TT("dve", cbias[:], lnb[:], fcs[:], ALU.subtract, lnb.b + fcs.b, cbias.b)
        TT("dve", wsp[:], cbias[:], tot[:], ALU.add, cbias.b + tot.b, wsp.b)
        ACT(wsp[:], wsp[:], AF.Exp, wsp.b, wsp.b)
        fd = RW.alloc("fd", [128, NB, 4], F32)
        GT = {}
        gsets = [("m", 0, 0, 0), ("m", 1, 0, 4), ("s", 0, 0, 8), ("s", 0, 1, 12), ("s", 1, 0, 16), ("s", 1, 1, 20)]
        for gi, (fam, d, g, c0) in enumerate(gsets):
            CP("dve", fd[:], fcs[:, :, c0:c0 + 4], fcs.b, fd.b)
            ps = P32()
            TR(ps[0:72, 0:128], fd[:].rearrange("p b c -> p (b c)"), ident_f, fd.b + CB, ps.b)
            gt = GTt[gi]
            CP("dve", gt[:], ps[0:72, 0:128], ps.b, gt.b)
            CP("act", GTh[gi][:], gt[:], gt.b, GTh[gi].b)
            TT("dve", GTl[gi][:], gt[:], GTh[gi][:], ALU.subtract, gt.b + GTh[gi].b, GTl[gi].b)
            GT[(fam, d, g)] = (GTh[gi], GTl[gi])
        dump("cbias", cbias, [128, NB, GW]); dump("wt", wt, [128, NB, GW]); dump("wsp", wsp, [128, NB, GW])
        S.fence()
        RW.reset()

        def scan_order(d):
            return list(range(NB)) if d == 0 else [1, 0] + list(range(NB - 1, 1, -1))

        def dt_tile(gt, r, d, tb, col, dst, ps=None, c0=0):
            ps = ps or PD()
            gh, gl = gt
            sel = ident_b[0:72, r:r + 1].to_broadcast([72, 128])
            MM(ps[:, c0:c0 + 128], sel, gh[:, :], True, False, gh.b + CB, ps.b)
            MM(ps[:, c0:c0 + 128], sel, gl[:, :], False, False, gl.b + CB, ps.b)
            MM(ps[:, c0:c0 + 128], ident_b, mnegb[:, d * 128:(d + 1) * 128], False, True, CB, ps.b)
            ACT(dst[:, :], ps[:, c0:c0 + 128], AF.Exp, ps.b + cbias.b, dst.b, bias=cbias[:, tb, col:col + 1], scale=1.0)

        if stop_after == 'gates' and l == 0:
            raise _Stop()
        wq = [RW.alloc(f"wq{i}", [128, 8, 512], BF16) for i in range(2)]
        hbuf = []
        for si in range(2):
            if si == 0:
                al = lambda nm, shp, dt: RX.alloc(nm, shp, dt)
            else:
                yoff = [4 * NT * 2]

                def al(nm, shp, dt):
                    nbytes = (int(np.prod(shp[1:])) * (4 if dt == F32 else 2) + 31) // 32 * 32
                    t_ = RY.alias(nm, shp, dt, yoff[0], [Buf(nm)])
                    yoff[0] += nbytes
                    assert yoff[0] <= 12 * NT * 2
                    return t_
            hbuf.append([al(f"qT{si}", [128, NT], BF16), al(f"kT{si}", [128, NT], BF16),
                         al(f"ktok{si}", [128, NB, 128], BF16), al(f"vaug{si}", [128, NB, 130], BF16),
                         al(f"osig{si}", [128, NB, 128], F32)])
        hacc = RX.alloc("hacc", [128, NB, 128], F32, nb=NB)
        C32c = [RX.alloc(f"C32{i}", [128, 132], F32) for i in range(2)]
        Cbc = [[RX.alloc(f"Cb{i}{j}", [128, 132], BF16) for j in range(2)] for i in range(2)]
        DTs = [RX.alloc(f"DTs{i}", [128, 128], F32) for i in range(2)]
        PTs = [RX.alloc(f"PTs{i}", [128, 128], BF16) for i in range(2)]
        kws = [RX.alloc(f"kws{i}", [128, 128], BF16) for i in range(2)]
        tA = [RX.alloc(f"tA{i}", [128, 132], F32) for i in range(2)]
        tB = [RX.alloc(f"tB{i}", [128, 132], F32) for i in range(2)]
        dn = [RX.alloc(f"dn{i}", [128, 2], F32) for i in range(2)]
        mout = RX.alloc("mout", [128, NB, 128], BF16)
        ssq = RX.alloc("ssq", [128, NB], F32)
        sqt = RX.alloc("sqt", [128, NB, 128], F32)
        for si in range(2):
            MSET("dve", hbuf[si][3][:, :, 128:130], 1.0, hbuf[si][3].b)
        kscale = 128.0 ** -0.5
        orders = [scan_order(0), scan_order(1)]
        hset = set()

        def ml_proj(h):
            qT, kT, ktok, vaug, osig = hbuf[h % 2]
            w = wq[h % 2]
            for gq in range(4):
                load_w(w, l, gq * 512 + h * 128, 128, gq * 128)
            pc = 0
            for ti in tiles_all:
                s0, n, j = TILES[ti]
                ps = proj_feat(w, 0, ti, ps=ps32[4 + pc % 2]); pc += 1
                CP("act", qT[:, s0:s0 + n], ps[:, 0:n], ps.b, qT.b)
                yield
                ps = proj_feat(w, 128, ti, ps=ps32[4 + pc % 2]); pc += 1
                ACT(kT[:, s0:s0 + n], ps[:, 0:n], AF.Copy, ps.b, kT.b, scale=kscale)
                yield
            for tb in range(NB):
                ps = proj_tok(w, 128, 384, tb, tile_of(tb), ps=ps32[4 + pc % 2]); pc += 1
                ACT(ktok[:, tb, :], ps[:, 0:128], AF.Copy, ps.b, ktok.b, scale=kscale)
                CP("dve", vaug[:, tb, 0:128], ps[:, 128:256], ps.b, vaug.b)
                ACT(osig[:, tb, :], ps[:, 256:384], AF.Sigmoid, ps.b, osig.b)
                yield

        def ml_chain(d, h, hb_):
            for it in range(NB):
                tb = orders[d][it]
                gt = GT[("m", d, 0)]
                col = d * 4 + h
                blk = slice(tb * 128, (tb + 1) * 128)
                want = tb in out_blocks
                bank1, bank2 = ps32[2 * d], ps32[2 * d + 1]
                qT, kT, ktok, vaug = hb_[0], hb_[1], hb_[2], hb_[3]
                S_, B_ = bank1[:, 0:128], bank1[:, 128:258]
                A_, C_ = bank2[:, 0:130], bank2[:, 130:260]
                cb_cur, cb_nxt = Cbc[d][it % 2], Cbc[d][(it + 1) % 2]
                if want:
                    MM(S_, kT[:, blk], qT[:, blk], True, True, kT.b + qT.b, bank1.b)
                    yield
                    MM(B_, qT[:, blk], cb_cur[:, 0:130], True, True, qT.b + cb_cur.b, bank1.b)
                    yield
                if it < NB - 1:
                    ACT(kws[d][:], ktok[:, tb, :], AF.Identity, ktok.b + wsp.b, kws[d].b, scale=wsp[:, tb, col:col + 1])
                    yield
                    MM(C_, kws[d][:], vaug[:, tb, 0:130], True, True, kws[d].b + vaug.b, bank2.b)
                    yield
                    STT(C32c[d][:, 0:130], C32c[d][:, 0:130], dec[:, tb, col:col + 1], C_, ALU.mult, ALU.add,
                        C32c[d].b + dec.b + bank2.b, C32c[d].b)
                    yield
                    CP("act", cb_nxt[:, 0:130], C32c[d][:, 0:130], C32c[d].b, cb_nxt.b)
                    yield
                if want:
                    dt_tile(gt, tb * 4 + h, d, tb, col, DTs[d], ps=bank1, c0=260)
                    yield
                    TT("dve", PTs[d][:], S_, DTs[d][:], ALU.mult, bank1.b + DTs[d].b, PTs[d].b)
                    yield
                    MM(A_, PTs[d][:], vaug[:, tb, 0:130], True, True, PTs[d].b + vaug.b, bank2.b)
                    yield
                    ACT(tB[d][:, 0:130], B_, AF.Identity, bank1.b + wt.b, tB[d].b, scale=wt[:, tb, col:col + 1])
                    yield
                    TT("dve", tA[d][:, 0:130], A_, tB[d][:, 0:130], ALU.add, bank2.b + tB[d].b, tA[d].b)
                    yield
                    TS("dve", dn[d][:, 0:1], tA[d][:, 128:129], 1.0, ALU.max, tA[d].b, dn[d].b)
                    yield
                    STT(dn[d][:, 0:1], tA[d][:, 128:129], -1.0, dn[d][:, 0:1], ALU.mult, ALU.max, tA[d].b + dn[d].b, dn[d].b)
                    yield
                    RECIP(dn[d][:, 1:2], dn[d][:, 0:1], dn[d].b, dn[d].b)
                    yield
                    hb = [hacc.b[tb]]
                    if tb not in hset:
                        hset.add(tb)
                        TS("dve", hacc[:, tb, :], tA[d][:, 0:128], dn[d][:, 1:2], ALU.mult, tA[d].b + dn[d].b, hb)
                        yield
                    else:
                        STT(hacc[:, tb, :], tA[d][:, 0:128], dn[d][:, 1:2], hacc[:, tb, :], ALU.mult, ALU.add,
                            tA[d].b + dn[d].b + hb, hb)
                        yield


        def run_rr(gens):
            gens = list(gens)
            while gens:
                for g_ in list(gens):
                    try:
                        next(g_)
                    except StopIteration:
                        gens.remove(g_)

        run_rr([ml_proj(0)])
        for h in range(4):
            osig = hbuf[h % 2][4]
            for d in range(2):
                MSET("dve", C32c[d][:], 0.0, C32c[d].b)
                MSET("dve", Cbc[d][0][:], 0.0, Cbc[d][0].b)
            hset.clear()
            gl_ = [ml_chain(0, h, hbuf[h % 2]), ml_chain(1, h, hbuf[h % 2])]
            if h + 1 < 4:
                gl_.append(ml_proj(h + 1))
            run_rr(gl_)
            b0 = out_blocks[0]
            nbk = len(out_blocks)
            hv = hacc[:, b0:NB, :]
            TT("dve", osig[:, b0:NB, :], osig[:, b0:NB, :],
               rv(RV_MLG + h * 128, 128).unsqueeze(1).to_broadcast([128, nbk, 128]), ALU.mult, osig.b + rowv.b, osig.b)
            TT("dve", sqt[:, b0:NB, :], hv, hv, ALU.mult, hacc.b, sqt.b)
            RED(ssq[:, b0:NB], sqt[:, b0:NB, :], ALU.add, sqt.b, ssq.b)
            ACT(ssq[:, b0:NB], ssq[:, b0:NB], AF.Ln, ssq.b, ssq.b, bias=EPS, scale=1.0 / 128)
            ACT(ssq[:, b0:NB], ssq[:, b0:NB], AF.Exp, ssq.b, ssq.b, scale=-0.5)
            TT("dve", hv, hv, ssq[:, b0:NB].unsqueeze(2).to_broadcast([128, nbk, 128]), ALU.mult, hacc.b + ssq.b, hacc.b)
            TT("dve", mout[:, b0:NB, :], hv, osig[:, b0:NB, :], ALU.mult, hacc.b + osig.b, mout.b)
            transpose_to_yT(mout, h, out_blocks)
        S.fence()
        RX.cur = gate_mark
        RW.reset()

        if stop_after == 'mlstm' and l == 0:
            raise _Stop()
        wz = [RW.alloc(f"wz{i}", [128, 8, 256], BF16) for i in range(2)]
        wx = [RW.alloc(f"wx{i}", [128, 8, 256], BF16) for i in range(2)]
        wbc = RW.alloc("wbc", [128, 8, 256], BF16)
        DTs = [RW.alloc(f"DTs{i}", [128, 128], F32) for i in range(8)]
        PTs = [RW.alloc(f"PTs{i}", [128, 128], BF16) for i in range(8)]
        BT = RX.alloc("BT", [128, NT], BF16)
        CT = RX.alloc("CT", [128, NT], BF16)
        Btok = RX.alloc("Btok", [128, NB, 128], BF16)
        raw0_off = RX.cur
        raw = [RX.alloc(f"raw{i}", [128, NT], F32) for i in range(2)]
        raw[1].b = raw[0].b
        zs = RX.alias("zs", [128, NB, 256], BF16, raw0_off, raw[0].b)
        gg = RX.alias("gg", [128, 512], F32, raw0_off + 9216, raw[0].b)
        sout = RX.alias("sout", [128, NB, 128], BF16, raw0_off + 9216 + 2048, raw[0].b)
        xcT = RX.alloc("xcT", [128, NT], BF16)
        yacc = RX.alloc("yacc", [128, NB, 256], F32, nb=NB)
        H32c = [RX.alloc(f"H32{i}", [128, 256], F32) for i in range(2)]
        Hbc = [[RX.alloc(f"Hb{i}{j}", [128, 256], BF16) for j in range(2)] for i in range(2)]
        BwT = [RX.alloc(f"BwT{i}", [128, 128], BF16) for i in range(8)]
        ytmp = [RX.alloc(f"ytmp{i}", [128, 256], F32) for i in range(2)]
        ssq2 = RX.alloc("ssq2", [128, NB, 2], F32)
        rstd = RX.alloc("rstd", [128, NB], F32)
        G = RY.alias("G", [128, NB, 512], BF16, 8 * NT * 2, yT.b)
        xtok = RY.alias("xtok", [128, NB, 256], BF16, 4 * NT * 2, yT.b)

        def conv_silu(ps_src_fn, chunk, dst):
            r = raw[chunk % 2]
            for ti in tiles_all:
                s0, n, j = TILES[ti]
                ps = ps_src_fn(ti)
                CP("dve", r[:, s0:s0 + n], ps[:, 0:n], ps.b, r.b)
            acc = raw[(chunk + 1) % 2]
            cw = lambda i: convp[:, chunk * 4 + i:chunk * 4 + i + 1]
            ACT(acc[:, :], r[:, :], AF.Identity, r.b + convp.b, acc.b, bias=cw(3), scale=cw(1))
            for (a, b) in ((0, CTX), (CTX, NT)):
                STT(acc[:, a + 1:b], r[:, a:b - 1], cw(0), acc[:, a + 1:b], ALU.mult, ALU.add, r.b + acc.b + convp.b, acc.b)
                STT(acc[:, a:b - 1], r[:, a + 1:b], cw(2), acc[:, a:b - 1], ALU.mult, ALU.add, r.b + acc.b + convp.b, acc.b)
            ACT(dst[:, :], acc[:, :], AF.Silu, acc.b, dst.b)

        load_w(wbc, l, OFF_XBC + 512, 256)
        conv_silu(lambda ti: proj_feat(wbc, 0, ti), 4, BT)
        conv_silu(lambda ti: proj_feat(wbc, 128, ti), 5, CT)
        for i in range(0, NB, 4):
            pb = PB()
            for jj in range(4):
                tb = i + jj
                if tb < NB:
                    TR(pb[:, jj * 128:(jj + 1) * 128], BT[:, tb * 128:(tb + 1) * 128], ident_b, BT.b + CB, pb.b)
            nn = min(4, NB - i)
            CP("act", Btok[:, i:i + nn, :], pb[:, 0:nn * 128].rearrange("p (a c) -> p a c", a=nn), pb.b, Btok.b)
        for i in range(8):
            MSET("pool", BwT[i][:], 0.0, BwT[i].b)
        for g in range(2):
            load_w(wz[g], l, OFF_Z + g * 256, 256)
            load_w(wx[g], l, OFF_XBC + g * 256, 256)
            for cj in range(2):
                conv_silu(lambda ti, cj=cj: proj_feat(wx[g], cj * 128, ti), g * 2 + cj, xcT)
                for i in range(0, NB, 4):
                    pb = PB()
                    nn = min(4, NB - i)
                    for jj in range(nn):
                        tb = i + jj
                        TR(pb[:, jj * 128:(jj + 1) * 128], xcT[:, tb * 128:(tb + 1) * 128], ident_b, xcT.b + CB, pb.b)
                    CP("act", xtok[:, i:i + nn, cj * 128:(cj + 1) * 128],
                       pb[:, 0:nn * 128].rearrange("p (a c) -> p a c", a=nn), pb.b, xtok.b)
            for tb in range(NB):
                ps = proj_tok(wz[g], 0, 256, tb, tile_of(tb))
                ACT(zs[:, tb, :], ps[:, 0:256], AF.Silu, ps.b, zs.b)
            rows = slice(g * 64, (g + 1) * 64)
            orders = [scan_order(0), scan_order(1)]
            for d in range(2):
                MSET("dve", H32c[d][:], 0.0, H32c[d].b)
                MSET("pool", Hbc[d][0][:], 0.0, Hbc[d][0].b)
            yset = set()

            def ssd_chain(d):
                for it in range(NB):
                    tb = orders[d][it]
                    gt = GT[("s", d, g)]
                    c0 = 8 + d * 8 + g * 4
                    blk = slice(tb * 128, (tb + 1) * 128)
                    want = tb in out_blocks
                    lastit = it == NB - 1
                    bank1, bank2, bankD = ps32[2 * d], ps32[2 * d + 1], ps32[4 + d]
                    S_, B_ = bank1[:, 0:128], bank1[:, 128:384]
                    A_, C_ = bank2[:, 0:256], bank2[:, 256:512]
                    hb_cur, hb_nxt = Hbc[d][it % 2], Hbc[d][(it + 1) % 2]
                    H32 = H32c[d]
                    if want:
                        MM(S_, BT[rows, blk], CT[rows, blk], True, True, BT.b + CT.b, bank1.b)
                        yield
                        MM(B_, CT[rows, blk], hb_cur[rows, :], True, True, CT.b + hb_cur.b, bank1.b)
                        yield
                    if not lastit:
                        for hh in range(4):
                            col = c0 + hh
                            bw = BwT[d * 4 + hh]
                            ACT(bw[:, rows], Btok[:, tb, rows], AF.Identity, Btok.b + wsp.b, bw.b, scale=wsp[:, tb, col:col + 1])
                            yield
                            MM(bank2[:, 256 + hh * 64:256 + (hh + 1) * 64], bw[:], xtok[:, tb, hh * 64:(hh + 1) * 64], True, True,
                               bw.b + xtok.b, bank2.b)
                            yield
                        TT("dve", H32[rows, :].rearrange("p (a c) -> p a c", a=4), H32[rows, :].rearrange("p (a c) -> p a c", a=4),
                           dec[rows, tb, c0:c0 + 4].unsqueeze(2).to_broadcast([64, 4, 64]), ALU.mult, H32.b + dec.b, H32.b)
                        yield
                        TT("dve", H32[rows, :], H32[rows, :], bank2[rows, 256:512], ALU.add, H32.b + bank2.b, H32.b)
                        yield
                        CP("act", hb_nxt[rows, :], H32[rows, :], H32.b, hb_nxt.b)
                        yield
                    if want:
                        gh, gl = gt
                        for hh in range(4):
                            sel = ident_b[0:72, tb * 4 + hh:tb * 4 + hh + 1].to_broadcast([72, 128])
                            o_ = bankD[:, hh * 128:(hh + 1) * 128]
                            MM(o_, sel, gh[:, :], True, False, gh.b + CB, bankD.b)
                            MM(o_, sel, gl[:, :], False, False, gl.b + CB, bankD.b)
                            MM(o_, ident_b, mnegb[:, d * 128:(d + 1) * 128], False, True, CB, bankD.b)
                            yield
                        for hh in range(4):
                            col = c0 + hh
                            i2 = d * 4 + hh
                            ACT(DTs[i2][:, :], bankD[:, hh * 128:(hh + 1) * 128], AF.Exp, bankD.b + cbias.b, DTs[i2].b,
                                bias=cbias[:, tb, col:col + 1], scale=1.0)
                            yield
                        for hh in range(4):
                            i2 = d * 4 + hh
                            TT("dve", PTs[i2][:], S_, DTs[i2][:], ALU.mult, bank1.b + DTs[i2].b, PTs[i2].b)
                            yield
                        for hh in range(4):
                            i2 = d * 4 + hh
                            MM(bank2[:, hh * 64:(hh + 1) * 64], PTs[i2][:], xtok[:, tb, hh * 64:(hh + 1) * 64], True, True,
                               PTs[i2].b + xtok.b, bank2.b)
                            yield
                        yt = ytmp[d]
                        TT("dve", yt[:].rearrange("p (a c) -> p a c", a=4), B_.rearrange("p (a c) -> p a c", a=4),
                           wt[:, tb, c0:c0 + 4].unsqueeze(2).to_broadcast([128, 4, 64]), ALU.mult, bank1.b + wt.b, yt.b)
                        yield
                        yb = [yacc.b[tb]]
                        if tb not in yset:
                            yset.add(tb)
                            TT("dve", yacc[:, tb, :], A_, yt[:], ALU.add, bank2.b + yt.b, yb)
                            yield
                        else:
                            TT("dve", yt[:], A_, yt[:], ALU.add, bank2.b + yt.b, yt.b)
                            yield
                            TT("pool", yacc[:, tb, :], yacc[:, tb, :], yt[:], ALU.add, yb + yt.b, yb)
                            yield

            gens = [ssd_chain(0), ssd_chain(1)]
            while gens:
                for g_ in list(gens):
                    try:
                        next(g_)
                    except StopIteration:
                        gens.remove(g_)
            b0 = out_blocks[0]
            nbk = len(out_blocks)
            yv = yacc[:, b0:NB, :]
            for tb in out_blocks:
                yt = ytmp[tb % 2]
                TT("pool", yt[:].rearrange("p (a c) -> p a c", a=4), xtok[:, tb, :].rearrange("p (a c) -> p a c", a=4),
                   rv(RV_DSK + g * 4, 4).unsqueeze(2).to_broadcast([128, 4, 64]), ALU.mult, xtok.b + rowv.b, yt.b)
                TT("dve", yacc[:, tb, :], yacc[:, tb, :], yt[:], ALU.add, yacc.b + yt.b, yacc.b)
                TT("dve", yacc[:, tb, :], yacc[:, tb, :], zs[:, tb, :], ALU.mult, yacc.b + zs.b, yacc.b)
                CP("pool", G[:, tb, g * 256:(g + 1) * 256], yacc[:, tb, :], yacc.b, G.b)
                ACT(yt[:], yacc[:, tb, :], AF.Square, yacc.b, yt.b)
                RED(ssq2[:, tb, g:g + 1], yt[:], ALU.add, yt.b, ssq2.b)
        b0 = out_blocks[0]
        nbk = len(out_blocks)
        TT("dve", rstd[:, b0:NB], ssq2[:, b0:NB, 0], ssq2[:, b0:NB, 1], ALU.add, ssq2.b, rstd.b)
        ACT(rstd[:, b0:NB], rstd[:, b0:NB], AF.Ln, rstd.b, rstd.b, bias=EPS, scale=1.0 / 512)
        ACT(rstd[:, b0:NB], rstd[:, b0:NB], AF.Exp, rstd.b, rstd.b, scale=-0.5)
        for tb in out_blocks:
            TS("dve", gg[:], G[:, tb, :], rstd[:, tb:tb + 1], ALU.mult, G.b + rstd.b, gg.b)
            TT("pool", G[:, tb, :], gg[:], rv(RV_SSDG, 512), ALU.mult, gg.b + rowv.b, G.b)
        for cch in range(4):
            for tb in out_blocks:
                CP("pool", sout[:, tb, :], G[:, tb, cch * 128:(cch + 1) * 128], G.b, sout.b)
            transpose_to_yT(sout, 4 + cch, out_blocks)
        S.fence()
        RX.cur = gate_mark
        RW.reset()

        if stop_after == 'ssd' and l == 0:
            raise _Stop()
        wa = RW.alloc("wa", [128, 8, 768], BF16)
        load_w(wa, l, OFF_AQ, 768)
        qTa = RX.alloc("qTa", [128, 4, NT], BF16)
        kTa = RX.alloc("kTa", [128, 2, NT], BF16)
        va = RX.alloc("va", [128, NB, 2, 66], BF16)
        qraw = [RX.alloc(f"qraw{i}", [128, 640], F32) for i in range(2)]
        qsq = [RX.alloc(f"qsq{i}", [128, 640], F32) for i in range(2)]
        qn = [RX.alloc(f"qn{i}", [128, 640], F32) for i in range(2)]
        qr = [RX.alloc(f"qr{i}", [128, 640], BF16) for i in range(2)]
        kd = [RX.alloc(f"kd{i}", [128, 2, 128], BF16) for i in range(2)]
        rq = [RX.alloc(f"rq{i}", [128, 10], F32) for i in range(2)]
        t1 = [RX.alloc(f"t1{i}", [128, 10, 2, 16], F32) for i in range(2)]
        t2 = [RX.alloc(f"t2{i}", [128, 10, 2, 16], F32) for i in range(2)]
        PTa = [RW.alloc(f"PTa{i}", [128, 512], BF16) for i in range(10)]
        otok = [RX.alloc(f"otok{i}", [128, 512], BF16) for i in range(2)]
        dna = RX.alloc("dna", [128, 16], F32)
        esink = RX.alloc("esink", [128, 8], F32)
        gqk = RX.alloc("gqk", [128, 640], F32)
        MSET("pool", va[:, :, :, 64:66], 1.0, va.b)
        ACT(esink[:], rv(RV_SINK, 8), AF.Exp, rowv.b, esink.b)
        for hh in range(8):
            CP("dve", gqk[:, hh * 64:(hh + 1) * 64], rv(RV_QG, 64), rowv.b, gqk.b)
        for hh in range(2):
            TS("dve", gqk[:, 512 + hh * 64:512 + (hh + 1) * 64], rv(RV_KG, 64), 8.0, ALU.mult, rowv.b, gqk.b)
        def att_prep(tb):
            i2 = tb % 2
            ps = proj_tok(wa, 0, 512, tb, tile_of(tb), ps=ps32[2 * i2])
            yield
            CP("act", qraw[i2][:, 0:512], ps[:, 0:512], ps.b, qraw[i2].b)
            yield
            ps = proj_tok(wa, 512, 256, tb, tile_of(tb), ps=ps32[2 * i2 + 1])
            yield
            CP("act", qraw[i2][:, 512:640], ps[:, 0:128], ps.b, qraw[i2].b)
            yield
            CP("dve", va[:, tb, :, 0:64], ps[:, 128:256].rearrange("p (g c) -> p g c", g=2), ps.b, va.b)
            yield
            q3 = qraw[i2][:].rearrange("p (h c) -> p h c", h=10)
            TT("dve", qsq[i2][:], qraw[i2][:], qraw[i2][:], ALU.mult, qraw[i2].b, qsq[i2].b)
            yield
            RED(rq[i2][:], qsq[i2][:].rearrange("p (h c) -> p h c", h=10), ALU.add, qsq[i2].b, rq[i2].b)
            yield
            ACT(rq[i2][:], rq[i2][:], AF.Ln, rq[i2].b, rq[i2].b, bias=64 * EPS, scale=1.0)
            yield
            ACT(rq[i2][:], rq[i2][:], AF.Exp, rq[i2].b, rq[i2].b, scale=-0.5)
            yield
            qn3 = qn[i2][:].rearrange("p (h c) -> p h c", h=10)
            TT("dve", qn3, q3, rq[i2][:].unsqueeze(2).to_broadcast([128, 10, 64]), ALU.mult, qraw[i2].b + rq[i2].b, qn[i2].b)
            yield
            if tb >= 2:
                TT("pool", qn[i2][:], qn[i2][:], gqk[:], ALU.mult, qn[i2].b + gqk.b, qn[i2].b)
                yield
                q5 = qn[i2][:].rearrange("p (h a b i) -> p h a b i", h=10, a=2, b=2)
                o5 = qr[i2][:].rearrange("p (h a b i) -> p h a b i", h=10, a=2, b=2)
                cosb = rcos[:, tb - 2, :, :].unsqueeze(1).to_broadcast([128, 10, 2, 16])
                sinb = rsin[:, tb - 2, :, :].unsqueeze(1).to_broadcast([128, 10, 2, 16])
                x1, x2 = q5[:, :, :, 0, :], q5[:, :, :, 1, :]
                TT("dve", t1[i2][:], x1, cosb, ALU.mult, qn[i2].b + rope.b, t1[i2].b)
                yield
                TT("pool", t2[i2][:], x2, sinb, ALU.mult, qn[i2].b + rope.b, t2[i2].b)
                yield
                TT("dve", o5[:, :, :, 0, :], t1[i2][:], t2[i2][:], ALU.subtract, t1[i2].b + t2[i2].b, qr[i2].b)
                yield
                TT("dve", t1[i2][:], x2, cosb, ALU.mult, qn[i2].b + rope.b, t1[i2].b)
                yield
                TT("pool", t2[i2][:], x1, sinb, ALU.mult, qn[i2].b + rope.b, t2[i2].b)
                yield
                TT("dve", o5[:, :, :, 1, :], t1[i2][:], t2[i2][:], ALU.add, t1[i2].b + t2[i2].b, qr[i2].b)
                yield
            else:
                TT("pool", qr[i2][:], qn[i2][:], gqk[:], ALU.mult, qn[i2].b + gqk.b, qr[i2].b)
                yield
            for g in range(2):
                for hf in range(2):
                    CP("pool", kd[i2][:, g, hf * 64:(hf + 1) * 64], qr[i2][:, 512 + g * 64:512 + (g + 1) * 64], qr[i2].b, kd[i2].b)
                    yield
            pb = psb[i2]
            for a in range(4):
                TR(pb[:, a * 128:(a + 1) * 128], qr[i2][:, a * 128:(a + 1) * 128], ident_b, qr[i2].b + CB, pb.b)
                yield
            CP("act", qTa[:, :, tb * 128:(tb + 1) * 128], pb[:, 0:512].rearrange("p (a t) -> p a t", a=4), pb.b, qTa.b)
            yield
            pb = psb[i2]
            for g in range(2):
                TR(pb[:, g * 128:(g + 1) * 128], kd[i2][:, g, :], ident_b, kd[i2].b + CB, pb.b)
                yield
            CP("act", kTa[:, :, tb * 128:(tb + 1) * 128], pb[:, 0:256].rearrange("p (a t) -> p a t", a=2), pb.b, kTa.b)
            yield

        pend, active = list(range(NB)), []
        while pend or active:
            while len(active) < 2 and pend:
                active.append(att_prep(pend.pop(0)))
            for g_ in list(active):
                try:
                    next(g_)
                except StopIteration:
                    active.remove(g_)
        dump("qTa", qTa, [128, 4, NT], BF16); dump("kTa", kTa, [128, 2, NT], BF16)
        pi = 0
        for qi, tb in enumerate(out_blocks):
            keys = [(0, None), (1, None)]
            if tb >= 2:
                if tb - 1 >= 2:
                    keys.append((tb - 1, 0))
                keys.append((tb, None))
                if tb + 1 < NB:
                    keys.append((tb + 1, 1))
            psO = [ps32[0], ps32[1]]
            for ki, (kb, mk) in enumerate(keys):
                for g in range(2):
                    pt = PTa[ki * 2 + g]
                    for hf in range(2):
                        psS = ps32[2 + hf + 2 * (pi % 2)]
                        prow = slice(hf * 64, (hf + 1) * 64)
                        MM(psS[:, 0:256].rearrange("p (a t) -> p a t", a=2),
                           kTa[prow, g, kb * 128:(kb + 1) * 128], qTa[prow, 2 * g:2 * g + 2, tb * 128:(tb + 1) * 128],
                           True, True, kTa.b + qTa.b, psS.b)
                        ACT(pt[:, hf * 256:(hf + 1) * 256], psS[:, 0:256], AF.Exp, psS.b, pt.b)
                    pi += 1
                    if mk is not None:
                        TT("pool", pt[:], pt[:], m01[:, mk * 512:(mk + 1) * 512], ALU.mult, pt.b + m01.b, pt.b)
            for g in range(2):
                for hf in range(2):
                    for al in range(2):
                        cb_ = hf * 2 + al
                        hl = 2 * al + hf
                        for ki, (kb, mk) in enumerate(keys):
                            pt = PTa[ki * 2 + g]
                            MM(psO[g][:, hl * 66:(hl + 1) * 66], pt[:, cb_ * 128:(cb_ + 1) * 128], va[:, kb, g, 0:66],
                               ki == 0, ki == len(keys) - 1, pt.b + va.b, psO[g].b)
            ot = otok[qi % 2]
            for g in range(2):
                o4 = psO[g][:, 0:264].rearrange("p (h c) -> p h c", h=4)
                TT("dve", dna[:, g * 4:(g + 1) * 4], o4[:, :, 64], esink[:, g * 4:(g + 1) * 4], ALU.add, psO[g].b + esink.b, dna.b)
                RECIP(dna[:, 8 + g * 4:8 + (g + 1) * 4], dna[:, g * 4:(g + 1) * 4], dna.b, dna.b)
                TT("dve", ot[:, g * 256:(g + 1) * 256].rearrange("p (h c) -> p h c", h=4), o4[:, :, 0:64],
                   dna[:, 8 + g * 4:8 + (g + 1) * 4].unsqueeze(2).to_broadcast([128, 4, 64]), ALU.mult, psO[g].b + dna.b, ot.b)
            pb = PB()
            for cch in range(4):
                TR(pb[:, cch * 128:(cch + 1) * 128], ot[:, cch * 128:(cch + 1) * 128], ident_b, ot.b + CB, pb.b)
            CP("act", yT[:, 8:12, tb * 128:(tb + 1) * 128], pb[:, 0:512].rearrange("p (a t) -> p a t", a=4), pb.b, yT.b)
        dump("yT", yT, [128, 12, NT], BF16)
        S.fence()
        RX.reset()
        RW.reset()

        if stop_after == 'attn' and l == 0:
            raise _Stop()
        xT = RX.alloc("xT", [128, 8, NT], F32, nb=1)
        DMA("sp", xT[:].rearrange("p k t -> p (k t)"), xs_d, (), xT.b)
        tiles_out = tiles_all if need_ctx else tiles_all[1:]
        wo = RW.alloc("wo", [128, 12, 1024], BF16, nb=2)
        for hf in range(2):
            DMA("pool", wo[:, :, hf * 512:(hf + 1) * 512],
                wout_d[l].rearrange("(m p) f -> p m f", p=128)[:, :, hf * 512:(hf + 1) * 512], (), [wo.b[hf]])
        for ti in tiles_out:
            s0, n, j = TILES[ti]
            for c in range(8):
                ps = P32()
                for m in range(12):
                    MM(ps[:, 0:n], wo[:, m, c * 128:(c + 1) * 128], yT[:, m, s0:s0 + n], m == 0, m == 11, [wo.b[c // 4]] + yT.b, ps.b)
                STT(xT[:, c, s0:s0 + n], ps[:, 0:n], mp[:, 16 + c, j:j + 1], xT[:, c, s0:s0 + n], ALU.mult, ALU.add,
                    ps.b + mp.b + xT.b, xT.b)
        dump("x1", xT, [128, 8, NT])
        S.fence()
        RW.reset()
        RY.reset()
        if stop_after == 'outproj' and l == 0:
            raise _Stop()
        norm_to_h(l, 32, 24, tiles_out)
        wu = [RY.alloc(f"wu{i}", [128, 8, 512], BF16) for i in range(2)]
        wd = [RY.alloc(f"wd{i}", [128, 4, 1024], BF16) for i in range(2)]
        act = [RY.alloc(f"act{i}", [128, 4, 512], BF16) for i in range(2)]
        rl = [RY.alloc(f"rl{i}", [128, 512], F32) for i in range(2)]
        wup_v = wup_d[l].rearrange("(k p) f -> p k f", p=128)
        wdn_v = wdn_d[l].rearrange("(m p) f -> p m f", p=128)
        cnt = 0
        mg = None
        if l + 1 < depth:
            bm1 = RW.alloc("bm", [128, 48], F32)
            g121 = RW.alloc("g12", [128, 16], F32)
            wm1 = [RW.alloc("wm1", [128, 8, 512], BF16), RY.alloc("wm1b", [128, 8, 512], BF16)]
            mg = mod_gen(l + 1, wm1, bm1, g121)

        def load_mlp(sl):
            DMA("pool", wu[sl % 2][:], wup_v[:, :, sl * 512:(sl + 1) * 512], (), wu[sl % 2].b)
            DMA("pool", wd[sl % 2][:], wdn_v[:, sl * 4:(sl + 1) * 4, :], (), wd[sl % 2].b)

        load_mlp(0)
        for sl in range(8):
            u, dd = wu[sl % 2], wd[sl % 2]
            if sl + 1 < 8:
                load_mlp(sl + 1)
            for ti in tiles_out:
                s0, n, j = TILES[ti]
                a = act[cnt % 2]
                cnt += 1
                if mg is not None and cnt % 2 == 0:
                    if next(mg, "done") == "done":
                        mg = None
                for fc in range(4):
                    ps = P32()
                    for k in range(8):
                        MM(ps[:, 0:n], u[:, k, fc * 128:(fc + 1) * 128], hT[:, k, s0:s0 + n], k == 0, k == 7,
                           u.b + [hT.b[ti]], ps.b)
                    r_ = rl[fc % 2]
                    ACT(r_[:, 0:n], ps[:, 0:n], AF.Relu, ps.b, r_.b)
                    TT("pool", a[:, fc, 0:n], r_[:, 0:n], r_[:, 0:n], ALU.mult, r_.b, a.b)
                for c in range(8):
                    ps = P32()
                    for fc in range(4):
                        MM(ps[:, 0:n], dd[:, fc, c * 128:(c + 1) * 128], a[:, fc, 0:n], fc == 0, fc == 3, dd.b + a.b, ps.b)
                    STT(xT[:, c, s0:s0 + n], ps[:, 0:n], mp[:, 40 + c, j:j + 1], xT[:, c, s0:s0 + n], ALU.mult, ALU.add,
                        ps.b + mp.b + xT.b, xT.b)
        if mg is not None:
            for _ in mg:
                pass
        S.fence()
        RW.reset()
        RY.reset()
        yT = RY.alloc("yT", [128, 12, NT], BF16, nb=1)


    try:
        for l in range(depth):
            layer(l)
    except _Stop:
        S.fence()
        RW.reset()

    xo = [RW.alloc(f"xo{i}", [128, D], F32) for i in range(3)]
    finals = []
    for tb in range(2, NB):
        o = xo[tb % 3]
        for hf in range(2):
            ps = P32()
            for j in range(4):
                k = hf * 4 + j
                TR(ps[:, j * 128:(j + 1) * 128], xT[:, k, tb * 128:(tb + 1) * 128], ident_f, xT.b + CB, ps.b)
            CP("act" if hf else "dve", o[:, hf * 512:(hf + 1) * 512], ps[:, 0:512], ps.b, o.b)
        finals.append(DMA("sp", out_d[(tb - 2) * 128:(tb - 1) * 128, :], o[:], o.b, ()))
    finals += list(dbg_out.values())
    S.emit(finals)
    return S


def _host_constants():
    s = np.arange(128)[:, None]
    t = np.arange(128)[None, :]
    ident = np.eye(128, dtype=np.float32)
    triF = (s <= t).astype(np.float32)
    triR = (s >= t).astype(np.float32)
    mnegF = np.where(s <= t, 0.0, NEG).astype(np.float32)
    mnegR = np.where(s >= t, 0.0, NEG).astype(np.float32)
    ones = np.ones((128, 128), np.float32)
    cst = np.concatenate([ident, triF, triR, mnegF, mnegR, ones], axis=1)
    m01 = np.concatenate([np.tile(triR, (1, 4)), np.tile(triF, (1, 4))], axis=1).astype(np.float32)
    pos = np.arange(SEQ)
    rows, cols = pos // 64, pos % 64
    inv = 10000.0 ** (-np.arange(16, dtype=np.float32) / 16)
    ang = np.stack([rows[:, None] * inv[None, :], cols[:, None] * inv[None, :]], axis=1).astype(np.float32)
    ang = ang.reshape(16, 128, 2, 16).transpose(1, 0, 2, 3).reshape(128, 512)
    rope = np.concatenate([np.cos(ang), np.sin(ang)], axis=1).astype(np.float32)
    return cst, m01, rope


_CACHE = {}


def kernel(x, c, ctx, c_ctx, w_mod, b_mod, norm1_g, w_in, ml_b_i, ml_b_f, ml_norm_g,
           ssd_conv_w, ssd_conv_b, ssd_a_log, ssd_dt_bias, ssd_d, ssd_norm_g,
           att_qn_g, att_kn_g, att_sink, w_out, norm2_g, w_up, w_down, _dbg=(), _cores=8, _stop=None):
    f = lambda a: np.ascontiguousarray(np.asarray(a, dtype=np.float32))
    x, c, ctx, c_ctx = f(x), f(c), f(ctx), f(c_ctx)
    depth = w_mod.shape[0]
    cst, m01, rope = _host_constants()
    pk = lambda v: f(v).reshape(-1, 128).T
    bmod = np.stack([pk(b_mod[l]) for l in range(depth)])
    g1 = np.stack([pk(norm1_g[l]) for l in range(depth)])
    g2 = np.stack([pk(norm2_g[l]) for l in range(depth)])
    rowv = np.zeros((depth, 1, 1280), np.float32)
    convp = np.zeros((depth, 128, 24), np.float32)
    for l in range(depth):
        r = rowv[l, 0]
        r[0:8] = f(ml_b_i[l]).reshape(-1)
        r[8:16] = f(ml_b_f[l]).reshape(-1)
        r[16:528] = f(ml_norm_g[l])
        r[528:544] = f(ssd_a_log[l]).reshape(-1)
        r[544:560] = f(ssd_dt_bias[l]).reshape(-1)
        r[560:568] = f(ssd_d[l])
        r[568:1080] = f(ssd_norm_g[l])
        r[1080:1144] = f(att_qn_g[l])
        r[1144:1208] = f(att_kn_g[l])
        r[1208:1216] = f(att_sink[l])
        cw = f(ssd_conv_w[l])
        cb = f(ssd_conv_b[l])
        for ch in range(6):
            convp[l, :, ch * 4 + 0] = cw[0, ch * 128:(ch + 1) * 128]
            convp[l, :, ch * 4 + 1] = cw[1, ch * 128:(ch + 1) * 128]
            convp[l, :, ch * 4 + 2] = cw[2, ch * 128:(ch + 1) * 128]
            convp[l, :, ch * 4 + 3] = cb[ch * 128:(ch + 1) * 128]
    key = (depth, tuple(_dbg))
    nc = bass.Bass("TRN2", target_bir_lowering=False)
    build(nc, depth=depth, dbg=_dbg, stop_after=_stop)
    shared = {"w_mod": f(w_mod), "b_mod": bmod, "norm1_g": g1, "norm2_g": g2, "w_in": f(w_in), "w_out": f(w_out),
              "w_up": f(w_up), "w_down": f(w_down), "rowv": rowv, "convp": convp, "cst": cst, "m01": m01, "rope": rope}
    in_maps = []
    for b in range(_cores):
        cc = np.stack([pk(c[b]), pk(c_ctx)], axis=2).reshape(128, 16)
        m = dict(shared)
        m.update({"x": x[b], "ctx": ctx[b], "cc": np.ascontiguousarray(cc)})
        in_maps.append(m)
    res = run_bass_kernel_spmd(nc, in_maps, core_ids=list(range(_cores)))
    out = np.stack([res.results[b]["out"] for b in range(_cores)], axis=0).astype(np.float32)
    if _dbg:
        return out, res.results
    return out
```

```python
import math
from contextlib import ExitStack

import numpy as np
import ml_dtypes
import concourse.bass as bass
import concourse.mybir as mybir
from concourse.bass_utils import run_bass_kernel_spmd

F32 = mybir.dt.float32
BF16 = mybir.dt.bfloat16
ALU = mybir.AluOpType
AF = mybir.ActivationFunctionType
AX = mybir.AxisListType

D = 1024
SEQ = 2048
CTX = 256
NT = SEQ + CTX
NB = NT // 128
DEPTH = 2
EPS = 1e-6
IN_W = 4128
OFF_MLQ, OFF_MLK, OFF_MLV, OFF_MLO, OFF_MLI, OFF_MLF = 0, 512, 1024, 1536, 2048, 2056
OFF_Z, OFF_XBC, OFF_DT, OFF_AQ, OFF_AK, OFF_AV = 2064, 2576, 3344, 3360, 3872, 4000
TILES = [(0, 256, 1)] + [(256 + 512 * i, 512, 0) for i in range(4)]
NEG = -30000.0


class Buf:
    __slots__ = ("name", "writer", "readers", "dma_readers", "excl")

    def __init__(self, name=""):
        self.excl = False
        self.name = name
        self.writer = None
        self.readers = {}
        self.dma_readers = []


class Rec:
    __slots__ = ("eng", "fn", "deps", "inc", "is_dma", "sem", "count")

    def __init__(self, eng, fn, is_dma):
        self.eng = eng
        self.fn = fn
        self.deps = []
        self.inc = False
        self.is_dma = is_dma
        self.sem = None
        self.count = 0


class Sched:
    ENGS = ("pe", "dve", "act", "pool", "sp")
    NDMA = 24

    def __init__(self, nc):
        self.nc = nc
        self.streams = {e: [] for e in self.ENGS}
        self.dma_rr = 0
        self.dma_rr2 = [0, 0]
        self.dma_last = [None] * self.NDMA
        self.dma_cnt = [0] * self.NDMA
        self.last = {e: None for e in self.ENGS}

    def op(self, eng, fn, R=(), W=(), dma=False, extra=()):
        rec = Rec(eng, fn, dma)
        deps = {}

        def add(d):
            if d is None or d is rec:
                return
            if d.eng == "pe" and eng == "pe" and not d.is_dma and not dma:
                return
            deps[id(d)] = d

        for b in R:
            add(b.writer)
            if b.excl:
                for r in b.readers.values():
                    if r.eng != eng:
                        add(r)
        for b in W:
            add(b.writer)
            for r in b.readers.values():
                add(r)
            for r in b.dma_readers:
                add(r)
        for d in extra:
            add(d)
        if dma:
            half = self.NDMA // 2
            sw = 1 if eng == "pool" else 0
            k = sw * half + self.dma_rr2[sw]
            self.dma_rr2[sw] = (self.dma_rr2[sw] + 1) % half
            add(self.dma_last[k])
            self.dma_last[k] = rec
            self.dma_cnt[k] += 16
            rec.sem = k
            rec.count = self.dma_cnt[k]
        elif fn is not None:
            self.last[eng] = rec
        rec.deps = list(deps.values())
        for d in rec.deps:
            d.inc = True
        for b in R:
            if dma:
                b.dma_readers.append(rec)
            else:
                b.readers[eng] = rec
        for b in W:
            b.writer = rec
            b.readers = {}
            b.dma_readers = []
        self.streams[eng].append(rec)
        return rec

    def fence(self):
        pend = [r for r in self.last.values() if r is not None]
        pend += [r for r in self.dma_last if r is not None]
        for e in self.ENGS:
            self.op(e, None, extra=pend)

    def emit(self, final_recs):
        nc = self.nc
        for r in final_recs:
            r.inc = True
        for e in self.ENGS:
            c = 0
            for r in self.streams[e]:
                if r.is_dma or r.fn is None:
                    continue
                if r.inc:
                    c += 1
                    r.count = c
        with ExitStack() as es:
            esem = {e: es.enter_context(nc.semaphore(f"sem_{e}")) for e in self.ENGS}
            dsem = [es.enter_context(nc.semaphore(f"dsem{k}")) for k in range(self.NDMA)]
            block = es.enter_context(nc.Block())

            def replay(e, engine, extra_final=None):
                waited = {}

                def wait(d):
                    if d.is_dma:
                        key, sem = ("d", d.sem), dsem[d.sem]
                    else:
                        key, sem = ("e", d.eng), esem[d.eng]
                    if waited.get(key, 0) >= d.count:
                        return
                    waited[key] = d.count
                    engine.wait_ge(sem, d.count)

                for r in self.streams[e]:
                    for d in r.deps:
                        wait(d)
                    if r.fn is None:
                        continue
                    ins = r.fn(engine)
                    if r.is_dma:
                        ins.then_inc(dsem[r.sem], 16)
                    elif r.inc:
                        ins.then_inc(esem[e], 1)
                for d in extra_final or ():
                    wait(d)

            @block.tensor
            def _(eng):
                replay("pe", eng)

            @block.vector
            def _(eng):
                replay("dve", eng)

            @block.scalar
            def _(eng):
                replay("act", eng)

            @block.gpsimd
            def _(eng):
                replay("pool", eng)

            @block.sync
            def _(eng):
                replay("sp", eng, extra_final=final_recs)


class Tl:
    def __init__(self, t, nb, name):
        self.t = t
        self.b = [Buf(f"{name}.{i}") for i in range(nb)]

    def __getitem__(self, idx):
        return self.t[idx]


class Region:
    def alias(self, name, shape, dtype, off, bufs):
        self.n += 1
        t = self.nc.alloc_sbuf_tensor_at(f"{self.name}_{name}_{self.n}", list(shape), dtype, offset=self.base + off)
        tl = Tl(t, 0, name)
        tl.b = bufs
        return tl

    def __init__(self, nc, base, size, name):
        self.nc, self.base, self.size, self.cur, self.name, self.n = nc, base, size, 0, name, 0

    def reset(self):
        self.cur = 0

    def alloc(self, name, shape, dtype, nb=1):
        nbytes = int(np.prod(shape[1:])) * (4 if dtype == F32 else 2)
        nbytes = (nbytes + 31) // 32 * 32
        assert self.cur + nbytes <= self.size, f"region {self.name} overflow at {name}: {self.cur}+{nbytes}>{self.size}"
        self.n += 1
        t = self.nc.alloc_sbuf_tensor_at(f"{self.name}_{name}_{self.n}", list(shape), dtype, offset=self.base + self.cur)
        self.cur += nbytes
        return Tl(t, nb, name)


class _Stop(Exception):
    pass


def build(nc, depth=DEPTH, dbg=(), stop_after=None):
    S = Sched(nc)
    dbg_out = {}

    def din(name, shape, dt=F32):
        return nc.dram_tensor(name, list(shape), dt, kind="ExternalInput").ap()

    x_d = din("x", [SEQ, D])
    ctx_d = din("ctx", [CTX, D])
    cc_d = din("cc", [128, 16])
    wmod_d = din("w_mod", [depth, D, 6 * D])
    bmod_d = din("b_mod", [depth, 128, 48])
    g1_d = din("norm1_g", [depth, 128, 8])
    g2_d = din("norm2_g", [depth, 128, 8])
    win_d = din("w_in", [depth, D, IN_W])
    wout_d = din("w_out", [depth, 1536, D])
    wup_d = din("w_up", [depth, D, 4 * D])
    wdn_d = din("w_down", [depth, 4 * D, D])
    rowv_d = din("rowv", [depth, 1, 1280])
    convp_d = din("convp", [depth, 128, 24])
    cst_d = din("cst", [128, 128 * 6])
    m01_d = din("m01", [128, 1024])
    rope_d = din("rope", [128, 2 * 16 * 32])
    out_d = nc.dram_tensor("out", [SEQ, D], F32, kind="ExternalOutput").ap()
    xs_d = nc.dram_tensor("xs_scratch", [128, 8 * NT], F32, kind="Internal").ap()

    base0 = 16512
    slab = nc.alloc_sbuf_tensor("slab", [128, (229344 - base0) // 4 - 8], F32)
    sizes = dict(C=16384, X=73728, H=36864, Y=55296)
    off = base0
    RC = Region(nc, off, sizes["C"], "C"); off += sizes["C"]
    RX = Region(nc, off, sizes["X"], "X"); off += sizes["X"]
    RH = Region(nc, off, sizes["H"], "H"); off += sizes["H"]
    RY = Region(nc, off, sizes["Y"], "Y"); off += sizes["Y"]
    RW = Region(nc, off, 229344 - off - 64, "W")

    ps32 = [Tl(nc.alloc_psum_tensor(f"ps{i}", [128, 512], F32), 1, f"ps{i}") for i in range(6)]
    psb = [Tl(nc.alloc_psum_tensor(f"psb{i}", [128, 1024], BF16), 1, f"psb{i}") for i in range(2)]
    for _t in ps32 + psb:
        _t.b[0].excl = True
    pctr = [0, 0, 0]

    def P32():
        pctr[0] += 1
        return ps32[pctr[0] % 6]

    def PD():
        pctr[2] += 1
        return ps32[4 + pctr[2] % 2]

    def PB():
        pctr[1] += 1
        return psb[pctr[1] % 2]

    def MM(out, lhsT, rhs, start, stop, R, W):
        S.op("pe", lambda e: e.matmul(out, lhsT=lhsT, rhs=rhs, start=start, stop=stop), R, W)

    def TR(out, in_, ident, R, W):
        S.op("pe", lambda e: e.transpose(out=out, in_=in_, identity=ident), R, W)

    def ACT(out, in_, func, R, W, bias=None, scale=None):
        kw = {}
        if bias is not None:
            kw["bias"] = bias
        if scale is not None:
            kw["scale"] = scale
        S.op("act", lambda e: e.activation(out=out, in_=in_, func=func, **kw), R, W)

    import os as _os
    _nopool = _os.environ.get("KPOOL") != "1"

    def _pe(eng):
        return "dve" if (_nopool and eng == "pool") else eng

    def TT(eng, out, in0, in1, op, R, W):
        eng = _pe(eng)
        S.op(eng, lambda e: e.tensor_tensor(out=out, in0=in0, in1=in1, op=op), R, W)

    def TS(eng, out, in0, s1, op0, R, W, s2=None, op1=None):
        eng = _pe(eng)
        if op1 is None:
            S.op(eng, lambda e: e.tensor_scalar(out=out, in0=in0, scalar1=s1, scalar2=None, op0=op0), R, W)
        else:
            S.op(eng, lambda e: e.tensor_scalar(out=out, in0=in0, scalar1=s1, scalar2=s2, op0=op0, op1=op1), R, W)

    def STT(out, in0, scalar, in1, op0, op1, R, W):
        S.op("dve", lambda e: e.scalar_tensor_tensor(out=out, in0=in0, scalar=scalar, in1=in1, op0=op0, op1=op1), R, W)

    def CP(eng, out, in_, R, W):
        eng = _pe(eng)
        if eng == "act":
            S.op("act", lambda e: e.copy(out=out, in_=in_), R, W)
        else:
            S.op(eng, lambda e: e.tensor_copy(out=out, in_=in_), R, W)

    def RED(out, in_, op, R, W):
        S.op("dve", lambda e: e.tensor_reduce(out=out, in_=in_, axis=AX.X, op=op), R, W)

    def RECIP(out, in_, R, W):
        S.op("dve", lambda e: e.reciprocal(out=out, in_=in_), R, W)

    def MSET(eng, ap, val, W):
        eng = _pe(eng)
        S.op(eng, lambda e: e.memset(ap, val), (), W)

    def DMA(eng, out, in_, R, W, slow=False):
        if slow:
            return S.op(eng, lambda e: e.dma_start(out=out, in_=in_, allow_slow_non_contiguous=True), R, W, dma=True)
        return S.op(eng, lambda e: e.dma_start(out=out, in_=in_), R, W, dma=True)

    def dump(name, tl, shape, dt=F32):
        if name in dbg and name not in dbg_out:
            d = nc.dram_tensor("dbg_" + name, list(shape), dt, kind="ExternalOutput").ap()
            dbg_out[name] = DMA("sp", d, tl.t[:], tl.b, ())

    def rsqrt_chain(out, in_, add, R, W):
        ACT(out, in_, AF.Ln, R, W, bias=add, scale=1.0)
        ACT(out, out, AF.Exp, W, W, scale=-0.5)

    cst = RC.alloc("cst", [128, 768], F32)
    DMA("sp", cst[:], cst_d, (), cst.b)
    ident_f, triF, triR = cst[:, 0:128], cst[:, 128:256], cst[:, 256:384]
    mneg = [cst[:, 384:512], cst[:, 512:640]]
    ones_f = cst[:, 640:768]
    cstb = RC.alloc("cstb", [128, 256], BF16)
    DMA("pool", cstb[:, 0:128], cst_d[:, 0:128], (), cstb.b)
    DMA("pool", cstb[:, 128:256], cst_d[:, 640:768], (), cstb.b)
    ident_b, ones_b = cstb[:, 0:128], cstb[:, 128:256]
    m01 = RC.alloc("m01", [128, 1024], BF16)
    DMA("pool", m01[:], m01_d, (), m01.b)
    rope = RC.alloc("rope", [128, 1024], F32)
    DMA("sp", rope[:], rope_d, (), rope.b)
    rcos = rope[:, 0:512].rearrange("p (b h i) -> p b h i", b=16, h=2)
    rsin = rope[:, 512:1024].rearrange("p (b h i) -> p b h i", b=16, h=2)
    CB = cst.b + cstb.b

    modp = [RC.alloc(f"modp{l}", [128, 48, 2], F32) for l in range(depth)]
    rowv = RC.alloc("rowv", [128, 1280], F32)
    convp = RC.alloc("convp", [128, 24], F32)

    cc = RC.alloc("cc", [128, 16], F32)
    DMA("sp", cc[:], cc_d, (), cc.b)
    s2b = RC.alloc("s2b", [128, 8, 2], BF16)
    ACT(s2b[:].rearrange("p k j -> p (k j)"), cc[:], AF.Silu, cc.b, s2b.b)
    mnegb = RC.alloc("mnegb", [128, 256], BF16)
    DMA("pool", mnegb[:], cst_d[:, 384:640], (), mnegb.b)
    CB = CB + mnegb.b

    def mod_gen(l, wm, bm, g12):
        DMA("sp", bm[:], bmod_d[l], (), bm.b)
        DMA("sp", g12[:, 0:8], g1_d[l], (), g12.b)
        DMA("sp", g12[:, 8:16], g2_d[l], (), g12.b)
        mp = modp[l]
        def ld(fg):
            w_ = wm[fg % len(wm)]
            DMA("pool", w_[:], wmod_d[l].rearrange("(k p) f -> p k f", p=128)[:, :, fg * 512:(fg + 1) * 512], (), w_.b)

        ld(0)
        for fg in range(12):
            w = wm[fg % len(wm)]
            if fg + 1 < 12 and len(wm) > 1:
                ld(fg + 1)
            elif fg > 0 and len(wm) == 1:
                ld(fg)
            ps = P32()
            for j in range(4):
                for k in range(8):
                    MM(ps[:, 2 * j:2 * j + 2], w[:, k, j * 128:(j + 1) * 128], s2b[:, k, :], k == 0, k == 7,
                       w.b + s2b.b, ps.b)
            TT("dve", mp[:, fg * 4:(fg + 1) * 4, :], ps[:, 0:8].rearrange("p (a j) -> p a j", j=2),
               bm[:, fg * 4:(fg + 1) * 4].unsqueeze(2).to_broadcast([128, 4, 2]), ALU.add, ps.b + bm.b, mp.b)
            yield
        for (so, go) in ((8, 0), (32, 8)):
            TS("dve", mp[:, so:so + 8, :], mp[:, so:so + 8, :], 1.0, ALU.add, mp.b, mp.b, s2=32.0, op1=ALU.mult)
            TT("dve", mp[:, so:so + 8, :], mp[:, so:so + 8, :],
               g12[:, go:go + 8].unsqueeze(2).to_broadcast([128, 8, 2]), ALU.mult, mp.b + g12.b, mp.b)
        yield

    bm0 = RW.alloc("bm", [128, 48], F32)
    g120 = RW.alloc("g12", [128, 16], F32)
    wm0 = [RW.alloc(f"wm{i}", [128, 8, 512], BF16) for i in range(2)]
    xT = RX.alloc("xT", [128, 8, NT], F32, nb=1)
    xin = [RW.alloc(f"xin{i}", [128, D], F32) for i in range(3)]
    def xload_gen():
      for tb in range(NB):
          xi = xin[tb % 3]
          src = ctx_d[tb * 128:(tb + 1) * 128, :] if tb < 2 else x_d[(tb - 2) * 128:(tb - 1) * 128, :]
          DMA("sp", xi[:], src, (), xi.b)
          for hf in range(2):
              ps = P32()
              for j in range(4):
                  k = hf * 4 + j
                  TR(ps[:, j * 128:(j + 1) * 128], xi[:, k * 128:(k + 1) * 128], ident_f, xi.b + CB, ps.b)
              CP("act" if hf else "dve", xT[:, hf * 4:(hf + 1) * 4, tb * 128:(tb + 1) * 128],
                 ps[:, :].rearrange("p (a t) -> p a t", a=4), ps.b, xT.b)
          yield

    g_a, g_b = xload_gen(), mod_gen(0, wm0, bm0, g120)
    live = [g_a, g_b]
    while live:
        for g_ in list(live):
            try:
                next(g_)
            except StopIteration:
                live.remove(g_)
    S.fence()
    RW.reset()

    hT = RH.alloc("hT", [128, 8, NT], BF16, nb=len(TILES))
    yT = RY.alloc("yT", [128, 12, NT], BF16, nb=1)

    def norm_to_h(l, a_off, b_off, tiles):
        mp = modp[l]
        sq = RW.alloc("nsq", [128, 8, 512], BF16)
        rs = RW.alloc("nrs", [128, 512], F32)
        tmp = [RW.alloc(f"ntmp{i}", [128, 512], F32) for i in range(2)]
        for ti, (s0, n, j) in enumerate(TILES):
            if ti not in tiles:
                continue
            ACT(sq[:, :, 0:n], xT[:, :, s0:s0 + n], AF.Square, xT.b, sq.b)
            ps = P32()
            for k in range(8):
                MM(ps[:, 0:n], ones_b, sq[:, k, 0:n], k == 0, k == 7, sq.b + CB, ps.b)
            rsqrt_chain(rs[:, 0:n], ps[:, 0:n], float(D * EPS), ps.b, rs.b)
            for k in range(8):
                t = tmp[k % 2]
                STT(t[:, 0:n], xT[:, k, s0:s0 + n], mp[:, a_off + k, j:j + 1], rs[:, 0:n], ALU.mult, ALU.mult,
                    xT.b + mp.b + rs.b, t.b)
                ACT(hT[:, k, s0:s0 + n], t[:, 0:n], AF.Identity, t.b + mp.b, [hT.b[ti]],
                    bias=mp[:, b_off + k, j:j + 1], scale=1.0)

    win_v = [win_d[l].rearrange("(k p) f -> p k f", p=128) for l in range(depth)]

    def load_w(tl, l, col0, ncols, dst0=0):
        DMA("pool", tl[:, :, dst0:dst0 + ncols], win_v[l][:, :, col0:col0 + ncols], (), tl.b)

    def proj_tok(w, c0, ncols, tb, ti, ps=None):
        ps = ps or P32()
        for k in range(8):
            MM(ps[:, 0:ncols], hT[:, k, tb * 128:(tb + 1) * 128], w[:, k, c0:c0 + ncols], k == 0, k == 7,
               w.b + [hT.b[ti]], ps.b)
        return ps

    def proj_feat(w, c0, ti, ps=None):
        s0, n, j = TILES[ti]
        ps = ps or P32()
        for k in range(8):
            MM(ps[:, 0:n], w[:, k, c0:c0 + 128], hT[:, k, s0:s0 + n], k == 0, k == 7, w.b + [hT.b[ti]], ps.b)
        return ps

    def tile_of(tb):
        return 0 if tb < 2 else 1 + (tb - 2) // 4

    def transpose_to_yT(src, chunk, blocks):
        blocks = list(blocks)
        for i in range(0, len(blocks), 4):
            grp = blocks[i:i + 4]
            pb = PB()
            for jj, tb in enumerate(grp):
                TR(pb[:, jj * 128:(jj + 1) * 128], src[:, tb, :], ident_b, src.b + CB, pb.b)
            if grp[-1] - grp[0] == len(grp) - 1:
                CP("act", yT[:, chunk, grp[0] * 128:(grp[-1] + 1) * 128], pb[:, 0:len(grp) * 128], pb.b, yT.b)
            else:
                for jj, tb in enumerate(grp):
                    CP("act", yT[:, chunk, tb * 128:(tb + 1) * 128], pb[:, jj * 128:(jj + 1) * 128], pb.b, yT.b)

    def layer(l):
        nonlocal xT, yT
        last = l == depth - 1
        need_ctx = not last
        out_blocks = list(range(NB)) if need_ctx else list(range(2, NB))
        tiles_all = list(range(len(TILES)))
        mp = modp[l]
        DMA("sp", rowv[:], rowv_d[l].partition_broadcast(128), (), rowv.b)
        DMA("sp", convp[:], convp_d[l], (), convp.b)
        rv = lambda a, n: rowv[:, a:a + n]
        RV_BI, RV_BF, RV_MLG, RV_ALOG, RV_DTB, RV_DSK, RV_SSDG, RV_QG, RV_KG, RV_SINK = 0, 8, 16, 528, 544, 560, 568, 1080, 1144, 1208

        norm_to_h(l, 8, 0, tiles_all)
        if stop_after == 'norm1' and l == 0:
            raise _Stop()
        DMA("sp", xs_d, xT[:].rearrange("p k t -> p (k t)"), xT.b, ())
        S.fence()
        RW.reset()
        RX.reset()

        wg = RW.alloc("wg", [128, 8, 32], BF16)
        load_w(wg, l, OFF_MLI, 16, 0)
        load_w(wg, l, OFF_DT, 16, 16)
        GW = 24
        wt = RX.alloc("wt", [128, NB, GW], F32)
        wsp = RX.alloc("wsp", [128, NB, GW], F32)
        cbias = RX.alloc("cbias", [128, NB, GW], F32)
        dec = RX.alloc("dec", [128, NB, GW], F32)
        GTt = {k: RW.alloc(f"GT{k}", [72, 128], F32) for k in range(6)}
        GTh = {k: RX.alloc(f"GTh{k}", [72, 128], BF16) for k in range(6)}
        GTl = {k: RX.alloc(f"GTl{k}", [72, 128], BF16) for k in range(6)}
        gate_mark = RX.cur
        graw = RW.alloc("graw", [128, NB, 32], F32)
        for tb in range(NB):
            ps = proj_tok(wg, 0, 32, tb, tile_of(tb))
            CP("dve", graw[:, tb, :], ps[:, 0:32], ps.b, graw.b)
        lnb = RW.alloc("lnb", [128, NB, GW], F32)
        ldec = RW.alloc("ldec", [128, NB, GW], F32)
        fcs = RW.alloc("fcs", [128, NB, GW], F32)
        tot = RW.alloc("tot", [128, NB, GW], F32)
        gtmp = RW.alloc("gtmp", [128, NB, 16], F32)
        gtmp2 = RW.alloc("gtmp2", [128, NB, 16], F32)

        def bc(a, n):
            return rv(a, n).unsqueeze(1).to_broadcast([128, NB, n])

        TT("dve", lnb[:, :, 0:8], graw[:, :, 0:8], bc(RV_BI, 8), ALU.add, graw.b + rowv.b, lnb.b)
        TT("dve", gtmp[:, :, 0:8], graw[:, :, 8:16], bc(RV_BF, 8), ALU.add, graw.b + rowv.b, gtmp.b)
        ACT(gtmp[:, :, 0:8], gtmp[:, :, 0:8], AF.Exp, gtmp.b, gtmp.b, scale=-1.0)
        ACT(gtmp[:, :, 0:8], gtmp[:, :, 0:8], AF.Ln, gtmp.b, gtmp.b, bias=1.0, scale=1.0)
        TS("dve", ldec[:, :, 0:8], gtmp[:, :, 0:8], -1.0, ALU.mult, gtmp.b, ldec.b)
        TT("dve", gtmp[:], graw[:, :, 16:32], bc(RV_DTB, 16), ALU.add, graw.b + rowv.b, gtmp.b)
        STT(gtmp2[:], gtmp[:], -1.0, gtmp[:], ALU.mult, ALU.max, gtmp.b, gtmp2.b)
        ACT(gtmp2[:], gtmp2[:], AF.Exp, gtmp2.b, gtmp2.b, scale=-1.0)
        ACT(gtmp2[:], gtmp2[:], AF.Ln, gtmp2.b, gtmp2.b, bias=1.0, scale=1.0)
        STT(gtmp[:], gtmp[:], 0.0, gtmp2[:], ALU.max, ALU.add, gtmp.b + gtmp2.b, gtmp.b)
        ACT(lnb[:, :, 8:24], gtmp[:], AF.Ln, gtmp.b, lnb.b)
        aexp = RW.alloc("aexp", [128, 16], F32)
        ACT(aexp[:], rv(RV_ALOG, 16), AF.Exp, rowv.b, aexp.b)
        TT("dve", gtmp2[:], gtmp[:], aexp[:].unsqueeze(1).to_broadcast([128, NB, 16]), ALU.mult, gtmp.b + aexp.b, gtmp2.b)
        TS("dve", ldec[:, :, 8:24], gtmp2[:], -1.0, ALU.mult, gtmp2.b, ldec.b)
        colsets = [(0, 4, 0), (4, 8, 1), (8, 16, 0), (16, 24, 1)]
        for (c0, c1, d) in colsets:
            ps = P32()
            nn = (c1 - c0) * NB
            MM(ps[:, 0:nn].rearrange("p (b c) -> p b c", b=NB), triR if d else triF, ldec[:, :, c0:c1], True, True, ldec.b + CB, ps.b)
            CP("dve", fcs[:, :, c0:c1], ps[:, 0:nn].rearrange("p (b c) -> p b c", b=NB), ps.b, fcs.b)
        ps = P32()
        MM(ps[:, 0:NB * GW], ones_f, ldec[:].rearrange("p b c -> p (b c)"), True, True, ldec.b + CB, ps.b)
        CP("dve", tot[:].rearrange("p b c -> p (b c)"), ps[:, 0:NB * GW], ps.b, tot.b)
        ACT(wt[:], fcs[:], AF.Exp, fcs.b, wt.b)
        ACT(dec[:], tot[:], AF.Exp, tot.b, dec.b)
        TT("dve", cbias[:], lnb[:], fcs[:], ALU.subtract, lnb.b + fcs.b, cbias.b)
        TT("dve", wsp[:], cbias[:], tot[:], ALU.add, cbias.b + tot.b, wsp.b)
        ACT(wsp[:], wsp[:], AF.Exp, wsp.b, wsp.b)
        fd = RW.alloc("fd", [128, NB, 4], F32)
        GT = {}
        gsets = [("m", 0, 0, 0), ("m", 1, 0, 4), ("s", 0, 0, 8), ("s", 0, 1, 12), ("s", 1, 0, 16), ("s", 1, 1, 20)]
        for gi, (fam, d, g, c0) in enumerate(gsets):
            CP("dve", fd[:], fcs[:, :, c0:c0 + 4], fcs.b, fd.b)
            ps = P32()
            TR(ps[0:72, 0:128], fd[:].rearrange("p b c -> p (b c)"), ident_f, fd.b + CB, ps.b)
            gt = GTt[gi]
            CP("dve", gt[:], ps[0:72, 0:128], ps.b, gt.b)
            CP("act", GTh[gi][:], gt[:], gt.b, GTh[gi].b)
            TT("dve", GTl[gi][:], gt[:], GTh[gi][:], ALU.subtract, gt.b + GTh[gi].b, GTl[gi].b)
            GT[(fam, d, g)] = (GTh[gi], GTl[gi])
        dump("cbias", cbias, [128, NB, GW]); dump("wt", wt, [128, NB, GW]); dump("wsp", wsp, [128, NB, GW])
        S.fence()
        RW.reset()

        def scan_order(d):
            return list(range(NB)) if d == 0 else [1, 0] + list(range(NB - 1, 1, -1))

        def dt_tile(gt, r, d, tb, col, dst, ps=None, c0=0):
            ps = ps or PD()
            gh, gl = gt
            sel = ident_b[0:72, r:r + 1].to_broadcast([72, 128])
            MM(ps[:, c0:c0 + 128], sel, gh[:, :], True, False, gh.b + CB, ps.b)
            MM(ps[:, c0:c0 + 128], sel, gl[:, :], False, False, gl.b + CB, ps.b)
            MM(ps[:, c0:c0 + 128], ident_b, mnegb[:, d * 128:(d + 1) * 128], False, True, CB, ps.b)
            ACT(dst[:, :], ps[:, c0:c0 + 128], AF.Exp, ps.b + cbias.b, dst.b, bias=cbias[:, tb, col:col + 1], scale=1.0)

        if stop_after == 'gates' and l == 0:
            raise _Stop()
        wq = [RW.alloc(f"wq{i}", [128, 8, 512], BF16) for i in range(2)]
        hbuf = []
        for si in range(2):
            if si == 0:
                al = lambda nm, shp, dt: RX.alloc(nm, shp, dt)
            else:
                yoff = [4 * NT * 2]

                def al(nm, shp, dt):
                    nbytes = (int(np.prod(shp[1:])) * (4 if dt == F32 else 2) + 31) // 32 * 32
                    t_ = RY.alias(nm, shp, dt, yoff[0], [Buf(nm)])
                    yoff[0] += nbytes
                    assert yoff[0] <= 12 * NT * 2
                    return t_
            hbuf.append([al(f"qT{si}", [128, NT], BF16), al(f"kT{si}", [128, NT], BF16),
                         al(f"ktok{si}", [128, NB, 128], BF16), al(f"vaug{si}", [128, NB, 130], BF16),
                         al(f"osig{si}", [128, NB, 128], F32)])
        hacc = RX.alloc("hacc", [128, NB, 128], F32, nb=NB)
        C32c = [RX.alloc(f"C32{i}", [128, 132], F32) for i in range(2)]
        Cbc = [[RX.alloc(f"Cb{i}{j}", [128, 132], BF16) for j in range(2)] for i in range(2)]
        DTs = [RX.alloc(f"DTs{i}", [128, 128], F32) for i in range(2)]
        PTs = [RX.alloc(f"PTs{i}", [128, 128], BF16) for i in range(2)]
        kws = [RX.alloc(f"kws{i}", [128, 128], BF16) for i in range(2)]
        tA = [RX.alloc(f"tA{i}", [128, 132], F32) for i in range(2)]
        tB = [RX.alloc(f"tB{i}", [128, 132], F32) for i in range(2)]
        dn = [RX.alloc(f"dn{i}", [128, 2], F32) for i in range(2)]
        mout = RX.alloc("mout", [128, NB, 128], BF16)
        ssq = RX.alloc("ssq", [128, NB], F32)
        sqt = RX.alloc("sqt", [128, NB, 128], F32)
        for si in range(2):
            MSET("dve", hbuf[si][3][:, :, 128:130], 1.0, hbuf[si][3].b)
        kscale = 128.0 ** -0.5
        orders = [scan_order(0), scan_order(1)]
        hset = set()

        def ml_proj(h):
            qT, kT, ktok, vaug, osig = hbuf[h % 2]
            w = wq[h % 2]
            for gq in range(4):
                load_w(w, l, gq * 512 + h * 128, 128, gq * 128)
            pc = 0
            for ti in tiles_all:
                s0, n, j = TILES[ti]
                ps = proj_feat(w, 0, ti, ps=ps32[4 + pc % 2]); pc += 1
                CP("act", qT[:, s0:s0 + n], ps[:, 0:n], ps.b, qT.b)
                yield
                ps = proj_feat(w, 128, ti, ps=ps32[4 + pc % 2]); pc += 1
                ACT(kT[:, s0:s0 + n], ps[:, 0:n], AF.Copy, ps.b, kT.b, scale=kscale)
                yield
            for tb in range(NB):
                ps = proj_tok(w, 128, 384, tb, tile_of(tb), ps=ps32[4 + pc % 2]); pc += 1
                ACT(ktok[:, tb, :], ps[:, 0:128], AF.Copy, ps.b, ktok.b, scale=kscale)
                CP("dve", vaug[:, tb, 0:128], ps[:, 128:256], ps.b, vaug.b)
                ACT(osig[:, tb, :], ps[:, 256:384], AF.Sigmoid, ps.b, osig.b)
                yield

        def ml_chain(d, h, hb_):
            for it in range(NB):
                tb = orders[d][it]
                gt = GT[("m", d, 0)]
                col = d * 4 + h
                blk = slice(tb * 128, (tb + 1) * 128)
                want = tb in out_blocks
                bank1, bank2 = ps32[2 * d], ps32[2 * d + 1]
                qT, kT, ktok, vaug = hb_[0], hb_[1], hb_[2], hb_[3]
                S_, B_ = bank1[:, 0:128], bank1[:, 128:258]
                A_, C_ = bank2[:, 0:130], bank2[:, 130:260]
                cb_cur, cb_nxt = Cbc[d][it % 2], Cbc[d][(it + 1) % 2]
                if want:
                    MM(S_, kT[:, blk], qT[:, blk], True, True, kT.b + qT.b, bank1.b)
                    yield
                    MM(B_, qT[:, blk], cb_cur[:, 0:130], True, True, qT.b + cb_cur.b, bank1.b)
                    yield
                if it < NB - 1:
                    ACT(kws[d][:], ktok[:, tb, :], AF.Identity, ktok.b + wsp.b, kws[d].b, scale=wsp[:, tb, col:col + 1])
                    yield
                    MM(C_, kws[d][:], vaug[:, tb, 0:130], True, True, kws[d].b + vaug.b, bank2.b)
                    yield
                    STT(C32c[d][:, 0:130], C32c[d][:, 0:130], dec[:, tb, col:col + 1], C_, ALU.mult, ALU.add,
                        C32c[d].b + dec.b + bank2.b, C32c[d].b)
                    yield
                    CP("act", cb_nxt[:, 0:130], C32c[d][:, 0:130], C32c[d].b, cb_nxt.b)
                    yield
                if want:
                    dt_tile(gt, tb * 4 + h, d, tb, col, DTs[d], ps=bank1, c0=260)
                    yield
                    TT("dve", PTs[d][:], S_, DTs[d][:], ALU.mult, bank1.b + DTs[d].b, PTs[d].b)
                    yield
                    MM(A_, PTs[d][:], vaug[:, tb, 0:130], True, True, PTs[d].b + vaug.b, bank2.b)
                    yield
                    ACT(tB[d][:, 0:130], B_, AF.Identity, bank1.b + wt.b, tB[d].b, scale=wt[:, tb, col:col + 1])
                    yield
                    TT("dve", tA[d][:, 0:130], A_, tB[d][:, 0:130], ALU.add, bank2.b + tB[d].b, tA[d].b)
                    yield
                    TS("dve", dn[d][:, 0:1], tA[d][:, 128:129], 1.0, ALU.max, tA[d].b, dn[d].b)
                    yield
                    STT(dn[d][:, 0:1], tA[d][:, 128:129], -1.0, dn[d][:, 0:1], ALU.mult, ALU.max, tA[d].b + dn[d].b, dn[d].b)
                    yield
                    RECIP(dn[d][:, 1:2], dn[d][:, 0:1], dn[d].b, dn[d].b)
                    yield
                    hb = [hacc.b[tb]]
                    if tb not in hset:
                        hset.add(tb)
                        TS("dve", hacc[:, tb, :], tA[d][:, 0:128], dn[d][:, 1:2], ALU.mult, tA[d].b + dn[d].b, hb)
                        yield
                    else:
                        STT(hacc[:, tb, :], tA[d][:, 0:128], dn[d][:, 1:2], hacc[:, tb, :], ALU.mult, ALU.add,
                            tA[d].b + dn[d].b + hb, hb)
                        yield


        def run_rr(gens):
            gens = list(gens)
            while gens:
                for g_ in list(gens):
                    try:
                        next(g_)
                    except StopIteration:
                        gens.remove(g_)

        run_rr([ml_proj(0)])
        for h in range(4):
            osig = hbuf[h % 2][4]
            for d in range(2):
                MSET("dve", C32c[d][:], 0.0, C32c[d].b)
                MSET("dve", Cbc[d][0][:], 0.0, Cbc[d][0].b)
            hset.clear()
            gl_ = [ml_chain(0, h, hbuf[h % 2]), ml_chain(1, h, hbuf[h % 2])]
            if h + 1 < 4:
                gl_.append(ml_proj(h + 1))
            run_rr(gl_)
            b0 = out_blocks[0]
            nbk = len(out_blocks)
            hv = hacc[:, b0:NB, :]
            TT("dve", osig[:, b0:NB, :], osig[:, b0:NB, :],
               rv(RV_MLG + h * 128, 128).unsqueeze(1).to_broadcast([128, nbk, 128]), ALU.mult, osig.b + rowv.b, osig.b)
            TT("dve", sqt[:, b0:NB, :], hv, hv, ALU.mult, hacc.b, sqt.b)
            RED(ssq[:, b0:NB], sqt[:, b0:NB, :], ALU.add, sqt.b, ssq.b)
            ACT(ssq[:, b0:NB], ssq[:, b0:NB], AF.Ln, ssq.b, ssq.b, bias=EPS, scale=1.0 / 128)
            ACT(ssq[:, b0:NB], ssq[:, b0:NB], AF.Exp, ssq.b, ssq.b, scale=-0.5)
            TT("dve", hv, hv, ssq[:, b0:NB].unsqueeze(2).to_broadcast([128, nbk, 128]), ALU.mult, hacc.b + ssq.b, hacc.b)
            TT("dve", mout[:, b0:NB, :], hv, osig[:, b0:NB, :], ALU.mult, hacc.b + osig.b, mout.b)
            transpose_to_yT(mout, h, out_blocks)
        S.fence()
        RX.cur = gate_mark
        RW.reset()

        if stop_after == 'mlstm' and l == 0:
            raise _Stop()
        wz = [RW.alloc(f"wz{i}", [128, 8, 256], BF16) for i in range(2)]
        wx = [RW.alloc(f"wx{i}", [128, 8, 256], BF16) for i in range(2)]
        wbc = RW.alloc("wbc", [128, 8, 256], BF16)
        DTs = [RW.alloc(f"DTs{i}", [128, 128], F32) for i in range(8)]
        PTs = [RW.alloc(f"PTs{i}", [128, 128], BF16) for i in range(8)]
        BT = RX.alloc("BT", [128, NT], BF16)
        CT = RX.alloc("CT", [128, NT], BF16)
        Btok = RX.alloc("Btok", [128, NB, 128], BF16)
        raw0_off = RX.cur
        raw = [RX.alloc(f"raw{i}", [128, NT], F32) for i in range(2)]
        raw[1].b = raw[0].b
        zs = RX.alias("zs", [128, NB, 256], BF16, raw0_off, raw[0].b)
        gg = RX.alias("gg", [128, 512], F32, raw0_off + 9216, raw[0].b)
        sout = RX.alias("sout", [128, NB, 128], BF16, raw0_off + 9216 + 2048, raw[0].b)
        xcT = RX.alloc("xcT", [128, NT], BF16)
        yacc = RX.alloc("yacc", [128, NB, 256], F32, nb=NB)
        H32c = [RX.alloc(f"H32{i}", [128, 256], F32) for i in range(2)]
        Hbc = [[RX.alloc(f"Hb{i}{j}", [128, 256], BF16) for j in range(2)] for i in range(2)]
        BwT = [RX.alloc(f"BwT{i}", [128, 128], BF16) for i in range(8)]
        ytmp = [RX.alloc(f"ytmp{i}", [128, 256], F32) for i in range(2)]
        ssq2 = RX.alloc("ssq2", [128, NB, 2], F32)
        rstd = RX.alloc("rstd", [128, NB], F32)
        G = RY.alias("G", [128, NB, 512], BF16, 8 * NT * 2, yT.b)
        xtok = RY.alias("xtok", [128, NB, 256], BF16, 4 * NT * 2, yT.b)

        def conv_silu(ps_src_fn, chunk, dst):
            r = raw[chunk % 2]
            for ti in tiles_all:
                s0, n, j = TILES[ti]
                ps = ps_src_fn(ti)
                CP("dve", r[:, s0:s0 + n], ps[:, 0:n], ps.b, r.b)
            acc = raw[(chunk + 1) % 2]
            cw = lambda i: convp[:, chunk * 4 + i:chunk * 4 + i + 1]
            ACT(acc[:, :], r[:, :], AF.Identity, r.b + convp.b, acc.b, bias=cw(3), scale=cw(1))
            for (a, b) in ((0, CTX), (CTX, NT)):
                STT(acc[:, a + 1:b], r[:, a:b - 1], cw(0), acc[:, a + 1:b], ALU.mult, ALU.add, r.b + acc.b + convp.b, acc.b)
                STT(acc[:, a:b - 1], r[:, a + 1:b], cw(2), acc[:, a:b - 1], ALU.mult, ALU.add, r.b + acc.b + convp.b, acc.b)
            ACT(dst[:, :], acc[:, :], AF.Silu, acc.b, dst.b)

        load_w(wbc, l, OFF_XBC + 512, 256)
        conv_silu(lambda ti: proj_feat(wbc, 0, ti), 4, BT)
        conv_silu(lambda ti: proj_feat(wbc, 128, ti), 5, CT)
        for i in range(0, NB, 4):
            pb = PB()
            for jj in range(4):
                tb = i + jj
                if tb < NB:
                    TR(pb[:, jj * 128:(jj + 1) * 128], BT[:, tb * 128:(tb + 1) * 128], ident_b, BT.b + CB, pb.b)
            nn = min(4, NB - i)
            CP("act", Btok[:, i:i + nn, :], pb[:, 0:nn * 128].rearrange("p (a c) -> p a c", a=nn), pb.b, Btok.b)
        for i in range(8):
            MSET("pool", BwT[i][:], 0.0, BwT[i].b)
        for g in range(2):
            load_w(wz[g], l, OFF_Z + g * 256, 256)
            load_w(wx[g], l, OFF_XBC + g * 256, 256)
            for cj in range(2):
                conv_silu(lambda ti, cj=cj: proj_feat(wx[g], cj * 128, ti), g * 2 + cj, xcT)
                for i in range(0, NB, 4):
                    pb = PB()
                    nn = min(4, NB - i)
                    for jj in range(nn):
                        tb = i + jj
                        TR(pb[:, jj * 128:(jj + 1) * 128], xcT[:, tb * 128:(tb + 1) * 128], ident_b, xcT.b + CB, pb.b)
                    CP("act", xtok[:, i:i + nn, cj * 128:(cj + 1) * 128],
                       pb[:, 0:nn * 128].rearrange("p (a c) -> p a c", a=nn), pb.b, xtok.b)
            for tb in range(NB):
                ps = proj_tok(wz[g], 0, 256, tb, tile_of(tb))
                ACT(zs[:, tb, :], ps[:, 0:256], AF.Silu, ps.b, zs.b)
            rows = slice(g * 64, (g + 1) * 64)
            orders = [scan_order(0), scan_order(1)]
            for d in range(2):
                MSET("dve", H32c[d][:], 0.0, H32c[d].b)
                MSET("pool", Hbc[d][0][:], 0.0, Hbc[d][0].b)
            yset = set()

            def ssd_chain(d):
                for it in range(NB):
                    tb = orders[d][it]
                    gt = GT[("s", d, g)]
                    c0 = 8 + d * 8 + g * 4
                    blk = slice(tb * 128, (tb + 1) * 128)
                    want = tb in out_blocks
                    lastit = it == NB - 1
                    bank1, bank2, bankD = ps32[2 * d], ps32[2 * d + 1], ps32[4 + d]
                    S_, B_ = bank1[:, 0:128], bank1[:, 128:384]
                    A_, C_ = bank2[:, 0:256], bank2[:, 256:512]
                    hb_cur, hb_nxt = Hbc[d][it % 2], Hbc[d][(it + 1) % 2]
                    H32 = H32c[d]
                    if want:
                        MM(S_, BT[rows, blk], CT[rows, blk], True, True, BT.b + CT.b, bank1.b)
                        yield
                        MM(B_, CT[rows, blk], hb_cur[rows, :], True, True, CT.b + hb_cur.b, bank1.b)
                        yield
                    if not lastit:
                        for hh in range(4):
                            col = c0 + hh
                            bw = BwT[d * 4 + hh]
                            ACT(bw[:, rows], Btok[:, tb, rows], AF.Identity, Btok.b + wsp.b, bw.b, scale=wsp[:, tb, col:col + 1])
                            yield
                            MM(bank2[:, 256 + hh * 64:256 + (hh + 1) * 64], bw[:], xtok[:, tb, hh * 64:(hh + 1) * 64], True, True,
                               bw.b + xtok.b, bank2.b)
                            yield
                        TT("dve", H32[rows, :].rearrange("p (a c) -> p a c", a=4), H32[rows, :].rearrange("p (a c) -> p a c", a=4),
                           dec[rows, tb, c0:c0 + 4].unsqueeze(2).to_broadcast([64, 4, 64]), ALU.mult, H32.b + dec.b, H32.b)
                        yield
                        TT("dve", H32[rows, :], H32[rows, :], bank2[rows, 256:512], ALU.add, H32.b + bank2.b, H32.b)
                        yield
                        CP("act", hb_nxt[rows, :], H32[rows, :], H32.b, hb_nxt.b)
                        yield
                    if want:
                        gh, gl = gt
                        for hh in range(4):
                            sel = ident_b[0:72, tb * 4 + hh:tb * 4 + hh + 1].to_broadcast([72, 128])
                            o_ = bankD[:, hh * 128:(hh + 1) * 128]
                            MM(o_, sel, gh[:, :], True, False, gh.b + CB, bankD.b)
                            MM(o_, sel, gl[:, :], False, False, gl.b + CB, bankD.b)
                            MM(o_, ident_b, mnegb[:, d * 128:(d + 1) * 128], False, True, CB, bankD.b)
                            yield
                        for hh in range(4):
                            col = c0 + hh
                            i2 = d * 4 + hh
                            ACT(DTs[i2][:, :], bankD[:, hh * 128:(hh + 1) * 128], AF.Exp, bankD.b + cbias.b, DTs[i2].b,
                                bias=cbias[:, tb, col:col + 1], scale=1.0)
                            yield
                        for hh in range(4):
                            i2 = d * 4 + hh
                            TT("dve", PTs[i2][:], S_, DTs[i2][:], ALU.mult, bank1.b + DTs[i2].b, PTs[i2].b)
                            yield
                        for hh in range(4):
                            i2 = d * 4 + hh
                            MM(bank2[:, hh * 64:(hh + 1) * 64], PTs[i2][:], xtok[:, tb, hh * 64:(hh + 1) * 64], True, True,
                               PTs[i2].b + xtok.b, bank2.b)
                            yield
                        yt = ytmp[d]
                        TT("dve", yt[:].rearrange("p (a c) -> p a c", a=4), B_.rearrange("p (a c) -> p a c", a=4),
                           wt[:, tb, c0:c0 + 4].unsqueeze(2).to_broadcast([128, 4, 64]), ALU.mult, bank1.b + wt.b, yt.b)
                        yield
                        yb = [yacc.b[tb]]
                        if tb not in yset:
                            yset.add(tb)
                            TT("dve", yacc[:, tb, :], A_, yt[:], ALU.add, bank2.b + yt.b, yb)
                            yield
                        else:
                            TT("dve", yt[:], A_, yt[:], ALU.add, bank2.b + yt.b, yt.b)
                            yield
                            TT("pool", yacc[:, tb, :], yacc[:, tb, :], yt[:], ALU.add, yb + yt.b, yb)
                            yield

            gens = [ssd_chain(0), ssd_chain(1)]
            while gens:
                for g_ in list(gens):
                    try:
                        next(g_)
                    except StopIteration:
                        gens.remove(g_)
            b0 = out_blocks[0]
            nbk = len(out_blocks)
            yv = yacc[:, b0:NB, :]
            for tb in out_blocks:
                yt = ytmp[tb % 2]
                TT("pool", yt[:].rearrange("p (a c) -> p a c", a=4), xtok[:, tb, :].rearrange("p (a c) -> p a c", a=4),
                   rv(RV_DSK + g * 4, 4).unsqueeze(2).to_broadcast([128, 4, 64]), ALU.mult, xtok.b + rowv.b, yt.b)
                TT("dve", yacc[:, tb, :], yacc[:, tb, :], yt[:], ALU.add, yacc.b + yt.b, yacc.b)
                TT("dve", yacc[:, tb, :], yacc[:, tb, :], zs[:, tb, :], ALU.mult, yacc.b + zs.b, yacc.b)
                CP("pool", G[:, tb, g * 256:(g + 1) * 256], yacc[:, tb, :], yacc.b, G.b)
                ACT(yt[:], yacc[:, tb, :], AF.Square, yacc.b, yt.b)
                RED(ssq2[:, tb, g:g + 1], yt[:], ALU.add, yt.b, ssq2.b)
        b0 = out_blocks[0]
        nbk = len(out_blocks)
        TT("dve", rstd[:, b0:NB], ssq2[:, b0:NB, 0], ssq2[:, b0:NB, 1], ALU.add, ssq2.b, rstd.b)
        ACT(rstd[:, b0:NB], rstd[:, b0:NB], AF.Ln, rstd.b, rstd.b, bias=EPS, scale=1.0 / 512)
        ACT(rstd[:, b0:NB], rstd[:, b0:NB], AF.Exp, rstd.b, rstd.b, scale=-0.5)
        for tb in out_blocks:
            TS("dve", gg[:], G[:, tb, :], rstd[:, tb:tb + 1], ALU.mult, G.b + rstd.b, gg.b)
            TT("pool", G[:, tb, :], gg[:], rv(RV_SSDG, 512), ALU.mult, gg.b + rowv.b, G.b)
        for cch in range(4):
            for tb in out_blocks:
                CP("pool", sout[:, tb, :], G[:, tb, cch * 128:(cch + 1) * 128], G.b, sout.b)
            transpose_to_yT(sout, 4 + cch, out_blocks)
        S.fence()
        RX.cur = gate_mark
        RW.reset()

        if stop_after == 'ssd' and l == 0:
            raise _Stop()
        wa = RW.alloc("wa", [128, 8, 768], BF16)
        load_w(wa, l, OFF_AQ, 768)
        qTa = RX.alloc("qTa", [128, 4, NT], BF16)
        kTa = RX.alloc("kTa", [128, 2, NT], BF16)
        va = RX.alloc("va", [128, NB, 2, 66], BF16)
        qraw = [RX.alloc(f"qraw{i}", [128, 640], F32) for i in range(2)]
        qsq = [RX.alloc(f"qsq{i}", [128, 640], F32) for i in range(2)]
        qn = [RX.alloc(f"qn{i}", [128, 640], F32) for i in range(2)]
        qr = [RX.alloc(f"qr{i}", [128, 640], BF16) for i in range(2)]
        kd = [RX.alloc(f"kd{i}", [128, 2, 128], BF16) for i in range(2)]
        rq = [RX.alloc(f"rq{i}", [128, 10], F32) for i in range(2)]
        t1 = [RX.alloc(f"t1{i}", [128, 10, 2, 16], F32) for i in range(2)]
        t2 = [RX.alloc(f"t2{i}", [128, 10, 2, 16], F32) for i in range(2)]
        PTa = [RW.alloc(f"PTa{i}", [128, 512], BF16) for i in range(10)]
        otok = [RX.alloc(f"otok{i}", [128, 512], BF16) for i in range(2)]
        dna = RX.alloc("dna", [128, 16], F32)
        esink = RX.alloc("esink", [128, 8], F32)
        gqk = RX.alloc("gqk", [128, 640], F32)
        MSET("pool", va[:, :, :, 64:66], 1.0, va.b)
        ACT(esink[:], rv(RV_SINK, 8), AF.Exp, rowv.b, esink.b)
        for hh in range(8):
            CP("dve", gqk[:, hh * 64:(hh + 1) * 64], rv(RV_QG, 64), rowv.b, gqk.b)
        for hh in range(2):
            TS("dve", gqk[:, 512 + hh * 64:512 + (hh + 1) * 64], rv(RV_KG, 64), 8.0, ALU.mult, rowv.b, gqk.b)
        def att_prep(tb):
            i2 = tb % 2
            ps = proj_tok(wa, 0, 512, tb, tile_of(tb), ps=ps32[2 * i2])
            yield
            CP("act", qraw[i2][:, 0:512], ps[:, 0:512], ps.b, qraw[i2].b)
            yield
            ps = proj_tok(wa, 512, 256, tb, tile_of(tb), ps=ps32[2 * i2 + 1])
            yield
            CP("act", qraw[i2][:, 512:640], ps[:, 0:128], ps.b, qraw[i2].b)
            yield
            CP("dve", va[:, tb, :, 0:64], ps[:, 128:256].rearrange("p (g c) -> p g c", g=2), ps.b, va.b)
            yield
            q3 = qraw[i2][:].rearrange("p (h c) -> p h c", h=10)
            TT("dve", qsq[i2][:], qraw[i2][:], qraw[i2][:], ALU.mult, qraw[i2].b, qsq[i2].b)
            yield
            RED(rq[i2][:], qsq[i2][:].rearrange("p (h c) -> p h c", h=10), ALU.add, qsq[i2].b, rq[i2].b)
            yield
            ACT(rq[i2][:], rq[i2][:], AF.Ln, rq[i2].b, rq[i2].b, bias=64 * EPS, scale=1.0)
            yield
            ACT(rq[i2][:], rq[i2][:], AF.Exp, rq[i2].b, rq[i2].b, scale=-0.5)
            yield
            qn3 = qn[i2][:].rearrange("p (h c) -> p h c", h=10)
            TT("dve", qn3, q3, rq[i2][:].unsqueeze(2).to_broadcast([128, 10, 64]), ALU.mult, qraw[i2].b + rq[i2].b, qn[i2].b)
            yield
            if tb >= 2:
                TT("pool", qn[i2][:], qn[i2][:], gqk[:], ALU.mult, qn[i2].b + gqk.b, qn[i2].b)
                yield
                q5 = qn[i2][:].rearrange("p (h a b i) -> p h a b i", h=10, a=2, b=2)
                o5 = qr[i2][:].rearrange("p (h a b i) -> p h a b i", h=10, a=2, b=2)
                cosb = rcos[:, tb - 2, :, :].unsqueeze(1).to_broadcast([128, 10, 2, 16])
                sinb = rsin[:, tb - 2, :, :].unsqueeze(1).to_broadcast([128, 10, 2, 16])
                x1, x2 = q5[:, :, :, 0, :], q5[:, :, :, 1, :]
                TT("dve", t1[i2][:], x1, cosb, ALU.mult, qn[i2].b + rope.b, t1[i2].b)
                yield
                TT("pool", t2[i2][:], x2, sinb, ALU.mult, qn[i2].b + rope.b, t2[i2].b)
                yield
                TT("dve", o5[:, :, :, 0, :], t1[i2][:], t2[i2][:], ALU.subtract, t1[i2].b + t2[i2].b, qr[i2].b)
                yield
                TT("dve", t1[i2][:], x2, cosb, ALU.mult, qn[i2].b + rope.b, t1[i2].b)
                yield
                TT("pool", t2[i2][:], x1, sinb, ALU.mult, qn[i2].b + rope.b, t2[i2].b)
                yield
                TT("dve", o5[:, :, :, 1, :], t1[i2][:], t2[i2][:], ALU.add, t1[i2].b + t2[i2].b, qr[i2].b)
                yield
            else:
                TT("pool", qr[i2][:], qn[i2][:], gqk[:], ALU.mult, qn[i2].b + gqk.b, qr[i2].b)
                yield
            for g in range(2):
                for hf in range(2):
                    CP("pool", kd[i2][:, g, hf * 64:(hf + 1) * 64], qr[i2][:, 512 + g * 64:512 + (g + 1) * 64], qr[i2].b, kd[i2].b)
                    yield
            pb = psb[i2]
            for a in range(4):
                TR(pb[:, a * 128:(a + 1) * 128], qr[i2][:, a * 128:(a + 1) * 128], ident_b, qr[i2].b + CB, pb.b)
                yield
            CP("act", qTa[:, :, tb * 128:(tb + 1) * 128], pb[:, 0:512].rearrange("p (a t) -> p a t", a=4), pb.b, qTa.b)
            yield
            pb = psb[i2]
            for g in range(2):
                TR(pb[:, g * 128:(g + 1) * 128], kd[i2][:, g, :], ident_b, kd[i2].b + CB, pb.b)
                yield
            CP("act", kTa[:, :, tb * 128:(tb + 1) * 128], pb[:, 0:256].rearrange("p (a t) -> p a t", a=2), pb.b, kTa.b)
            yield

        pend, active = list(range(NB)), []
        while pend or active:
            while len(active) < 2 and pend:
                active.append(att_prep(pend.pop(0)))
            for g_ in list(active):
                try:
                    next(g_)
                except StopIteration:
                    active.remove(g_)
        dump("qTa", qTa, [128, 4, NT], BF16); dump("kTa", kTa, [128, 2, NT], BF16)
        pi = 0
        for qi, tb in enumerate(out_blocks):
            keys = [(0, None), (1, None)]
            if tb >= 2:
                if tb - 1 >= 2:
                    keys.append((tb - 1, 0))
                keys.append((tb, None))
                if tb + 1 < NB:
                    keys.append((tb + 1, 1))
            psO = [ps32[0], ps32[1]]
            for ki, (kb, mk) in enumerate(keys):
                for g in range(2):
                    pt = PTa[ki * 2 + g]
                    for hf in range(2):
                        psS = ps32[2 + hf + 2 * (pi % 2)]
                        prow = slice(hf * 64, (hf + 1) * 64)
                        MM(psS[:, 0:256].rearrange("p (a t) -> p a t", a=2),
                           kTa[prow, g, kb * 128:(kb + 1) * 128], qTa[prow, 2 * g:2 * g + 2, tb * 128:(tb + 1) * 128],
                           True, True, kTa.b + qTa.b, psS.b)
                        ACT(pt[:, hf * 256:(hf + 1) * 256], psS[:, 0:256], AF.Exp, psS.b, pt.b)
                    pi += 1
                    if mk is not None:
                        TT("pool", pt[:], pt[:], m01[:, mk * 512:(mk + 1) * 512], ALU.mult, pt.b + m01.b, pt.b)
            for g in range(2):
                for hf in range(2):
                    for al in range(2):
                        cb_ = hf * 2 + al
                        hl = 2 * al + hf
                        for ki, (kb, mk) in enumerate(keys):
                            pt = PTa[ki * 2 + g]
                            MM(psO[g][:, hl * 66:(hl + 1) * 66], pt[:, cb_ * 128:(cb_ + 1) * 128], va[:, kb, g, 0:66],
                               ki == 0, ki == len(keys) - 1, pt.b + va.b, psO[g].b)
            ot = otok[qi % 2]
            for g in range(2):
                o4 = psO[g][:, 0:264].rearrange("p (h c) -> p h c", h=4)
                TT("dve", dna[:, g * 4:(g + 1) * 4], o4[:, :, 64], esink[:, g * 4:(g + 1) * 4], ALU.add, psO[g].b + esink.b, dna.b)
                RECIP(dna[:, 8 + g * 4:8 + (g + 1) * 4], dna[:, g * 4:(g + 1) * 4], dna.b, dna.b)
                TT("dve", ot[:, g * 256:(g + 1) * 256].rearrange("p (h c) -> p h c", h=4), o4[:, :, 0:64],
                   dna[:, 8 + g * 4:8 + (g + 1) * 4].unsqueeze(2).to_broadcast([128, 4, 64]), ALU.mult, psO[g].b + dna.b, ot.b)
            pb = PB()
            for cch in range(4):
                TR(pb[:, cch * 128:(cch + 1) * 128], ot[:, cch * 128:(cch + 1) * 128], ident_b, ot.b + CB, pb.b)
            CP("act", yT[:, 8:12, tb * 128:(tb + 1) * 128], pb[:, 0:512].rearrange("p (a t) -> p a t", a=4), pb.b, yT.b)
        dump("yT", yT, [128, 12, NT], BF16)
        S.fence()
        RX.reset()
        RW.reset()

        if stop_after == 'attn' and l == 0:
            raise _Stop()
        xT = RX.alloc("xT", [128, 8, NT], F32, nb=1)
        DMA("sp", xT[:].rearrange("p k t -> p (k t)"), xs_d, (), xT.b)
        tiles_out = tiles_all if need_ctx else tiles_all[1:]
        wo = RW.alloc("wo", [128, 12, 1024], BF16, nb=2)
        for hf in range(2):
            DMA("pool", wo[:, :, hf * 512:(hf + 1) * 512],
                wout_d[l].rearrange("(m p) f -> p m f", p=128)[:, :, hf * 512:(hf + 1) * 512], (), [wo.b[hf]])
        for ti in tiles_out:
            s0, n, j = TILES[ti]
            for c in range(8):
                ps = P32()
                for m in range(12):
                    MM(ps[:, 0:n], wo[:, m, c * 128:(c + 1) * 128], yT[:, m, s0:s0 + n], m == 0, m == 11, [wo.b[c // 4]] + yT.b, ps.b)
                STT(xT[:, c, s0:s0 + n], ps[:, 0:n], mp[:, 16 + c, j:j + 1], xT[:, c, s0:s0 + n], ALU.mult, ALU.add,
                    ps.b + mp.b + xT.b, xT.b)
        dump("x1", xT, [128, 8, NT])
        S.fence()
        RW.reset()
        RY.reset()
        if stop_after == 'outproj' and l == 0:
            raise _Stop()
        norm_to_h(l, 32, 24, tiles_out)
        wu = [RY.alloc(f"wu{i}", [128, 8, 512], BF16) for i in range(2)]
        wd = [RY.alloc(f"wd{i}", [128, 4, 1024], BF16) for i in range(2)]
        act = [RY.alloc(f"act{i}", [128, 4, 512], BF16) for i in range(2)]
        rl = [RY.alloc(f"rl{i}", [128, 512], F32) for i in range(2)]
        wup_v = wup_d[l].rearrange("(k p) f -> p k f", p=128)
        wdn_v = wdn_d[l].rearrange("(m p) f -> p m f", p=128)
        cnt = 0
        mg = None
        if l + 1 < depth:
            bm1 = RW.alloc("bm", [128, 48], F32)
            g121 = RW.alloc("g12", [128, 16], F32)
            wm1 = [RW.alloc("wm1", [128, 8, 512], BF16), RY.alloc("wm1b", [128, 8, 512], BF16)]
            mg = mod_gen(l + 1, wm1, bm1, g121)

        def load_mlp(sl):
            DMA("pool", wu[sl % 2][:], wup_v[:, :, sl * 512:(sl + 1) * 512], (), wu[sl % 2].b)
            DMA("pool", wd[sl % 2][:], wdn_v[:, sl * 4:(sl + 1) * 4, :], (), wd[sl % 2].b)

        load_mlp(0)
        for sl in range(8):
            u, dd = wu[sl % 2], wd[sl % 2]
            if sl + 1 < 8:
                load_mlp(sl + 1)
            for ti in tiles_out:
                s0, n, j = TILES[ti]
                a = act[cnt % 2]
                cnt += 1
                if mg is not None and cnt % 2 == 0:
                    if next(mg, "done") == "done":
                        mg = None
                for fc in range(4):
                    ps = P32()
                    for k in range(8):
                        MM(ps[:, 0:n], u[:, k, fc * 128:(fc + 1) * 128], hT[:, k, s0:s0 + n], k == 0, k == 7,
                           u.b + [hT.b[ti]], ps.b)
                    r_ = rl[fc % 2]
                    ACT(r_[:, 0:n], ps[:, 0:n], AF.Relu, ps.b, r_.b)
                    TT("pool", a[:, fc, 0:n], r_[:, 0:n], r_[:, 0:n], ALU.mult, r_.b, a.b)
                for c in range(8):
                    ps = P32()
                    for fc in range(4):
                        MM(ps[:, 0:n], dd[:, fc, c * 128:(c + 1) * 128], a[:, fc, 0:n], fc == 0, fc == 3, dd.b + a.b, ps.b)
                    STT(xT[:, c, s0:s0 + n], ps[:, 0:n], mp[:, 40 + c, j:j + 1], xT[:, c, s0:s0 + n], ALU.mult, ALU.add,
                        ps.b + mp.b + xT.b, xT.b)
        if mg is not None:
            for _ in mg:
                pass
        S.fence()
        RW.reset()
        RY.reset()
        yT = RY.alloc("yT", [128, 12, NT], BF16, nb=1)


    try:
        for l in range(depth):
            layer(l)
    except _Stop:
        S.fence()
        RW.reset()

    xo = [RW.alloc(f"xo{i}", [128, D], F32) for i in range(3)]
    finals = []
    for tb in range(2, NB):
        o = xo[tb % 3]
        for hf in range(2):
            ps = P32()
            for j in range(4):
                k = hf * 4 + j
                TR(ps[:, j * 128:(j + 1) * 128], xT[:, k, tb * 128:(tb + 1) * 128], ident_f, xT.b + CB, ps.b)
            CP("act" if hf else "dve", o[:, hf * 512:(hf + 1) * 512], ps[:, 0:512], ps.b, o.b)
        finals.append(DMA("sp", out_d[(tb - 2) * 128:(tb - 1) * 128, :], o[:], o.b, ()))
    finals += list(dbg_out.values())
    S.emit(finals)
    return S


def _host_constants():
    s = np.arange(128)[:, None]
    t = np.arange(128)[None, :]
    ident = np.eye(128, dtype=np.float32)
    triF = (s <= t).astype(np.float32)
    triR = (s >= t).astype(np.float32)
    mnegF = np.where(s <= t, 0.0, NEG).astype(np.float32)
    mnegR = np.where(s >= t, 0.0, NEG).astype(np.float32)
    ones = np.ones((128, 128), np.float32)
    cst = np.concatenate([ident, triF, triR, mnegF, mnegR, ones], axis=1)
    m01 = np.concatenate([np.tile(triR, (1, 4)), np.tile(triF, (1, 4))], axis=1).astype(np.float32)
    pos = np.arange(SEQ)
    rows, cols = pos // 64, pos % 64
    inv = 10000.0 ** (-np.arange(16, dtype=np.float32) / 16)
    ang = np.stack([rows[:, None] * inv[None, :], cols[:, None] * inv[None, :]], axis=1).astype(np.float32)
    ang = ang.reshape(16, 128, 2, 16).transpose(1, 0, 2, 3).reshape(128, 512)
    rope = np.concatenate([np.cos(ang), np.sin(ang)], axis=1).astype(np.float32)
    return cst, m01, rope


_CACHE = {}


def kernel(x, c, ctx, c_ctx, w_mod, b_mod, norm1_g, w_in, ml_b_i, ml_b_f, ml_norm_g,
           ssd_conv_w, ssd_conv_b, ssd_a_log, ssd_dt_bias, ssd_d, ssd_norm_g,
           att_qn_g, att_kn_g, att_sink, w_out, norm2_g, w_up, w_down, _dbg=(), _cores=8, _stop=None):
    f = lambda a: np.ascontiguousarray(np.asarray(a, dtype=np.float32))
    x, c, ctx, c_ctx = f(x), f(c), f(ctx), f(c_ctx)
    depth = w_mod.shape[0]
    cst, m01, rope = _host_constants()
    pk = lambda v: f(v).reshape(-1, 128).T
    bmod = np.stack([pk(b_mod[l]) for l in range(depth)])
    g1 = np.stack([pk(norm1_g[l]) for l in range(depth)])
    g2 = np.stack([pk(norm2_g[l]) for l in range(depth)])
    rowv = np.zeros((depth, 1, 1280), np.float32)
    convp = np.zeros((depth, 128, 24), np.float32)
    for l in range(depth):
        r = rowv[l, 0]
        r[0:8] = f(ml_b_i[l]).reshape(-1)
        r[8:16] = f(ml_b_f[l]).reshape(-1)
        r[16:528] = f(ml_norm_g[l])
        r[528:544] = f(ssd_a_log[l]).reshape(-1)
        r[544:560] = f(ssd_dt_bias[l]).reshape(-1)
        r[560:568] = f(ssd_d[l])
        r[568:1080] = f(ssd_norm_g[l])
        r[1080:1144] = f(att_qn_g[l])
        r[1144:1208] = f(att_kn_g[l])
        r[1208:1216] = f(att_sink[l])
        cw = f(ssd_conv_w[l])
        cb = f(ssd_conv_b[l])
        for ch in range(6):
            convp[l, :, ch * 4 + 0] = cw[0, ch * 128:(ch + 1) * 128]
            convp[l, :, ch * 4 + 1] = cw[1, ch * 128:(ch + 1) * 128]
            convp[l, :, ch * 4 + 2] = cw[2, ch * 128:(ch + 1) * 128]
            convp[l, :, ch * 4 + 3] = cb[ch * 128:(ch + 1) * 128]
    key = (depth, tuple(_dbg))
    nc = bass.Bass("TRN2", target_bir_lowering=False)
    build(nc, depth=depth, dbg=_dbg, stop_after=_stop)
    shared = {"w_mod": f(w_mod), "b_mod": bmod, "norm1_g": g1, "norm2_g": g2, "w_in": f(w_in), "w_out": f(w_out),
              "w_up": f(w_up), "w_down": f(w_down), "rowv": rowv, "convp": convp, "cst": cst, "m01": m01, "rope": rope}
    in_maps = []
    for b in range(_cores):
        cc = np.stack([pk(c[b]), pk(c_ctx)], axis=2).reshape(128, 16)
        m = dict(shared)
        m.update({"x": x[b], "ctx": ctx[b], "cc": np.ascontiguousarray(cc)})
        in_maps.append(m)
    res = run_bass_kernel_spmd(nc, in_maps, core_ids=list(range(_cores)))
    out = np.stack([res.results[b]["out"] for b in range(_cores)], axis=0).astype(np.float32)
    if _dbg:
        return out, res.results
    return out
```
